# Optimizing a Trainium2 kernel written in Bass

```python
import jax
import jax.numpy as jnp
from jax import lax
import numpy as np

D_MODEL = 1024
BATCH = 8
SEQ = 2048
DEPTH = 1

CTX_LEN = 256
GRID_W = 64
N_MOD = 6
EPS = 1e-6

LRU_WIDTH = 1024
LRU_BLOCKS = 8
LRU_BLOCK = LRU_WIDTH // LRU_BLOCKS
CONV_WIDTH = 4
CONV_LEFT = 2
LRU_C = 8.0

HG_HEADS = 8
HG_DK = 128
HG_DV = 128
HG_QK_WIDTH = HG_HEADS * HG_DK
HG_V_WIDTH = HG_HEADS * HG_DV
HG_CHUNK = 64

N_EXPERTS = 32
TOP_K = 4
D_FF = 1024
SWIGLU_LIMIT = 7.0
SWIGLU_ALPHA = 1.702
MOE_BLOCK = 128

IN_SIZES = (LRU_WIDTH, LRU_WIDTH, HG_QK_WIDTH, HG_QK_WIDTH, HG_QK_WIDTH, HG_V_WIDTH, HG_V_WIDTH, D_MODEL, D_MODEL)
IN_COLS = sum(IN_SIZES)

kernel_name = "hybrid_rglru_hgrn2_moe_dit_block"


def rms_norm(x, g):
    xf = x.astype(jnp.float32)
    y = xf * lax.rsqrt(jnp.mean(xf * xf, axis=-1, keepdims=True) + EPS)
    return (y * g.astype(jnp.float32)).astype(x.dtype)


def split_in(p):
    offs = np.cumsum(IN_SIZES)[:-1].tolist()
    return jnp.split(p, offs, axis=-1)


def split_heads(t):
    return t.reshape(t.shape[0], t.shape[1], HG_HEADS, t.shape[2] // HG_HEADS)


def short_conv(u, w, b, row_len):
    L = u.shape[1]
    up = jnp.pad(u, ((0, 0), (CONV_LEFT, CONV_WIDTH - 1 - CONV_LEFT), (0, 0)))
    col = jnp.arange(L) % row_len
    out = b
    for k in range(CONV_WIDTH):
        d = k - CONV_LEFT
        valid = ((col + d >= 0) & (col + d < row_len))[None, :, None]
        out = out + jnp.where(valid, up[:, k:k + L], 0) * w[k]
    return out


def linear_scan(a, b, h0, reverse):
    if reverse:
        a, b = a[:, ::-1], b[:, ::-1]
    b = b.at[:, 0].add(a[:, 0] * h0)
    _, h = lax.associative_scan(lambda l, r: (l[0] * r[0], r[0] * l[1] + r[1]), (a, b), axis=1)
    h_last = h[:, -1]
    if reverse:
        h = h[:, ::-1]
    return h, h_last


def rglru_direction(u, h0, wa, ba, wx, bx, lam, reverse):
    B, L, W = u.shape
    ub = u.reshape(B, L, LRU_BLOCKS, LRU_BLOCK)
    r = jax.nn.sigmoid(jnp.einsum("blgi,gij->blgj", ub, wa) + ba).reshape(B, L, W)
    i = jax.nn.sigmoid(jnp.einsum("blgi,gij->blgj", ub, wx) + bx).reshape(B, L, W)
    log_a = -LRU_C * r * jax.nn.softplus(-lam)
    a = jnp.exp(log_a)
    mult = jnp.sqrt(-jnp.expm1(2.0 * log_a))
    return linear_scan(a, mult * (i * u), h0, reverse)


def rglru_mixer(ax_c, ax_l, conv_w, conv_b, wa, ba, wx, bx, lam):
    u_c = short_conv(ax_c, conv_w, conv_b, ax_c.shape[1])
    u_l = short_conv(ax_l, conv_w, conv_b, GRID_W)
    h0 = jnp.zeros((u_c.shape[0], LRU_WIDTH), u_c.dtype)
    y_c, y_l = [], []
    for d in range(2):
        rev = d == 1
        hc, hc_last = rglru_direction(u_c, h0, wa[d], ba[d], wx[d], bx[d], lam[d], rev)
        hl, _ = rglru_direction(u_l, hc_last, wa[d], ba[d], wx[d], bx[d], lam[d], rev)
        y_c.append(hc)
        y_l.append(hl)
    return y_c[0] + y_c[1], y_l[0] + y_l[1]


def hgrn2_chunk_scan(q, logf, k, v, s0, reverse):
    if reverse:
        q, logf, k, v = (t[:, ::-1] for t in (q, logf, k, v))
    B, L, H, _ = q.shape
    n_chunks = L // HG_CHUNK
    q, logf, k, v = (t.reshape(B, n_chunks, HG_CHUNK, H, t.shape[-1]) for t in (q, logf, k, v))
    g_cum = jnp.cumsum(logf, axis=2)
    g_mid = g_cum[:, :, HG_CHUNK // 2 - 1:HG_CHUNK // 2]
    g_end = g_cum[:, :, -1]
    scores = jnp.einsum("bnthd,bnshd->bnhts", q * jnp.exp(g_cum - g_mid), k * jnp.exp(g_mid - g_cum))
    lower_tri = jnp.tril(jnp.ones((HG_CHUNK, HG_CHUNK), bool))
    scores = jnp.where(lower_tri, scores, 0)
    o_intra = jnp.einsum("bnhts,bnshe->bnthe", scores, v)
    upd = jnp.einsum("bnshd,bnshe->bnhde", k * jnp.exp(g_end[:, :, None] - g_cum), v)
    decay = jnp.exp(g_end)

    def step(s, inp):
        dec, u = inp
        return dec[..., None] * s + u, s

    s_last, s_prev = lax.scan(step, s0, (jnp.moveaxis(decay, 1, 0), jnp.moveaxis(upd, 1, 0)))
    o_inter = jnp.einsum("bnthd,nbhde->bnthe", q * jnp.exp(g_cum), s_prev)
    o = (o_intra + o_inter).reshape(B, L, H, v.shape[-1])
    if reverse:
        o = o[:, ::-1]
    return o, s_last


def hgrn2_forget(f_raw, lb):
    f = lb + (1.0 - lb) * jax.nn.sigmoid(f_raw.astype(jnp.float32))
    return split_heads(jnp.log(f)), split_heads(1.0 - f)


def hgrn2_mixer(p_c, p_l, lb, norm_g):
    q_c, ff_c, fb_c, i_c, g_c = p_c
    q_l, ff_l, fb_l, i_l, g_l = p_l
    qh_c, vh_c = split_heads(jax.nn.silu(q_c)), split_heads(i_c)
    qh_l, vh_l = split_heads(jax.nn.silu(q_l)), split_heads(i_l)
    s0 = jnp.zeros((q_c.shape[0], HG_HEADS, HG_DK, HG_DV), jnp.float32)
    o_c, o_l = [], []
    for d, (f_c, f_l) in enumerate(((ff_c, ff_l), (fb_c, fb_l))):
        rev = d == 1
        logf_c, k_c = hgrn2_forget(f_c, lb)
        logf_l, k_l = hgrn2_forget(f_l, lb)
        oc, s_ctx = hgrn2_chunk_scan(qh_c, logf_c, k_c, vh_c, s0, rev)
        ol, _ = hgrn2_chunk_scan(qh_l, logf_l, k_l, vh_l, s_ctx, rev)
        o_c.append(oc)
        o_l.append(ol)

    def readout(o, g):
        y = rms_norm(o, norm_g) * jax.nn.silu(split_heads(g))
        return y.reshape(g.shape).astype(g.dtype)

    return readout(o_c[0] + o_c[1], g_c), readout(o_l[0] + o_l[1], g_l)


def moe(t, rw, rb, w1, b1, w2, b2):
    T, D = t.shape
    logits = (t @ rw + rb).astype(jnp.float32)
    top_val, top_idx = lax.top_k(logits, TOP_K)
    gates = jax.nn.softmax(top_val, axis=-1)
    n_assign = T * TOP_K
    e_flat = top_idx.reshape(n_assign)
    order = jnp.argsort(e_flat)
    e_sorted = e_flat[order]
    tok_sorted = (order // TOP_K).astype(jnp.int32)
    gate_sorted = gates.reshape(n_assign)[order]
    counts = jnp.bincount(e_flat, length=N_EXPERTS)
    padded = (counts + MOE_BLOCK - 1) // MOE_BLOCK * MOE_BLOCK
    start = jnp.cumsum(counts) - counts
    pend = jnp.cumsum(padded)
    pstart = pend - padded
    dest = pstart[e_sorted] + jnp.arange(n_assign) - start[e_sorted]
    n_blocks = -(-n_assign // MOE_BLOCK) + N_EXPERTS
    n_slots = n_blocks * MOE_BLOCK
    slot_tok = jnp.zeros((n_slots,), jnp.int32).at[dest].set(tok_sorted)
    slot_gate = jnp.zeros((n_slots,), jnp.float32).at[dest].set(gate_sorted)
    block_start = jnp.arange(n_blocks) * MOE_BLOCK
    block_expert = jnp.minimum(jnp.sum(block_start[:, None] >= pend[None, :], axis=1), N_EXPERTS - 1)

    def expert_block(args):
        tok, e = args
        h = t[tok] @ w1[e] + b1[e]
        hg = jnp.minimum(h[:, 0::2], SWIGLU_LIMIT)
        hu = jnp.clip(h[:, 1::2], -SWIGLU_LIMIT, SWIGLU_LIMIT)
        act = hg * jax.nn.sigmoid(SWIGLU_ALPHA * hg) * (hu + 1)
        return act @ w2[e] + b2[e]

    y = lax.map(expert_block, (slot_tok.reshape(n_blocks, MOE_BLOCK), block_expert))
    y = y.reshape(n_slots, D) * slot_gate[:, None].astype(t.dtype)
    return jnp.zeros_like(t).at[slot_tok].add(y)


def mixer_sublayer(x, xc, mod_l, mod_c, norm_g, w_in, conv_w, conv_b, wa, ba, wx, bx, lam,
                   lb, hg_norm_g, w_ba, w_bb, w_out, update_ctx):
    h_l = rms_norm(x, norm_g) * (1 + mod_l[1]) + mod_l[0]
    h_c = rms_norm(xc, norm_g) * (1 + mod_c[1]) + mod_c[0]
    ax_l, ag_l, q_l, ff_l, fb_l, i_l, g_l, ma_l, mb_l = split_in(h_l @ w_in)
    ax_c, ag_c, q_c, ff_c, fb_c, i_c, g_c, ma_c, mb_c = split_in(h_c @ w_in)
    ya_c, ya_l = rglru_mixer(ax_c, ax_l, conv_w, conv_b, wa, ba, wx, bx, lam)
    yb_c, yb_l = hgrn2_mixer((q_c, ff_c, fb_c, i_c, g_c), (q_l, ff_l, fb_l, i_l, g_l), lb, hg_norm_g)

    def merge(ya, a_gate, yb, ma, mb):
        za = (ya * jax.nn.gelu(a_gate)) @ w_ba
        zb = yb @ w_bb
        return (jax.nn.sigmoid(ma) * za + jax.nn.sigmoid(mb) * zb) @ w_out

    x = x + mod_l[2] * merge(ya_l, ag_l, yb_l, ma_l, mb_l)
    if update_ctx:
        xc = xc + mod_c[2] * merge(ya_c, ag_c, yb_c, ma_c, mb_c)
    return x, xc


def moe_sublayer(x, xc, mod_l, mod_c, norm_g, rw, rb, w1, b1, w2, b2, update_ctx):
    h_l = (rms_norm(x, norm_g) * (1 + mod_l[4]) + mod_l[3]).reshape(-1, D_MODEL)
    if update_ctx:
        h_c = (rms_norm(xc, norm_g) * (1 + mod_c[4]) + mod_c[3]).reshape(-1, D_MODEL)
        y = moe(jnp.concatenate([h_l, h_c], axis=0), rw, rb, w1, b1, w2, b2)
        n_l = h_l.shape[0]
        x = x + mod_l[5] * y[:n_l].reshape(x.shape)
        xc = xc + mod_c[5] * y[n_l:].reshape(xc.shape)
    else:
        x = x + mod_l[5] * moe(h_l, rw, rb, w1, b1, w2, b2).reshape(x.shape)
    return x, xc


def setup_inputs(seed: int = 0) -> dict:
    key = jax.random.key(seed)
    ks = jax.random.split(key, 32)
    D = D_MODEL

    def nrm(k, shape, scale):
        return jax.random.normal(k, shape, jnp.float32) * scale

    lam_u = jax.random.uniform(ks[14], (DEPTH, 2, LRU_WIDTH), jnp.float32, 0.9, 0.999)
    a_base = lam_u ** (1.0 / LRU_C)
    lru_lam = jnp.log(a_base) - jnp.log1p(-a_base)
    return {
        "x": nrm(ks[0], (BATCH, SEQ, D), 1.0),
        "c": nrm(ks[1], (BATCH, D), 1.0),
        "ctx": nrm(ks[2], (BATCH, CTX_LEN, D), 1.0),
        "c_ctx": nrm(ks[3], (D,), 1.0),
        "ada_w": nrm(ks[4], (DEPTH, D, N_MOD * D), 0.5 * D ** -0.5),
        "ada_b": nrm(ks[5], (DEPTH, N_MOD * D), 0.01),
        "norm1_g": 1.0 + nrm(ks[6], (DEPTH, D), 0.01),
        "norm2_g": 1.0 + nrm(ks[7], (DEPTH, D), 0.01),
        "w_in": nrm(ks[8], (DEPTH, D, IN_COLS), D ** -0.5),
        "lru_conv_w": nrm(ks[9], (DEPTH, CONV_WIDTH, LRU_WIDTH), CONV_WIDTH ** -0.5),
        "lru_conv_b": nrm(ks[10], (DEPTH, LRU_WIDTH), 0.01),
        "lru_wa": nrm(ks[11], (DEPTH, 2, LRU_BLOCKS, LRU_BLOCK, LRU_BLOCK), LRU_BLOCK ** -0.5),
        "lru_ba": nrm(ks[12], (DEPTH, 2, LRU_BLOCKS, LRU_BLOCK), 0.01),
        "lru_wx": nrm(ks[13], (DEPTH, 2, LRU_BLOCKS, LRU_BLOCK, LRU_BLOCK), LRU_BLOCK ** -0.5),
        "lru_bx": nrm(ks[15], (DEPTH, 2, LRU_BLOCKS, LRU_BLOCK), 0.01),
        "lru_lam": lru_lam,
        "hg_lb_logits": nrm(ks[16], (DEPTH + 1, HG_QK_WIDTH), 0.1),
        "hg_norm_g": 1.0 + nrm(ks[17], (DEPTH, HG_DV), 0.01),
        "w_branch_a": nrm(ks[18], (DEPTH, LRU_WIDTH, D), LRU_WIDTH ** -0.5),
        "w_branch_b": nrm(ks[19], (DEPTH, HG_V_WIDTH, D), HG_V_WIDTH ** -0.5),
        "w_out": nrm(ks[20], (DEPTH, D, D), D ** -0.5),
        "router_w": nrm(ks[21], (DEPTH, D, N_EXPERTS), D ** -0.5),
        "router_b": nrm(ks[22], (DEPTH, N_EXPERTS), 0.01),
        "moe_w1": nrm(ks[23], (DEPTH, N_EXPERTS, D, 2 * D_FF), D ** -0.5),
        "moe_b1": nrm(ks[24], (DEPTH, N_EXPERTS, 2 * D_FF), 0.01),
        "moe_w2": nrm(ks[25], (DEPTH, N_EXPERTS, D_FF, D), D_FF ** -0.5),
        "moe_b2": nrm(ks[26], (DEPTH, N_EXPERTS, D), 0.01),
        "final_g": 1.0 + nrm(ks[27], (D,), 0.01),
    }


def reference(x, c, ctx, c_ctx, ada_w, ada_b, norm1_g, norm2_g, w_in, lru_conv_w, lru_conv_b,
              lru_wa, lru_ba, lru_wx, lru_bx, lru_lam, hg_lb_logits, hg_norm_g, w_branch_a,
              w_branch_b, w_out, router_w, router_b, moe_w1, moe_b1, moe_w2, moe_b2, final_g):
    B = x.shape[0]
    lb_all = jnp.cumsum(jax.nn.softmax(hg_lb_logits.astype(jnp.float32), axis=0), axis=0)
    xc = ctx
    for layer in range(DEPTH):
        update_ctx = layer < DEPTH - 1
        mod_l = (jax.nn.silu(c) @ ada_w[layer] + ada_b[layer]).reshape(B, N_MOD, D_MODEL)
        mod_l = jnp.transpose(mod_l, (1, 0, 2))[:, :, None, :]
        mod_c = (jax.nn.silu(c_ctx) @ ada_w[layer] + ada_b[layer]).reshape(N_MOD, D_MODEL)[:, None, None, :]
        x, xc = mixer_sublayer(x, xc, mod_l, mod_c, norm1_g[layer], w_in[layer],
                               lru_conv_w[layer], lru_conv_b[layer], lru_wa[layer], lru_ba[layer],
                               lru_wx[layer], lru_bx[layer], lru_lam[layer], lb_all[layer],
                               hg_norm_g[layer], w_branch_a[layer], w_branch_b[layer], w_out[layer],
                               update_ctx)
        x, xc = moe_sublayer(x, xc, mod_l, mod_c, norm2_g[layer], router_w[layer], router_b[layer],
                             moe_w1[layer], moe_b1[layer], moe_w2[layer], moe_b2[layer], update_ctx)
    return rms_norm(x, final_g)
```

```python
import types
import numpy as np
from contextlib import ExitStack
import concourse.bass as bass
import concourse.mybir as mybir
from concourse.bass_utils import run_bass_kernel_spmd

F32 = mybir.dt.float32
BF16 = mybir.dt.bfloat16
I32 = mybir.dt.int32
AF = mybir.ActivationFunctionType
ALU = mybir.AluOpType
AXL = mybir.AxisListType

D = 1024
SEQ = 2048
CTX = 256
T = SEQ + CTX
NT = T // 128
NE = 32
EPS = 1e-6
NPASS = 64
PSZ = 256
NSLOT = NPASS * PSZ
ENGS = ("pe", "act", "dve", "pool", "sp")

CO = {}
_o = 0
for _n, _w in (("n1g", 8), ("n2g", 8), ("convw", 32), ("convb", 8), ("ba", 16), ("bx", 16), ("lam", 16),
               ("lb0", 8), ("lb1", 8), ("hgng", 1), ("adab", 48), ("b1g", 256), ("b1u", 256)):
    CO[_n] = _o
    _o += _w
NCP = _o


def _snap(fn):
    if fn.__closure__ is None:
        return fn
    cells = []
    for c in fn.__closure__:
        try:
            cells.append(types.CellType(c.cell_contents))
        except ValueError:
            cells.append(c)
    return types.FunctionType(fn.__code__, fn.__globals__, fn.__name__, fn.__defaults__, tuple(cells))


class Sched:
    def __init__(self, nc, stack):
        self.nc = nc
        self.stack = stack
        self.ops = {e: [] for e in ENGS}
        self.cnt = {e: 0 for e in ENGS}
        self.esem = {e: stack.enter_context(nc.semaphore("s_" + e)) for e in ENGS}
        self.known = {e: {} for e in ENGS}
        self.last_w = {}
        self.readers = {}
        self.dma_sems = {}

    def _need(self, reads, writes):
        need = []
        for k in reads:
            t = self.last_w.get(k)
            if t is not None:
                need.append(t)
        for k in writes:
            t = self.last_w.get(k)
            if t is not None:
                need.append(t)
            need.extend(self.readers.get(k, ()))
        return need

    def _emit_waits(self, eng, need):
        best = {}
        for (sem, val, src) in need:
            if src == eng and eng == "pe":
                continue
            key = id(sem)
            if key not in best or best[key][1] < val:
                best[key] = (sem, val)
        kn = self.known[eng]
        for key, (sem, val) in best.items():
            if kn.get(key, 0) >= val:
                continue
            kn[key] = val
            self.ops[eng].append(("wait", sem, val))

    def _commit(self, token, reads, writes):
        for k in reads:
            self.readers.setdefault(k, []).append(token)
        for k in writes:
            self.last_w[k] = token
            self.readers[k] = []

    def op(self, eng, fn, reads=(), writes=(), sig=True):
        fn = _snap(fn)
        self._emit_waits(eng, self._need(reads, writes))
        if sig:
            self.cnt[eng] += 1
            token = (self.esem[eng], self.cnt[eng], eng)
            self.ops[eng].append(("op", fn, self.esem[eng], 1))
        else:
            token = (self.esem[eng], self.cnt[eng] + 1, eng)
            self.ops[eng].append(("op", fn, None, 0))
        self._commit(token, reads, writes)
        return token

    def dma(self, eng, fn, semkey, reads=(), writes=()):
        fn = _snap(fn)
        if semkey not in self.dma_sems:
            self.dma_sems[semkey] = [self.stack.enter_context(self.nc.semaphore("d_%d" % len(self.dma_sems))), 0]
        ent = self.dma_sems[semkey]
        self._emit_waits(eng, self._need(reads, writes))
        ent[1] += 16
        token = (ent[0], ent[1], "dma")
        self.ops[eng].append(("op", fn, ent[0], 16))
        self._commit(token, reads, writes)
        return token

    def wait_all(self, eng, tokens):
        self._emit_waits(eng, tokens)

    def barrier(self):
        toks = [(self.esem[e], self.cnt[e], "bar") for e in ENGS if self.cnt[e] > 0]
        toks += [(v[0], v[1], "dma") for v in self.dma_sems.values()]
        for e in ENGS:
            self._emit_waits(e, toks)

    def emit(self):
        with self.nc.Block() as block:
            def mk(name):
                def body(e):
                    for item in self.ops[name]:
                        if item[0] == "wait":
                            e.wait_ge(item[1], item[2])
                        else:
                            ins = item[1](e)
                            if item[2] is not None:
                                ins.then_inc(item[2], item[3])
                return body
            block.tensor(mk("pe"))
            block.scalar(mk("act"))
            block.vector(mk("dve"))
            block.gpsimd(mk("pool"))
            block.sync(mk("sp"))


_BND = {}


def bnd(e, val):
    key = (id(e), val)
    if key not in _BND:
        r = e.alloc_register("bnd%d" % val)
        e.reg_mov(r, val)
        _BND[key] = r
    return _BND[key]


def chunk_range(c):
    if c == 0:
        return 0, 256
    return 256 + 512 * (c - 1), 256 + 512 * c


def build_nc(stage="full"):
    _BND.clear()
    nc = bass.Bass("TRN2", target_bir_lowering=False)

    def din(name, shape, dt=F32):
        return nc.dram_tensor(name, list(shape), dt, kind="ExternalInput").ap()

    xin = din("xin", [T, D])
    cvec = din("cvec", [128, 8, 2])
    cpack = din("cpack", [128, NCP])
    adaw = din("adaw", [6, 128, 8, 1024])
    adabrow = din("adabrow", [1, 6144])
    win = din("win", [8, 128, 7, 8, 128])
    win78 = din("win78", [8, 128, 2, 8, 128])
    lruw = din("lruw", [128, 8, 4, 128])
    wba = din("wba", [128, 8, 1024])
    wbb = din("wbb", [128, 8, 1024])
    wout = din("wout", [128, 8, 1024])
    fgrow = din("fgrow", [1, 1024])
    rw = din("rw", [128, 8, 32])
    rbrow = din("rbrow", [1, 32])
    w1g = din("w1g", [NE * 128, 8192])
    w1u = din("w1u", [NE * 128, 8192])
    w2 = din("w2", [NE * 128, 8192])
    b1gt = din("b1gt", [NE * 128, 8])
    b1ut = din("b1ut", [NE * 128, 8])
    iconst = din("iconst", [128, 256])
    b2 = din("b2", [NE, 1024])
    out = nc.dram_tensor("out", [SEQ, D], F32, kind="ExternalOutput").ap()
    dk = "Internal" if stage == "full" else "ExternalOutput"
    ya_d = nc.dram_tensor("ya_d", [8, 128, SEQ], BF16, kind=dk).ap()
    yb_d = nc.dram_tensor("yb_d", [8, 128, SEQ], BF16, kind=dk).ap()
    x1_d = nc.dram_tensor("x1_d", [SEQ, D], F32, kind=dk).ap()
    xn2_d = nc.dram_tensor("xn2_d", [SEQ, D], F32, kind="Internal").ap()
    acc_d = nc.dram_tensor("acc_d", [SEQ, D], F32, kind="Internal").ap()
    gates_d = nc.dram_tensor("gates_d", [SEQ * NE, 2], F32, kind="Internal").ap()
    slot_d = nc.dram_tensor("slot_d", [NSLOT, 2], F32, kind="Internal").ap()
    yslot_d = nc.dram_tensor("yslot_d", [NSLOT, D], F32, kind="Internal").ap()

    with ExitStack() as st:
        S = Sched(nc, st)

        def sb(name, shape, dt=F32):
            return st.enter_context(nc.sbuf_tensor(name, list(shape), dt))

        def finish():
            S.barrier()
            S.emit()
            return nc

        PS = [st.enter_context(nc.psum_tensor("ps%d" % i, [128, 512], F32)) for i in range(8)]
        ARENA = 176 * 1024
        AR = sb("AR", [128, ARENA // 2], BF16)

        class Region:
            def __init__(self, ranges):
                self.ranges = [[lo * 1024, hi * 1024] for lo, hi in ranges]

            def alloc(self, shape, dt=F32):
                nel = int(np.prod(shape[1:]))
                nb = nel * (4 if dt in (F32, I32) else 2)
                nb_al = (nb + 63) // 64 * 64
                for r in self.ranges:
                    if r[0] + nb_al <= r[1]:
                        off = r[0]
                        r[0] += nb_al
                        break
                else:
                    raise RuntimeError("arena region full for %s" % (shape,))
                v = AR[0:shape[0], off // 2:(off + nb) // 2]
                if dt in (F32, I32):
                    v = v.bitcast(dt)
                if len(shape) == 3:
                    v = v.rearrange("p (a b) -> p a b", b=shape[2])
                elif len(shape) == 4:
                    v = v.rearrange("p (a b c) -> p a b c", b=shape[2], c=shape[3])
                elif len(shape) == 5:
                    v = v.rearrange("p (a b c d) -> p a b c d", b=shape[2], c=shape[3], d=shape[4])
                return v

        CP = sb("CP", [128, NCP])
        CV = sb("CV", [128, 8, 2])
        SC = sb("SC", [128, 8, 2])
        ID32 = sb("ID32", [128, 128])
        ONESM = sb("ONESM", [128, 128])
        ONESROW = sb("ONESROW", [1, 128])
        TRI = sb("TRI", [128, 2, 128], BF16)
        MASKF = sb("MASKF", [128, 512], BF16)
        HMASK = sb("HMASK", [128, 2])
        MODT = sb("MODT", [128, 6, 8, 2])
        MODB = sb("MODB", [128, 2, 1024])
        S1 = sb("S1", [128, 8, 2])
        S2 = sb("S2", [128, 8])
        CL = sb("CL", [128, 16])
        CL2 = sb("CL2", [128, 16])
        LB = sb("LB", [128, 8])
        OML = sb("OML", [128, 8])
        NOML = sb("NOML", [128, 8])
        RWS = sb("RWS", [128, 8, 32])
        RBROW = sb("RBROW", [1, 32])
        GATES = sb("GATES", [128, 16, 32])
        SMALL = sb("SMALL", [128, 64])

        cpc = lambda name, i=0, n=1: CP[:, CO[name] + i: CO[name] + i + n]

        S.dma("sp", lambda e: e.dma_start(out=CP[:], in_=cpack), "c0", writes=["CP"])
        S.dma("sp", lambda e: e.dma_start(out=CV[:], in_=cvec), "c1", writes=["CV"])
        R0 = Region([(0, 140)])
        BIG = R0.alloc([128, 2, 8, 1024])
        SCB = R0.alloc([128, 8, 128])
        ADABROW = R0.alloc([1, 2, 1024])
        S.dma("sp", lambda e: e.dma_start(out=ADABROW[0:1, 0, :], in_=adabrow[0:1, 2048:3072]), "c2", writes=[("ADABROW", 0)])
        S.dma("sp", lambda e: e.dma_start(out=ADABROW[0:1, 1, :], in_=adabrow[0:1, 5120:6144]), "c2b", writes=[("ADABROW", 1)])
        S.dma("sp", lambda e: e.dma_start(out=RWS[:], in_=rw), "c3", writes=["RWS"])
        S.dma("sp", lambda e: e.dma_start(out=RBROW[:], in_=rbrow), "c4", writes=["RBROW"])

        S.op("pool", lambda e: e.memset(ID32[:], 0.0), writes=["ID32"])
        S.op("pool", lambda e: e.affine_select(out=ID32[:], in_=ID32[:], pattern=[[-1, 128]], compare_op=ALU.not_equal,
                                               fill=1.0, base=0, channel_multiplier=1), reads=["ID32"], writes=["ID32"])
        S.op("pool", lambda e: e.memset(ONESM[:], 1.0 / 128.0), writes=["ONESM"])
        S.op("pool", lambda e: e.memset(ONESROW[:], 1.0), writes=["ONESROW"])
        S.op("pool", lambda e: e.memset(MASKF[:], 1.0), writes=["MASKF"])
        S.op("pool", lambda e: e.memset(MASKF[:].rearrange("p (a b) -> p a b", b=64)[:, :, 0:1], 0.0),
             reads=["MASKF"], writes=["MASKF"])
        S.op("pool", lambda e: e.memset(HMASK[:], 0.0), writes=["HMASK"])
        S.op("pool", lambda e: e.memset(HMASK[0:64, 0:1], 1.0), reads=["HMASK"], writes=["HMASK"])
        S.op("pool", lambda e: e.memset(HMASK[64:128, 1:2], 1.0), reads=["HMASK"], writes=["HMASK"])
        S.op("pool", lambda e: e.memset(TRI[:], 0.0), writes=["TRI"])
        for blk in range(2):
            lo = blk * 64
            S.op("pool", lambda e, lo=lo: e.memset(TRI[lo:lo + 64, :, lo:lo + 64], 1.0), reads=["TRI"], writes=["TRI"])
        S.op("pool", lambda e: e.affine_select(out=TRI[:, 0, :], in_=TRI[:, 0, :], pattern=[[1, 128]], compare_op=ALU.is_ge,
                                               fill=0.0, base=0, channel_multiplier=-1), reads=["TRI"], writes=["TRI"])
        S.op("pool", lambda e: e.affine_select(out=TRI[:, 1, :], in_=TRI[:, 1, :], pattern=[[-1, 128]], compare_op=ALU.is_ge,
                                               fill=0.0, base=0, channel_multiplier=1), reads=["TRI"], writes=["TRI"])

        S.op("act", lambda e: e.activation(out=SC[:], in_=CV[:], func=AF.Silu), reads=["CV"], writes=["SC"])
        S.op("dve", lambda e: e.tensor_copy(out=SCB[:], in_=SC[:, :, 0:1].to_broadcast([128, 8, 128])), reads=["SC"], writes=["SCB"])
        S.op("act", lambda e: e.activation(out=CL[:], in_=cpc("lam", 0, 16), func=AF.Exp, scale=-1.0), reads=["CP"], writes=["CL"])
        S.op("act", lambda e: e.activation(out=CL[:], in_=CL[:], func=AF.Ln, bias=1.0, scale=1.0), reads=["CL"], writes=["CL"])
        S.op("dve", lambda e: e.tensor_scalar(out=CL2[:], in0=CL[:], scalar1=-16.0, scalar2=None, op0=ALU.mult), reads=["CL"], writes=["CL2"])
        S.op("dve", lambda e: e.tensor_scalar(out=CL[:], in0=CL[:], scalar1=-8.0, scalar2=None, op0=ALU.mult), reads=["CL", "CL2"], writes=["CL"])
        S.op("dve", lambda e: e.tensor_tensor(out=LB[:], in0=cpc("lb0", 0, 8), in1=cpc("lb1", 0, 8), op=ALU.subtract), reads=["CP"], writes=["LB"])
        S.op("act", lambda e: e.activation(out=LB[:], in_=LB[:], func=AF.Sigmoid), reads=["LB"], writes=["LB"])
        S.op("dve", lambda e: e.tensor_scalar(out=OML[:], in0=LB[:], scalar1=-1.0, scalar2=1.0, op0=ALU.mult, op1=ALU.add), reads=["LB"], writes=["OML"])
        S.op("dve", lambda e: e.tensor_scalar(out=NOML[:], in0=OML[:], scalar1=-1.0, scalar2=None, op0=ALU.mult), reads=["OML"], writes=["NOML"])

        HT = Region([(140, 176)]).alloc([128, 8, T], BF16)
        HT2 = Region([(64, 96)]).alloc([128, 8, SEQ], BF16)
        RX = Region([(104, 120)])
        XB = RX.alloc([128, 3, 1024])
        JUNK = RX.alloc([128, 1024])
        TMPn = [8]

        for j in range(6):
            buf = j % 2
            S.dma("sp", lambda e, j=j, buf=buf: e.dma_start(out=BIG[:, buf], in_=adaw[j]), "ada%d" % buf, writes=[("BIG", buf)])
            if j in (0, 1, 3, 4):
                pm = PS[j % 2]
                for fcn in range(8):
                    for kc in range(8):
                        S.op("pe", lambda e, pm=pm, fcn=fcn, kc=kc, buf=buf: e.matmul(
                            pm[:, fcn * 2:fcn * 2 + 2], lhsT=BIG[:, buf, kc, fcn * 128:(fcn + 1) * 128], rhs=SC[:, kc, :],
                            start=(kc == 0), stop=(kc == 7)),
                            reads=[("BIG", buf), "SC"], writes=[("PS", j % 2)], sig=(kc == 7))
                S.op("dve", lambda e, pm=pm, j=j: e.tensor_tensor(
                    out=MODT[:, j], in0=pm[:, 0:16].rearrange("p (a b) -> p a b", b=2),
                    in1=cpc("adab", j * 8, 8).unsqueeze(2).to_broadcast([128, 8, 2]), op=ALU.add),
                    reads=[("PS", j % 2), "CP"], writes=[("MODT", j)])
            else:
                jj = 0 if j == 2 else 1
                for nh in range(2):
                    pm = PS[2 + nh]
                    for kc in range(8):
                        S.op("pe", lambda e, pm=pm, kc=kc, buf=buf, nh=nh: e.matmul(
                            pm[:], lhsT=SCB[:, kc, :], rhs=BIG[:, buf, kc, nh * 512:(nh + 1) * 512], start=(kc == 0), stop=False),
                            reads=[("BIG", buf), "SCB"], writes=[("PS", 2 + nh)], sig=False)
                    S.op("pe", lambda e, pm=pm, j=j, nh=nh: e.matmul(
                        pm[:], lhsT=ONESROW[0:1, :], rhs=ADABROW[0:1, jj, nh * 512:(nh + 1) * 512], start=False, stop=True),
                        reads=["ONESROW", ("ADABROW", 0), ("ADABROW", 1)], writes=[("PS", 2 + nh)])
                    S.op("act", lambda e, pm=pm, jj=jj, nh=nh: e.copy(out=MODB[:, jj, nh * 512:(nh + 1) * 512], in_=pm[:]),
                         reads=[("PS", 2 + nh)], writes=[("MODB", jj, nh)])
        S.op("dve", lambda e: e.tensor_scalar(out=S1[:], in0=MODT[:, 1], scalar1=1.0, scalar2=None, op0=ALU.add), reads=[("MODT", 1)], writes=["S1"])
        S.op("dve", lambda e: e.tensor_tensor(out=S1[:], in0=S1[:], in1=cpc("n1g", 0, 8).unsqueeze(2).to_broadcast([128, 8, 2]), op=ALU.mult),
             reads=["S1", "CP"], writes=["S1"])
        S.op("dve", lambda e: e.tensor_scalar(out=S2[:], in0=MODT[:, 4, :, 0], scalar1=1.0, scalar2=None, op0=ALU.add), reads=[("MODT", 4)], writes=["S2"])
        S.op("dve", lambda e: e.tensor_tensor(out=S2[:], in0=S2[:], in1=cpc("n2g", 0, 8), op=ALU.mult), reads=["S2", "CP"], writes=["S2"])

        if stage == "p0":
            return finish()

        def norm_transpose(xt_ap, xkey, col, scale_ap_fn, bias_ap_fn, dst_fn, dst_keys_fn, extra=None, after_norm=None, skip_main=False):
            S.op("act", lambda e: e.activation(out=JUNK[:], in_=xt_ap, func=AF.Square), reads=[xkey], writes=["JUNK"])
            S.op("dve", lambda e: e.reduce_sum(out=SMALL[:, col:col + 1], in_=JUNK[:], axis=AXL.X), reads=["JUNK"], writes=[("SM", col)])
            S.op("act", lambda e: e.activation(out=SMALL[:, col:col + 1], in_=SMALL[:, col:col + 1], func=AF.Ln, bias=EPS, scale=1.0 / D),
                 reads=[("SM", col)], writes=[("SM", col)])
            S.op("act", lambda e: e.activation(out=SMALL[:, col:col + 1], in_=SMALL[:, col:col + 1], func=AF.Exp, scale=-0.5), reads=[("SM", col)], writes=[("SM", col)])
            S.op("dve", lambda e: e.tensor_scalar(out=xt_ap, in0=xt_ap, scalar1=SMALL[:, col:col + 1], scalar2=None, op0=ALU.mult),
                 reads=[xkey, ("SM", col)], writes=[xkey])
            if after_norm is not None:
                after_norm()
            if stage == "p1a":
                return
            for half in range(2):
                bank = 2 + half
                for q in range(4):
                    kc = half * 4 + q
                    S.op("pe", lambda e, kc=kc, q=q, bank=bank: e.transpose(out=PS[bank][:, q * 128:(q + 1) * 128],
                                                                          in_=xt_ap[:, kc * 128:(kc + 1) * 128], identity=ID32[:]),
                         reads=[xkey, "ID32"], writes=[("PS", bank)], sig=(q == 3))
                if stage == "p1b":
                    continue
                for q in range(4):
                    kc = half * 4 + q
                    src = PS[bank][:, q * 128:(q + 1) * 128]
                    if stage == "p1c" and q % 2 == 1:
                        continue
                    if stage == "p1d" and q % 2 == 0:
                        continue
                    if skip_main:
                        pass
                    elif (half == 0 if stage != "p1e" else q % 2 == 0):
                        S.op("act", lambda e, kc=kc, src=src: e.activation(out=dst_fn(kc), in_=src, func=AF.Identity,
                                                                         bias=bias_ap_fn(kc), scale=scale_ap_fn(kc)),
                             reads=[("PS", bank), "S1", "S2", ("MODT", 0), ("MODT", 3)], writes=dst_keys_fn(kc))
                    else:
                        S.op("dve", lambda e, kc=kc, src=src: e.tensor_scalar(out=dst_fn(kc), in0=src, scalar1=scale_ap_fn(kc),
                                                                            scalar2=bias_ap_fn(kc), op0=ALU.mult, op1=ALU.add),
                             reads=[("PS", bank), "S1", "S2", ("MODT", 0), ("MODT", 3)], writes=dst_keys_fn(kc))
                    if extra is not None:
                        extra(kc, src, bank)

        for tt in range(NT):
            xb = tt % 3
            S.dma("sp", lambda e, tt=tt, xb=xb: e.dma_start(out=XB[:, xb], in_=xin[tt * 128:(tt + 1) * 128, :]), "xb%d" % xb,
                  writes=[("XB", xb)])
            w = 1 if tt < 2 else 0
            norm_transpose(XB[:, xb], ("XB", xb), tt % 32,
                           lambda kc, w=w: S1[:, kc, w:w + 1], lambda kc, w=w: MODT[:, 0, kc, w:w + 1],
                           lambda kc, tt=tt: HT[:, kc, tt * 128:(tt + 1) * 128], lambda kc, tt=tt: [("HT", kc, tt)])

        if stage in ("p1", "p1a", "p1b", "p1c", "p1d", "p1e"):
            return finish()

        def ht_keys(t0, t1):
            return [("HT", kc, tt) for kc in range(8) for tt in range(t0 // 128, (t1 + 127) // 128)]

        S.barrier()
        RM = Region([(0, 140)])
        TMP = Region([(120, 136)]).alloc([128, 8, 512])
        RM = Region([(0, 120)])
        WG = RM.alloc([128, 1, 7, 8, 128], BF16)
        LW = RM.alloc([128, 1, 4, 128], BF16)
        U = RM.alloc([128, T])
        UB = RM.alloc([128, T], BF16)
        Y = RM.alloc([128, SEQ])
        GA = RM.alloc([128, SEQ], BF16)
        HC = RM.alloc([128, 2, CTX])
        HB = RM.alloc([128, 2, 512])
        TB = RM.alloc([128, 8, 512], BF16)
        YO = Region([(136, 140)]).alloc([128, 1, SEQ], BF16)
        QS = RM.alloc([128, SEQ], BF16)
        SGT = RM.alloc([128, SEQ], BF16)
        VT = RM.alloc([128, NT, 128], BF16)
        QM = RM.alloc([128, SEQ], BF16)
        KM = RM.alloc([128, SEQ], BF16)
        QG = RM.alloc([128, SEQ], BF16)
        KE = RM.alloc([128, T])
        KET = RM.alloc([128, 2, NT, 128], BF16)
        STMP = RM.alloc([128, 128])
        DEC = RM.alloc([128, 36])
        S32 = RM.alloc([128, 2, 128])
        S16 = RM.alloc([128, 32, 128], BF16)
        PT = RM.alloc([128, 3, 128], BF16)
        OF = RM.alloc([128, SEQ])

        tmp_i = [0]

        def tmp():
            i = tmp_i[0] % TMPn[0]
            tmp_i[0] += 1
            return TMP[:, i], ("TMP", i)

        tb_i = [0]

        def tbf():
            i = tb_i[0] % 8
            tb_i[0] += 1
            return TB[:, i], ("TB", i)

        pj_i = [0]

        def pj():
            i = pj_i[0] % 2
            pj_i[0] += 1
            return PS[i], ("PS", i)

        def proj(wbuf, slot, c):
            t0, t1 = chunk_range(c)
            n = t1 - t0
            ps, pk = pj()
            for kc in range(8):
                S.op("pe", lambda e, kc=kc, ps=ps: e.matmul(ps[:, 0:n], lhsT=WG[:, wbuf, slot, kc, :], rhs=HT[:, kc, t0:t1],
                                                          start=(kc == 0), stop=(kc == 7)),
                     reads=[("WG", wbuf)] + [("HT", kc, tt) for tt in range(t0 // 128, t1 // 128)], writes=[pk], sig=(kc == 7))
            return ps, pk, t0, t1, n

        ng = 8 if stage not in ("g1", "lru", "lruscan", "mix1", "h1", "h2", "h3", "h4", "h5") else 1
        for g in range(ng):
            wbuf = 0
            S.dma("pool", lambda e, g=g, wbuf=wbuf: e.dma_start(out=WG[:, wbuf], in_=win[g]), "wg%d" % wbuf, writes=[("WG", wbuf)])
            S.dma("pool", lambda e, g=g, wbuf=wbuf: e.dma_start(out=LW[:, wbuf], in_=lruw[:, g]), "lw%d" % wbuf, writes=[("LW", wbuf)])

            for c in range(5):
                ps, pk, t0, t1, n = proj(wbuf, 0, c)
                ax, axk = tmp()
                S.op("act", lambda e, ax=ax, ps=ps, n=n: e.copy(out=ax[:, 0:n], in_=ps[:, 0:n]), reads=[pk], writes=[axk])
                uk = ("U", c)
                S.op("dve", lambda e, ax=ax, t0=t0, t1=t1, n=n: e.tensor_scalar(
                    out=U[:, t0:t1], in0=ax[:, 0:n], scalar1=cpc("convw", 2 * 8 + g), scalar2=cpc("convb", g), op0=ALU.mult, op1=ALU.add),
                    reads=[axk, "CP"], writes=[uk])
                rw_ = 256 if c == 0 else 64
                for k, dlt in ((0, -2), (1, -1), (3, 1)):
                    lo, hi = max(0, -dlt), rw_ - max(0, dlt)
                    S.op("dve", lambda e, ax=ax, t0=t0, t1=t1, n=n, k=k, dlt=dlt, lo=lo, hi=hi, rw_=rw_: e.scalar_tensor_tensor(
                        out=U[:, t0:t1].rearrange("p (r c) -> p r c", c=rw_)[:, :, lo:hi],
                        in0=ax[:, 0:n].rearrange("p (r c) -> p r c", c=rw_)[:, :, lo + dlt:hi + dlt],
                        scalar=cpc("convw", k * 8 + g),
                        in1=U[:, t0:t1].rearrange("p (r c) -> p r c", c=rw_)[:, :, lo:hi], op0=ALU.mult, op1=ALU.add),
                        reads=[axk, uk, "CP"], writes=[uk])
                S.op("pool", lambda e, t0=t0, t1=t1: e.tensor_copy(out=UB[:, t0:t1], in_=U[:, t0:t1]), reads=[uk], writes=[("UB", c)])
                if c > 0:
                    ps, pk, t0, t1, n = proj(wbuf, 1, c)
                    l0 = t0 - CTX
                    ag, agk = tmp()
                    S.op("act", lambda e, ag=ag, ps=ps: e.copy(out=ag[:], in_=ps[:]), reads=[pk], writes=[agk])
                    z, zk = tmp()
                    S.op("pool", lambda e, ag=ag, z=z: e.tensor_tensor(out=z[:], in0=ag[:], in1=ag[:], op=ALU.mult), reads=[agk], writes=[zk])
                    S.op("pool", lambda e, z=z: e.tensor_scalar(out=z[:], in0=z[:], scalar1=0.044715, scalar2=1.0, op0=ALU.mult, op1=ALU.add),
                         reads=[zk], writes=[zk])
                    S.op("pool", lambda e, ag=ag, z=z: e.tensor_tensor(out=z[:], in0=z[:], in1=ag[:], op=ALU.mult), reads=[zk, agk], writes=[zk])
                    S.op("act", lambda e, z=z: e.activation(out=z[:], in_=z[:], func=AF.Sigmoid, scale=1.5957691216057308), reads=[zk], writes=[zk])
                    S.op("pool", lambda e, ag=ag, z=z, l0=l0: e.tensor_tensor(out=GA[:, l0:l0 + 512], in0=z[:], in1=ag[:], op=ALU.mult),
                         reads=[zk, agk], writes=[("GA", c)])
            for d in range(2):
                order = [0, 1, 2, 3, 4] if d == 0 else [0, 4, 3, 2, 1]
                prev_state = None
                def lstage_a(c):
                    t0, t1 = chunk_range(c)
                    n = t1 - t0
                    rps, rpk = pj()
                    S.op("pe", lambda e, rps=rps, t0=t0, t1=t1, n=n, d=d: e.matmul(rps[:, 0:n], lhsT=LW[:, wbuf, d * 2 + 0, :], rhs=UB[:, t0:t1],
                                                                                 start=True, stop=True),
                         reads=[("LW", wbuf), ("UB", c)], writes=[rpk])
                    ips, ipk = pj()
                    S.op("pe", lambda e, ips=ips, t0=t0, t1=t1, n=n, d=d: e.matmul(ips[:, 0:n], lhsT=LW[:, wbuf, d * 2 + 1, :], rhs=UB[:, t0:t1],
                                                                                 start=True, stop=True),
                         reads=[("LW", wbuf), ("UB", c)], writes=[ipk])
                    rt, rk = tmp()
                    it, ik = tmp()
                    S.op("act", lambda e, rt=rt, rps=rps, n=n, d=d: e.activation(out=rt[:, 0:n], in_=rps[:, 0:n], func=AF.Sigmoid,
                                                                               bias=cpc("ba", d * 8 + g), scale=1.0), reads=[rpk, "CP"], writes=[rk])
                    S.op("act", lambda e, it=it, ips=ips, n=n, d=d: e.activation(out=it[:, 0:n], in_=ips[:, 0:n], func=AF.Sigmoid,
                                                                               bias=cpc("bx", d * 8 + g), scale=1.0), reads=[ipk, "CP"], writes=[ik])
                    return (t0, t1, n, rt, rk, it, ik)

                pend_l = lstage_a(order[0])
                for oi, c in enumerate(order):
                    t0, t1, n, rt, rk, it, ik = pend_l
                    pend_l = lstage_a(order[oi + 1]) if oi + 1 < len(order) else None
                    a2, a2k = tmp()
                    S.op("act", lambda e, rt=rt, a2=a2, n=n, d=d: e.activation(out=a2[:, 0:n], in_=rt[:, 0:n], func=AF.Exp,
                                                                             scale=CL2[:, d * 8 + g:d * 8 + g + 1]), reads=[rk, "CL2"], writes=[a2k])
                    S.op("act", lambda e, rt=rt, n=n, d=d: e.activation(out=rt[:, 0:n], in_=rt[:, 0:n], func=AF.Exp,
                                                                      scale=CL[:, d * 8 + g:d * 8 + g + 1]), reads=[rk, "CL"], writes=[rk])
                    S.op("act", lambda e, a2=a2, n=n: e.activation(out=a2[:, 0:n], in_=a2[:, 0:n], func=AF.Sqrt, bias=1.0, scale=-1.0),
                         reads=[a2k], writes=[a2k])
                    S.op("pool", lambda e, it=it, t0=t0, t1=t1, n=n: e.tensor_tensor(out=it[:, 0:n], in0=it[:, 0:n], in1=U[:, t0:t1], op=ALU.mult),
                         reads=[ik, ("U", c)], writes=[ik])
                    S.op("pool", lambda e, it=it, a2=a2, n=n: e.tensor_tensor(out=it[:, 0:n], in0=it[:, 0:n], in1=a2[:, 0:n], op=ALU.mult),
                         reads=[ik, a2k], writes=[ik])
                    if d == 0:
                        if c == 0:
                            dst, dk = HC[:, 0, :], ("HC", 0)
                        else:
                            dst, dk = Y[:, t0 - CTX:t1 - CTX], ("Y", c)
                        init = 0.0 if prev_state is None else prev_state[0]
                        rd = [rk, ik] + ([] if prev_state is None else [prev_state[1]])
                        S.op("dve", lambda e, dst=dst, rt=rt, it=it, n=n, init=init: e.tensor_tensor_scan(
                            out=dst, data0=rt[:, 0:n], data1=it[:, 0:n], initial=init, op0=ALU.mult, op1=ALU.add), reads=rd, writes=[dk])
                        prev_state = (dst[:, n - 1:n], dk)
                    else:
                        if c == 0:
                            dst, dk = HC[:, 1, :], ("HC", 1)
                        else:
                            hb = oi % 2
                            dst, dk = HB[:, hb, :], ("HB", hb)
                        init = 0.0 if prev_state is None else prev_state[0]
                        rd = [rk, ik] + ([] if prev_state is None else [prev_state[1]])
                        S.op("dve", lambda e, dst=dst, rt=rt, it=it, n=n, init=init: e.tensor_tensor_scan(
                            out=dst[:, ::-1], data0=rt[:, 0:n][:, ::-1], data1=it[:, 0:n][:, ::-1], initial=init, op0=ALU.mult, op1=ALU.add),
                            reads=rd, writes=[dk])
                        prev_state = (dst[:, 0:1], dk)
                        if c > 0:
                            S.op("pool", lambda e, dst=dst, t0=t0, t1=t1: e.tensor_tensor(out=Y[:, t0 - CTX:t1 - CTX], in0=Y[:, t0 - CTX:t1 - CTX],
                                                                                        in1=dst, op=ALU.add), reads=[dk, ("Y", c)], writes=[("Y", c)])
            if stage == "lruscan":
                return finish()
            for c in range(1, 5):
                l0 = (c - 1) * 512
                S.op("dve", lambda e, l0=l0: e.tensor_tensor(out=YO[:, 0, l0:l0 + 512], in0=Y[:, l0:l0 + 512], in1=GA[:, l0:l0 + 512], op=ALU.mult),
                     reads=[("Y", c), ("GA", c)], writes=[("YO", 0)])
            S.dma("sp", lambda e, g=g: e.dma_start(out=ya_d[g], in_=YO[:, 0, :]), "yo0", reads=[("YO", 0)], writes=[("YAD", g)])

            if stage == "lru":
                return finish()
            for c in range(1, 5):
                ps, pk, t0, t1, n = proj(wbuf, 2, c)
                l0 = t0 - CTX
                S.op("act", lambda e, ps=ps, l0=l0: e.activation(out=QS[:, l0:l0 + 512], in_=ps[:], func=AF.Silu), reads=[pk], writes=[("QS", c)])
                ps, pk, t0, t1, n = proj(wbuf, 6, c)
                S.op("act", lambda e, ps=ps, l0=l0: e.activation(out=SGT[:, l0:l0 + 512], in_=ps[:], func=AF.Silu), reads=[pk], writes=[("SGT", c)])
            for t4 in range(0, NT, 4):
                nt4 = min(4, NT - t4)
                for q in range(nt4):
                    tt = t4 + q
                    for kc in range(8):
                        S.op("pe", lambda e, q=q, tt=tt, kc=kc: e.matmul(PS[2][:, q * 128:(q + 1) * 128], lhsT=HT[:, kc, tt * 128:(tt + 1) * 128],
                                                                      rhs=WG[:, wbuf, 5, kc, :], start=(kc == 0), stop=(kc == 7)),
                             reads=[("WG", wbuf), ("HT", kc, tt)], writes=[("PS", 2)], sig=(kc == 7))
                S.op("act", lambda e, t4=t4, nt4=nt4: e.copy(out=VT[:, t4:t4 + nt4, :], in_=PS[2][:, 0:nt4 * 128].rearrange("p (a b) -> p a b", b=128)),
                     reads=[("PS", 2)], writes=[("VT", t4 // 4)])
            if stage == "h1":
                return finish()
            for dd in range(2):
                mid = 31 if dd == 0 else 32
                end = 63 if dd == 0 else 0
                def stage_a(c):
                    ps, pk, t0, t1, n = proj(wbuf, 3 + dd, c)
                    nch = n // 64
                    t1_, k1 = tmp()
                    S.op("act", lambda e, t1_=t1_, ps=ps, n=n: e.activation(out=t1_[:, 0:n], in_=ps[:, 0:n], func=AF.Sigmoid), reads=[pk], writes=[k1])
                    kb, kbk = tbf()
                    S.op("pool", lambda e, kb=kb, t1_=t1_, n=n: e.tensor_scalar(out=kb[:, 0:n], in0=t1_[:, 0:n], scalar1=NOML[:, g:g + 1],
                                                                              scalar2=OML[:, g:g + 1], op0=ALU.mult, op1=ALU.add),
                         reads=[k1, "NOML", "OML"], writes=[kbk])
                    S.op("dve", lambda e, t1_=t1_, n=n: e.tensor_scalar(out=t1_[:, 0:n], in0=t1_[:, 0:n], scalar1=OML[:, g:g + 1],
                                                                      scalar2=LB[:, g:g + 1], op0=ALU.mult, op1=ALU.add),
                         reads=[k1, kbk, "OML", "LB"], writes=[k1])
                    return (t0, t1, n, nch, t1_, k1, kb, kbk)

                pend_a = stage_a(0)
                for c in range(5):
                    t0, t1, n, nch, t1_, k1, kb, kbk = pend_a
                    pend_a = stage_a(c + 1) if c + 1 < 5 else None
                    S.op("act", lambda e, t1_=t1_, n=n: e.activation(out=t1_[:, 0:n], in_=t1_[:, 0:n], func=AF.Ln), reads=[k1], writes=[k1])
                    gg, gk = tmp()
                    if dd == 0:
                        S.op("dve", lambda e, gg=gg, t1_=t1_, n=n: e.tensor_tensor_scan(out=gg[:, 0:n], data0=MASKF[:, 0:n], data1=t1_[:, 0:n],
                                                                                      initial=0.0, op0=ALU.mult, op1=ALU.add),
                             reads=[k1, "MASKF"], writes=[gk])
                    else:
                        S.op("dve", lambda e, gg=gg, t1_=t1_, n=n: e.tensor_tensor_scan(out=gg[:, 0:n][:, ::-1], data0=MASKF[:, 0:n],
                                                                                      data1=t1_[:, 0:n][:, ::-1], initial=0.0, op0=ALU.mult, op1=ALU.add),
                             reads=[k1, "MASKF"], writes=[gk])
                    g3 = gg[:, 0:n].rearrange("p (a b) -> p a b", b=64)
                    d1, d1k = tmp()
                    d13 = d1[:, 0:n].rearrange("p (a b) -> p a b", b=64)
                    if c > 0:
                        l0 = t0 - CTX
                        S.op("dve", lambda e, d13=d13, g3=g3, nch=nch: e.tensor_tensor(out=d13, in0=g3, in1=g3[:, :, mid:mid + 1].to_broadcast([128, nch, 64]),
                                                                                     op=ALU.subtract), reads=[gk], writes=[d1k])
                        e1, e1k = tbf()
                        S.op("act", lambda e, e1=e1, d1=d1: e.activation(out=e1[:], in_=d1[:], func=AF.Exp), reads=[d1k], writes=[e1k])
                        S.op("pool", lambda e, e1=e1, l0=l0: e.tensor_tensor(out=QM[:, l0:l0 + 512], in0=QS[:, l0:l0 + 512], in1=e1[:], op=ALU.mult),
                             reads=[e1k, ("QS", c)], writes=[("QM", c)])
                        e2, e2k = tbf()
                        S.op("act", lambda e, e2=e2, d1=d1: e.activation(out=e2[:], in_=d1[:], func=AF.Exp, scale=-1.0), reads=[d1k], writes=[e2k])
                        S.op("pool", lambda e, e2=e2, kb=kb, l0=l0: e.tensor_tensor(out=KM[:, l0:l0 + 512], in0=kb[:], in1=e2[:], op=ALU.mult),
                             reads=[e2k, kbk], writes=[("KM", c)])
                        e4, e4k = tbf()
                        S.op("act", lambda e, e4=e4, gg=gg: e.activation(out=e4[:], in_=gg[:], func=AF.Exp), reads=[gk], writes=[e4k])
                        S.op("pool", lambda e, e4=e4, l0=l0: e.tensor_tensor(out=QG[:, l0:l0 + 512], in0=QS[:, l0:l0 + 512], in1=e4[:], op=ALU.mult),
                             reads=[e4k, ("QS", c)], writes=[("QG", c)])
                    d3, d3k = tmp()
                    d33 = d3[:, 0:n].rearrange("p (a b) -> p a b", b=64)
                    S.op("dve", lambda e, d33=d33, g3=g3, nch=nch: e.tensor_tensor(out=d33, in0=g3, in1=g3[:, :, end:end + 1].to_broadcast([128, nch, 64]),
                                                                                 op=ALU.subtract), reads=[gk], writes=[d3k])
                    S.op("act", lambda e, d3=d3, n=n: e.activation(out=d3[:, 0:n], in_=d3[:, 0:n], func=AF.Exp, scale=-1.0), reads=[d3k], writes=[d3k])
                    S.op("pool", lambda e, d3=d3, kb=kb, t0=t0, t1=t1, n=n: e.tensor_tensor(out=KE[:, t0:t1], in0=kb[:, 0:n], in1=d3[:, 0:n], op=ALU.mult),
                         reads=[d3k, kbk], writes=[("KE", c)])
                    ch0 = t0 // 64
                    S.op("act", lambda e, g3=g3, ch0=ch0, nch=nch: e.activation(out=DEC[:, ch0:ch0 + nch].unsqueeze(2), in_=g3[:, :, end:end + 1], func=AF.Exp),
                         reads=[gk], writes=[("DEC", c)])
                if stage == "h2":
                    return finish()
                for t4 in range(0, NT, 4):
                    nt4 = min(4, NT - t4)
                    for q in range(nt4):
                        tt = t4 + q
                        cc = 0 if tt < 2 else 1 + (tt - 2) // 4
                        S.op("pe", lambda e, q=q, tt=tt: e.transpose(out=PS[2][:, q * 128:(q + 1) * 128], in_=KE[:, tt * 128:(tt + 1) * 128], identity=ID32[:]),
                             reads=[("KE", cc), "ID32"], writes=[("PS", 2)], sig=(q == nt4 - 1))
                    for hh in range(2):
                        S.op("dve", lambda e, t4=t4, nt4=nt4, hh=hh: e.tensor_scalar(
                            out=KET[:, hh, t4:t4 + nt4, :], in0=PS[2][:, 0:nt4 * 128].rearrange("p (a b) -> p a b", b=128),
                            scalar1=HMASK[:, hh:hh + 1], scalar2=None, op0=ALU.mult),
                            reads=[("PS", 2), "HMASK"], writes=[("KET", hh, t4 // 4)])
                if stage == "h3":
                    return finish()
                order = list(range(36)) if dd == 0 else [3, 2, 1, 0] + list(range(35, 3, -1))
                S.op("pool", lambda e: e.memset(S32[:, 0, :], 0.0), writes=[("S32", 0)])
                for i, nchk in enumerate(order):
                    par = i % 2
                    tt, half = nchk // 2, nchk % 2
                    if nchk >= 4:
                        S.op("act", lambda e, par=par, nchk=nchk: e.copy(out=S16[:, nchk - 4, :], in_=S32[:, par, :]),
                             reads=[("S32", par)], writes=[("S16", nchk - 4)])
                    if i == len(order) - 1:
                        break
                    reg = i % 4
                    cc = 0 if tt < 2 else 1 + (tt - 2) // 4
                    S.op("pe", lambda e, reg=reg, tt=tt, half=half: e.matmul(PS[3][:, reg * 128:(reg + 1) * 128],
                                                                           lhsT=KET[:, half, tt, :], rhs=VT[:, tt, :],
                                                                           start=True, stop=True),
                         reads=[("KET", half, tt // 4), ("VT", tt // 4)], writes=[("PS", 3, reg)])
                    S.op("dve", lambda e, par=par, nchk=nchk: e.tensor_scalar(out=STMP[:], in0=S32[:, par, :], scalar1=DEC[:, nchk:nchk + 1],
                                                                            scalar2=None, op0=ALU.mult),
                         reads=[("S32", par), ("DEC", cc)], writes=["STMP"])
                    S.op("dve", lambda e, reg=reg, par=par: e.tensor_tensor(out=S32[:, 1 - par, :], in0=PS[3][:, reg * 128:(reg + 1) * 128], in1=STMP[:],
                                                                          op=ALU.add),
                         reads=["STMP", ("PS", 3, reg)], writes=[("S32", 1 - par)])
                if stage == "h4":
                    return finish()
                for lt in range(16):
                    tt = lt + 2
                    c = 1 + lt // 4
                    reg = lt % 4
                    bank = 4 + lt // 4
                    S.op("pe", lambda e, reg=reg, lt=lt: e.matmul(PS[3][:, reg * 128:(reg + 1) * 128], lhsT=KM[:, lt * 128:(lt + 1) * 128],
                                                                rhs=QM[:, lt * 128:(lt + 1) * 128], start=True, stop=True),
                         reads=[("KM", c), ("QM", c)], writes=[("PS", 3, reg)])
                    pi = lt % 3
                    S.op("dve", lambda e, reg=reg, pi=pi, dd=dd: e.tensor_tensor(out=PT[:, pi, :], in0=PS[3][:, reg * 128:(reg + 1) * 128], in1=TRI[:, dd, :],
                                                                               op=ALU.mult), reads=[("PS", 3, reg), "TRI"], writes=[("PT", pi)])
                    ot = PS[bank][:, reg * 128:(reg + 1) * 128]
                    otk = ("PS", bank, reg)
                    S.op("pe", lambda e, ot=ot, tt=tt, pi=pi, dd=dd: e.matmul(ot, lhsT=VT[:, tt, :], rhs=PT[:, pi, :], start=True, stop=False,
                                                                            skip_group_check=True),
                         reads=[("VT", tt // 4), ("PT", pi)], writes=[otk], sig=False)
                    nA, nB = 2 * tt - 4, 2 * tt - 3
                    S.op("pe", lambda e, ot=ot, nA=nA, lt=lt: e.matmul(ot[:, 0:64], lhsT=S16[:, nA, :], rhs=QG[:, lt * 128:lt * 128 + 64], start=False, stop=False,
                                                                     skip_group_check=True),
                         reads=[("S16", nA), ("QG", c)], writes=[otk], sig=False)
                    S.op("pe", lambda e, ot=ot, nB=nB, lt=lt, dd=dd: e.matmul(ot[:, 64:128], lhsT=S16[:, nB, :], rhs=QG[:, lt * 128 + 64:lt * 128 + 128],
                                                                            start=False, stop=True, skip_group_check=True),
                         reads=[("S16", nB), ("QG", c)], writes=[otk])
                if dd == 0:
                    for bq in range(4):
                        S.op("act", lambda e, bq=bq: e.copy(out=OF[:, bq * 512:(bq + 1) * 512], in_=PS[4 + bq][:]),
                             reads=[("PS", 4 + bq, r) for r in range(4)], writes=[("OF", bq)])
            if stage == "h5":
                return finish()
            for bq in range(4):
                bank = 4 + bq
                bkeys = [("PS", bank, r) for r in range(4)]
                osum, osumk = tmp()
                S.op("dve", lambda e, osum=osum, bank=bank, bq=bq: e.tensor_tensor(out=osum[:], in0=PS[bank][:], in1=OF[:, bq * 512:(bq + 1) * 512], op=ALU.add),
                     reads=bkeys + [("OF", bq)], writes=[osumk])
                osq, osqk = tmp()
                S.op("act", lambda e, osq=osq, osum=osum: e.activation(out=osq[:], in_=osum[:], func=AF.Square), reads=[osumk], writes=[osqk])
                ps, pk = pj()
                S.op("pe", lambda e, ps=ps, osq=osq: e.matmul(ps[:], lhsT=ONESM[:], rhs=osq[:], start=True, stop=True), reads=["ONESM", osqk], writes=[pk])
                rs, rsk = tmp()
                S.op("act", lambda e, rs=rs, ps=ps: e.activation(out=rs[:], in_=ps[:], func=AF.Ln, bias=EPS, scale=1.0), reads=[pk], writes=[rsk])
                S.op("act", lambda e, rs=rs: e.activation(out=rs[:], in_=rs[:], func=AF.Exp, scale=-0.5), reads=[rsk], writes=[rsk])
                S.op("dve", lambda e, rs=rs, osum=osum: e.tensor_tensor(out=rs[:], in0=osum[:], in1=rs[:], op=ALU.mult), reads=[osumk, rsk], writes=[rsk])
                S.op("dve", lambda e, rs=rs, bq=bq: e.scalar_tensor_tensor(out=YO[:, 0, bq * 512:(bq + 1) * 512], in0=rs[:], scalar=cpc("hgng"),
                                                                         in1=SGT[:, bq * 512:(bq + 1) * 512], op0=ALU.mult, op1=ALU.mult),
                     reads=[rsk, ("SGT", bq + 1), "CP"], writes=[("YO", 0)])
            S.dma("sp", lambda e, g=g: e.dma_start(out=yb_d[g], in_=YO[:, 0, :]), "yo0", reads=[("YO", 0)], writes=[("YBD", g)])

        if stage == "mix1":
            return finish()
        S.barrier()
        RG = Region([(0, 64), (96, 104), (120, 140)])
        TMP = Region([(120, 128)]).alloc([128, 4, 512])
        TMPn[0] = 4
        RG = Region([(0, 64), (96, 104), (128, 140)])
        WBA = RG.alloc([128, 8, 1024], BF16)
        WBB = RG.alloc([128, 8, 1024], BF16)
        WOUT = RG.alloc([128, 8, 1024], BF16)
        YAC = RG.alloc([128, 8, 512], BF16)
        YBC = RG.alloc([128, 8, 512], BF16)
        MIX = RG.alloc([128, 8, 512], BF16)
        H2F = RG.alloc([128, 8, 128])
        W78ALL = Region([(64, 96)]).alloc([128, 8, 2, 8, 128], BF16)
        LG = RG.alloc([128, 32])
        MX8 = RG.alloc([128, 8])
        S.dma("pool", lambda e: e.dma_start(out=WBA[:], in_=wba), "wm0", writes=["WBA"])
        S.dma("pool", lambda e: e.dma_start(out=WBB[:], in_=wbb), "wm1", writes=["WBB"])
        S.dma("pool", lambda e: e.dma_start(out=WOUT[:], in_=wout), "wm2", writes=["WOUT"])
        for hf in range(2):
            S.dma("pool", lambda e, hf=hf: e.dma_start(out=W78ALL[:, hf * 4:(hf + 1) * 4], in_=win78[hf * 4:(hf + 1) * 4].rearrange("m p s k j -> p m s k j")),
                  "w78%d" % hf, writes=[("W78ALL", hf)])
        yad_keys = [("YAD", g) for g in range(ng)]
        ybd_keys = [("YBD", g) for g in range(ng)]
        for tc in range(4):
            S.dma("sp", lambda e, tc=tc: e.dma_start(out=YAC[:], in_=ya_d[:, :, tc * 512:(tc + 1) * 512].rearrange("g p t -> p g t")), "yac",
                  reads=yad_keys, writes=["YAC"])
            S.dma("sp", lambda e, tc=tc: e.dma_start(out=YBC[:], in_=yb_d[:, :, tc * 512:(tc + 1) * 512].rearrange("g p t -> p g t")), "ybc",
                  reads=ybd_keys, writes=["YBC"])
            t0 = CTX + tc * 512
            for mc in range(8):
                for kc in range(8):
                    S.op("pe", lambda e, kc=kc, mc=mc: e.matmul(PS[0][:], lhsT=WBA[:, kc, mc * 128:(mc + 1) * 128], rhs=YAC[:, kc, :], start=(kc == 0), stop=(kc == 7)),
                         reads=["WBA", "YAC"], writes=[("PS", 0)], sig=(kc == 7))
                for kc in range(8):
                    S.op("pe", lambda e, kc=kc, mc=mc: e.matmul(PS[1][:], lhsT=WBB[:, kc, mc * 128:(mc + 1) * 128], rhs=YBC[:, kc, :], start=(kc == 0), stop=(kc == 7)),
                         reads=["WBB", "YBC"], writes=[("PS", 1)], sig=(kc == 7))
                for which in range(2):
                    for kc in range(8):
                        S.op("pe", lambda e, kc=kc, mc=mc, which=which: e.matmul(PS[4 + which][:], lhsT=W78ALL[:, mc, which, kc, :], rhs=HT[:, kc, t0:t0 + 512],
                                                                              start=(kc == 0), stop=(kc == 7)),
                             reads=[("W78ALL", mc // 4)] + [("HT", kc, tt) for tt in range(t0 // 128, t0 // 128 + 4)], writes=[("PS", 4 + which)], sig=(kc == 7))
                sa, sak = tmp()
                sbb, sbk = tmp()
                S.op("act", lambda e, sa=sa: e.activation(out=sa[:], in_=PS[4][:], func=AF.Sigmoid), reads=[("PS", 4)], writes=[sak])
                S.op("act", lambda e, sbb=sbb: e.activation(out=sbb[:], in_=PS[5][:], func=AF.Sigmoid), reads=[("PS", 5)], writes=[sbk])
                S.op("dve", lambda e, sa=sa: e.tensor_tensor(out=sa[:], in0=PS[0][:], in1=sa[:], op=ALU.mult), reads=[("PS", 0), sak], writes=[sak])
                S.op("dve", lambda e, sbb=sbb: e.tensor_tensor(out=sbb[:], in0=PS[1][:], in1=sbb[:], op=ALU.mult), reads=[("PS", 1), sbk], writes=[sbk])
                S.op("pool", lambda e, sa=sa, sbb=sbb, mc=mc: e.tensor_tensor(out=MIX[:, mc, :], in0=sa[:], in1=sbb[:], op=ALU.add),
                     reads=[sak, sbk], writes=[("MIX", mc)])
            for l4 in range(4):
                lt = tc * 4 + l4
                xb = lt % 3
                S.dma("sp", lambda e, lt=lt, xb=xb: e.dma_start(out=XB[:, xb], in_=xin[CTX + lt * 128:CTX + (lt + 1) * 128, :]), "xb%d" % xb,
                      writes=[("XB", xb)])
                for nh in range(2):
                    bank = 6 + nh
                    for mc in range(8):
                        S.op("pe", lambda e, mc=mc, nh=nh, l4=l4, bank=bank: e.matmul(PS[bank][:], lhsT=MIX[:, mc, l4 * 128:(l4 + 1) * 128],
                                                                                   rhs=WOUT[:, mc, nh * 512:(nh + 1) * 512], start=(mc == 0), stop=(mc == 7)),
                             reads=["WOUT", ("MIX", mc)], writes=[("PS", bank)], sig=(mc == 7))
                    mo, mok = tmp()
                    S.op("dve", lambda e, mo=mo, bank=bank, nh=nh: e.tensor_tensor(out=mo[:], in0=PS[bank][:], in1=MODB[:, 0, nh * 512:(nh + 1) * 512], op=ALU.mult),
                         reads=[("PS", bank), ("MODB", 0, nh)], writes=[mok])
                    S.op("pool", lambda e, mo=mo, xb=xb, nh=nh: e.tensor_tensor(out=XB[:, xb, nh * 512:(nh + 1) * 512], in0=XB[:, xb, nh * 512:(nh + 1) * 512],
                                                                              in1=mo[:], op=ALU.add), reads=[mok, ("XB", xb)], writes=[("XB", xb)])
                S.dma("sp", lambda e, lt=lt, xb=xb: e.dma_start(out=x1_d[lt * 128:(lt + 1) * 128, :], in_=XB[:, xb]), "xb%d" % xb,
                      reads=[("XB", xb)], writes=[("X1D", lt)])

                def extra(kc, src, bank):
                    if bank == 2:
                        S.op("act", lambda e, kc=kc, src=src: e.activation(out=H2F[:, kc, :], in_=src, func=AF.Identity,
                                                                         bias=MODT[:, 3, kc, 0:1], scale=S2[:, kc:kc + 1]),
                             reads=[("PS", bank), "S2", ("MODT", 3)], writes=[("H2F", kc)])
                    else:
                        S.op("dve", lambda e, kc=kc, src=src: e.tensor_scalar(out=H2F[:, kc, :], in0=src, scalar1=S2[:, kc:kc + 1],
                                                                            scalar2=MODT[:, 3, kc, 0:1], op0=ALU.mult, op1=ALU.add),
                             reads=[("PS", bank), "S2", ("MODT", 3)], writes=[("H2F", kc)])
                norm_transpose(XB[:, xb], ("XB", xb), 32 + lt,
                               lambda kc: S2[:, kc:kc + 1], lambda kc: MODT[:, 3, kc, 0:1],
                               lambda kc, lt=lt: HT2[:, kc, lt * 128:(lt + 1) * 128], lambda kc, lt=lt: [("HT2", kc, lt)], extra=extra,
                               after_norm=lambda lt=lt, xb=xb: S.dma("sp", lambda e: e.dma_start(out=xn2_d[lt * 128:(lt + 1) * 128, :], in_=XB[:, xb]),
                                                                     "xb%d" % xb, reads=[("XB", xb)], writes=[("XN2D", lt)]), skip_main=True)
                for kc in range(8):
                    S.op("pe", lambda e, kc=kc: e.matmul(PS[3][:, 0:32], lhsT=H2F[:, kc, :], rhs=RWS[:, kc, :], start=(kc == 0), stop=False),
                         reads=[("H2F", kc), "RWS"], writes=[("PS", 3)], sig=False)
                S.op("pe", lambda e: e.matmul(PS[3][:, 0:32], lhsT=ONESROW[0:1, :], rhs=RBROW[0:1, :], start=False, stop=True),
                     reads=["ONESROW", "RBROW"], writes=[("PS", 3)])
                S.op("act", lambda e: e.copy(out=LG[:], in_=PS[3][:, 0:32]), reads=[("PS", 3)], writes=["LG"])
                S.op("dve", lambda e: e.max(out=MX8[:], in_=LG[:]), reads=["LG"], writes=["MX8"])
                gt = GATES[:, lt, :]
                gk_ = ("GATES", lt)
                S.op("dve", lambda e, gt=gt: e.tensor_scalar(out=gt, in0=LG[:], scalar1=MX8[:, 3:4], scalar2=None, op0=ALU.is_ge), reads=["LG", "MX8"], writes=[gk_])
                S.op("dve", lambda e: e.tensor_scalar(out=LG[:], in0=LG[:], scalar1=MX8[:, 0:1], scalar2=None, op0=ALU.subtract), reads=["LG", "MX8", gk_], writes=["LG"])
                S.op("act", lambda e: e.activation(out=LG[:], in_=LG[:], func=AF.Exp), reads=["LG"], writes=["LG"])
                S.op("dve", lambda e, gt=gt: e.tensor_tensor(out=gt, in0=gt, in1=LG[:], op=ALU.mult), reads=["LG", gk_], writes=[gk_])
                S.op("dve", lambda e, gt=gt: e.reduce_sum(out=MX8[:, 7:8], in_=gt, axis=AXL.X), reads=[gk_, "MX8"], writes=["MX8"])
                S.op("dve", lambda e: e.reciprocal(out=MX8[:, 7:8], in_=MX8[:, 7:8]), reads=["MX8"], writes=["MX8"])
                S.op("dve", lambda e, gt=gt: e.tensor_scalar(out=gt, in0=gt, scalar1=MX8[:, 7:8], scalar2=None, op0=ALU.mult), reads=[gk_, "MX8"], writes=[gk_])

        S.barrier()
        RE = Region([(0, 176)])
        W1Gs = RE.alloc([128, 2, 8192], BF16)
        W1Us = RE.alloc([128, 2, 8192], BF16)
        W2s = RE.alloc([128, 2, 8192], BF16)
        XG = RE.alloc([128, 2, 2, 1024])
        HG = RE.alloc([128, 2, 8, PSZ], BF16)
        ACTT = RE.alloc([128, 8, PSZ], BF16)
        YS = RE.alloc([128, 2, 1024])
        TMP = RE.alloc([128, 6, 512])
        TMPn[0] = 6
        B1S = RE.alloc([128, 2, 2, 8])
        SLTF = RE.alloc([128, 128])
        SLTI = RE.alloc([128, 128], I32)
        TABT = RE.alloc([128, 256])
        IC = RE.alloc([128, 256])
        MSK = RE.alloc([128, 16, 32])
        CNT = RE.alloc([128, 32])
        PADD = RE.alloc([128, 32])
        PEND = RE.alloc([128, 32])
        RUNB = RE.alloc([128, 32])
        DEST = RE.alloc([128, 32])
        KEY = RE.alloc([128, 32])
        OH = RE.alloc([128, 32])
        MX = RE.alloc([128, 8])
        EK4 = RE.alloc([128, 4])
        DK = RE.alloc([128, 64])
        DKI = RE.alloc([128, 64], I32)
        CMP = RE.alloc([128, NPASS, 32])
        EP = RE.alloc([128, NPASS])
        SK = RE.alloc([128, NPASS])
        IDXWF = RE.alloc([128, NPASS])
        IDXWI = RE.alloc([128, NPASS], I32)
        GIDXF = RE.alloc([128, 2, 2])
        GIDXI = RE.alloc([128, 2, 2], I32)
        GT = RE.alloc([128, 2, 2, 2])
        ONES1 = RE.alloc([128, 128])
        SLTM = RE.alloc([128, 128])
        OOBT = RE.alloc([128, 256])
        GTS = RE.alloc([32, 128])
        B2S = RE.alloc([32, 1024])
        ACI = RE.alloc([128, 2, 1024])
        IOTAE = IC[:, 0:32]
        W64 = IC[:, 32:64]
        THR = IC[:, 64:128]
        PIDX = IC[:, 128:129]
        TOKF = IC[:, 129:145]
        ONE32 = IC[:, 145:177]
        TOK2 = IC[:, 177:209].rearrange("p (l two) -> p l two", two=2)
        G2 = XG[:, 0, 0, :].rearrange("p (l e d) -> p l e d", e=NE, d=2)
        S.dma("sp", lambda e: e.dma_start(out=B2S[:], in_=b2), "c6", writes=["B2S"])
        S.dma("sp", lambda e: e.dma_start(out=IC[:], in_=iconst), "c7", writes=["IC"])
        S.op("pool", lambda e: e.memset(ONES1[:], 1.0), writes=["ONES1"])
        S.op("pool", lambda e: e.memset(SLTM[:], 1.0), writes=["SLTM"])
        S.op("pool", lambda e: e.affine_select(out=SLTM[:], in_=SLTM[:], pattern=[[1, 128]], compare_op=ALU.is_gt, fill=0.0, base=0, channel_multiplier=-1),
             reads=["SLTM"], writes=["SLTM"])
        S.op("pool", lambda e: e.memset(OOBT[:], 1.0e6), writes=["OOBT"])
        S.dma("sp", lambda e: e.dma_start(out=slot_d.rearrange("(q p) o -> q (p o)", p=128), in_=OOBT[:]), "c8", reads=["OOBT"], writes=["SLOTD"])
        S.op("dve", lambda e: e.tensor_copy(out=G2, in_=GATES[:].unsqueeze(3).to_broadcast([128, 16, NE, 2])), reads=[("GATES", lt) for lt in range(16)],
             writes=[("XG", 0), ("XG", 1)])
        S.dma("sp", lambda e: e.dma_start(out=gates_d.rearrange("(l p e) o -> p l (e o)", p=128, e=NE), in_=G2.rearrange("p l e d -> p l (e d)")), "c9",
              reads=[("XG", 0), ("XG", 1)], writes=["GATESD"])
        S.op("pool", lambda e: e.memset(XG[:], 0.0), writes=[("XG", 0), ("XG", 1)])
        for lt in range(16):
            S.op("pe", lambda e, lt=lt: e.transpose(out=PS[3][0:32, 0:128], in_=GATES[:, lt, :], identity=ID32[:]), reads=[("GATES", lt), "ID32"], writes=[("PS", 3)])
            S.op("act", lambda e: e.copy(out=GTS[:], in_=PS[3][0:32, 0:128]), reads=[("PS", 3)], writes=["GTS"])
            ab = lt % 2
            for nh in range(2):
                S.op("pe", lambda e, nh=nh: e.matmul(PS[nh][:], lhsT=GTS[:], rhs=B2S[:, nh * 512:(nh + 1) * 512], start=True, stop=True),
                     reads=["GTS", "B2S"], writes=[("PS", nh)])
                S.op("act", lambda e, nh=nh, ab=ab: e.copy(out=ACI[:, ab, nh * 512:(nh + 1) * 512], in_=PS[nh][:]), reads=[("PS", nh)], writes=[("ACI", ab)])
            S.dma("sp", lambda e, lt=lt, ab=ab: e.dma_start(out=acc_d[lt * 128:(lt + 1) * 128, :], in_=ACI[:, ab]), "aci%d" % ab, reads=[("ACI", ab)], writes=["ACCD"])
        gk_all = [("GATES", lt) for lt in range(16)]
        S.op("dve", lambda e: e.tensor_scalar(out=MSK[:], in0=GATES[:], scalar1=0.0, scalar2=None, op0=ALU.is_gt), reads=gk_all, writes=["MSK"])
        for lt in range(16):
            S.op("pe", lambda e, lt=lt: e.matmul(PS[2][:, 0:32], lhsT=ONES1[:], rhs=MSK[:, lt, :], start=(lt == 0), stop=(lt == 15)),
                 reads=["ONES1", "MSK"], writes=[("PS", 2)], sig=(lt == 15))
        S.op("dve", lambda e: e.tensor_copy(out=CNT[:], in_=PS[2][:, 0:32]), reads=[("PS", 2)], writes=["CNT"])
        S.op("dve", lambda e: e.tensor_scalar(out=PADD[:], in0=CNT[:], scalar1=0.0, scalar2=None, op0=ALU.is_gt), reads=["CNT"], writes=["PADD"])
        for j in range(1, 8):
            S.op("dve", lambda e, j=j: e.scalar_tensor_tensor(out=PADD[:], in0=CNT[:], scalar=float(PSZ * j), in1=PADD[:], op0=ALU.is_gt, op1=ALU.add),
                 reads=["CNT", "PADD"], writes=["PADD"])
        S.op("dve", lambda e: e.tensor_scalar(out=PADD[:], in0=PADD[:], scalar1=float(PSZ), scalar2=None, op0=ALU.mult), reads=["PADD"], writes=["PADD"])
        S.op("dve", lambda e: e.tensor_tensor_scan(out=PEND[:], data0=ONE32, data1=PADD[:], initial=0.0, op0=ALU.mult, op1=ALU.add),
             reads=["PADD", "IC"], writes=["PEND"])
        S.op("dve", lambda e: e.tensor_tensor(out=RUNB[:], in0=PEND[:], in1=PADD[:], op=ALU.subtract), reads=["PEND", "PADD"], writes=["RUNB"])
        S.op("dve", lambda e: e.tensor_tensor(out=CMP[:], in0=PEND[:].unsqueeze(1).to_broadcast([128, NPASS, 32]),
                                            in1=THR.unsqueeze(2).to_broadcast([128, NPASS, 32]), op=ALU.is_le), reads=["PEND", "IC"], writes=["CMP"])
        S.op("dve", lambda e: e.reduce_sum(out=EP[:], in_=CMP[:], axis=AXL.X), reads=["CMP"], writes=["EP"])
        S.op("dve", lambda e: e.tensor_scalar(out=EP[:], in0=EP[:], scalar1=31.0, scalar2=None, op0=ALU.min), reads=["EP"], writes=["EP"])
        S.op("dve", lambda e: e.tensor_scalar(out=IDXWF[:], in0=EP[:], scalar1=128.0, scalar2=PIDX, op0=ALU.mult, op1=ALU.add), reads=["EP", "IC"], writes=["IDXWF"])
        S.op("dve", lambda e: e.tensor_tensor(out=SK[:, 2:NPASS], in0=EP[:, 2:NPASS], in1=EP[:, 0:NPASS - 2], op=ALU.is_equal), reads=["EP"], writes=["SK"])
        S.op("dve", lambda e: e.scalar_tensor_tensor(out=IDXWF[:, 2:NPASS], in0=SK[:, 2:NPASS], scalar=1.0e7, in1=IDXWF[:, 2:NPASS], op0=ALU.mult, op1=ALU.add),
             reads=["SK", "IDXWF"], writes=["IDXWF"])
        S.op("dve", lambda e: e.tensor_copy(out=IDXWI[:], in_=IDXWF[:]), reads=["IDXWF"], writes=["IDXWI"])
        pre_w = set()

        def issue_weights(p):
            sl = p % 2
            idx = IDXWI[:, p:p + 1]
            for nm, dst, srcw in (("w1g", W1Gs, w1g), ("w1u", W1Us, w1u), ("w2", W2s, w2)):
                S.dma("pool", lambda e, dst=dst, srcw=srcw, sl=sl, idx=idx: e.indirect_dma_start(
                    out=dst[:, sl, :], out_offset=None, in_=srcw,
                    in_offset=bass.IndirectOffsetOnAxis(ap=idx, axis=0), bounds_check=bnd(e, NE * 128 - 1), oob_is_err=False),
                    "%s%d" % (nm, sl), reads=["IDXWI", (nm, sl)], writes=[(nm, sl)])
            for gi, srcb in ((0, b1gt), (1, b1ut)):
                S.dma("pool", lambda e, gi=gi, srcb=srcb, sl=sl, idx=idx: e.indirect_dma_start(
                    out=B1S[:, sl, gi, :], out_offset=None, in_=srcb, in_offset=bass.IndirectOffsetOnAxis(ap=idx, axis=0),
                    bounds_check=bnd(e, NE * 128 - 1), oob_is_err=False), "b1s%d" % sl, reads=["IDXWI", ("B1S", sl)], writes=[("B1S", sl)])

        if stage == "full":
            for p_ in (0, 1):
                issue_weights(p_)
                pre_w.add(p_)
        for lt in range(16):
            S.op("pe", lambda e, lt=lt: e.matmul(PS[0][:, 0:32], lhsT=SLTM[:], rhs=MSK[:, lt, :], start=True, stop=True), reads=["SLTM", "MSK"], writes=[("PS", 0)])
            S.op("pe", lambda e, lt=lt: e.matmul(PS[1][:, 0:32], lhsT=ONES1[:], rhs=MSK[:, lt, :], start=True, stop=True), reads=["ONES1", "MSK"], writes=[("PS", 1)])
            S.op("dve", lambda e: e.tensor_tensor(out=DEST[:], in0=PS[0][:, 0:32], in1=RUNB[:], op=ALU.add), reads=[("PS", 0), "RUNB"], writes=["DEST"])
            S.op("dve", lambda e: e.tensor_tensor(out=RUNB[:], in0=PS[1][:, 0:32], in1=RUNB[:], op=ALU.add), reads=[("PS", 1), "RUNB", "DEST"], writes=["RUNB"])
            S.op("dve", lambda e, lt=lt: e.tensor_tensor(out=KEY[:], in0=MSK[:, lt, :], in1=W64, op=ALU.mult), reads=["MSK", "IC"], writes=["KEY"])
            S.op("dve", lambda e: e.max(out=MX[:], in_=KEY[:]), reads=["KEY"], writes=["MX"])
            S.op("dve", lambda e: e.tensor_scalar(out=EK4[:], in0=MX[:, 0:4], scalar1=-1.0, scalar2=64.0, op0=ALU.mult, op1=ALU.add), reads=["MX"], writes=["EK4"])
            for k in range(4):
                S.op("dve", lambda e, k=k: e.tensor_scalar(out=OH[:], in0=IOTAE, scalar1=EK4[:, k:k + 1], scalar2=None, op0=ALU.is_equal), reads=["EK4", "IC"], writes=["OH"])
                S.op("dve", lambda e: e.tensor_tensor(out=OH[:], in0=OH[:], in1=DEST[:], op=ALU.mult), reads=["OH", "DEST"], writes=["OH"])
                S.op("dve", lambda e, lt=lt, k=k: e.reduce_sum(out=DK[:, lt * 4 + k:lt * 4 + k + 1], in_=OH[:], axis=AXL.X), reads=["OH"], writes=[("DK", lt)])
        S.op("dve", lambda e: e.tensor_copy(out=DKI[:], in_=DK[:]), reads=[("DK", lt) for lt in range(16)], writes=["DKI"])
        for i in range(64):
            S.dma("pool", lambda e, i=i: e.indirect_dma_start(out=slot_d, out_offset=bass.IndirectOffsetOnAxis(ap=DKI[:, i:i + 1], axis=0),
                                                             in_=TOK2[:, i // 4, :], in_offset=None, bounds_check=bnd(e, NSLOT - 1), oob_is_err=False),
                  "sct", reads=["DKI", "IC", "SLOTD"], writes=[("SLOTS", i)])
        S.dma("sp", lambda e: e.dma_start(out=TABT[:], in_=slot_d.rearrange("(q p) o -> q (p o)", p=128)), "c8",
              reads=[("SLOTS", i) for i in range(64)], writes=["TABT"])
        S.op("pe", lambda e: e.transpose(out=PS[2][:, 0:128], in_=TABT[:].rearrange("p (a two) -> p a two", two=2)[:, :, 0], identity=ID32[:]), reads=["TABT", "ID32"], writes=[("PS", 2)])
        S.op("act", lambda e: e.copy(out=SLTF[:], in_=PS[2][:, 0:128]), reads=[("PS", 2)], writes=["SLTF"])
        S.op("dve", lambda e: e.tensor_copy(out=SLTI[:], in_=SLTF[:]), reads=["SLTF"], writes=["SLTI"])

        npass = NPASS if stage == "full" else (1 if stage in ("mA", "mB", "mC", "mD", "mC1", "mC2") else 0)

        def issue_loads(p):
            buf = p % 2
            for j in range(2):
                q = 2 * p + j
                S.dma("pool", lambda e, buf=buf, j=j, q=q: e.indirect_dma_start(
                    out=XG[:, buf, j, :], out_offset=None, in_=xn2_d, in_offset=bass.IndirectOffsetOnAxis(ap=SLTI[:, q:q + 1], axis=0),
                    bounds_check=bnd(e, SEQ - 1), oob_is_err=False), "xg%d" % buf,
                    reads=["SLTI", ("XG", buf)] + [("XN2D", lt) for lt in range(16)], writes=[("XG", buf)])
            S.op("dve", lambda e, buf=buf, p=p: e.tensor_scalar(out=GIDXF[:, buf, :], in0=SLTF[:, 2 * p:2 * p + 2], scalar1=32.0, scalar2=EP[:, p:p + 1],
                                                              op0=ALU.mult, op1=ALU.add), reads=["SLTF", "EP"], writes=[("GIDXF", buf)])
            S.op("dve", lambda e, buf=buf: e.tensor_copy(out=GIDXI[:, buf, :], in_=GIDXF[:, buf, :]), reads=[("GIDXF", buf)], writes=[("GIDXI", buf)])
            S.op("pool", lambda e, buf=buf: e.memset(GT[:, buf], 0.0), writes=[("GT", buf)])
            for j in range(2):
                S.dma("pool", lambda e, buf=buf, j=j: e.indirect_dma_start(
                    out=GT[:, buf, j, :], out_offset=None, in_=gates_d, in_offset=bass.IndirectOffsetOnAxis(ap=GIDXI[:, buf, j:j + 1], axis=0),
                    bounds_check=bnd(e, SEQ * NE - 1), oob_is_err=False), "gt%d" % buf, reads=[("GIDXI", buf), ("GT", buf), "GATESD"], writes=[("GT", buf)])
            S.op("dve", lambda e, buf=buf: e.tensor_scalar(out=GT[:, buf], in0=GT[:, buf], scalar1=1.0 / 1.702, scalar2=None, op0=ALU.mult),
                 reads=[("GT", buf)], writes=[("GT", buf)])
            if stage == "mA":
                return
            if p in pre_w:
                return
            issue_weights(p)

        if npass > 0:
            issue_loads(0)
        for p in range(npass):
            buf = p % 2
            sl = p % 2
            if p + 1 < npass:
                issue_loads(p + 1)
            if stage in ("mA", "mB"):
                continue
            for pr in range(4):
                bank = pr % 2
                for h in range(2):
                    kc = 2 * pr + h
                    off = h * PSZ
                    for j in range(2):
                        S.op("pe", lambda e, kc=kc, j=j, bank=bank, off=off, buf=buf: e.transpose(out=PS[bank][:, off + j * 128:off + (j + 1) * 128],
                                                                                               in_=XG[:, buf, j, kc * 128:(kc + 1) * 128], identity=ID32[:]),
                             reads=[("XG", buf), "ID32"], writes=[("PS", bank)], sig=(h == 1 and j == 1))
                for h in range(2):
                    kc = 2 * pr + h
                    src = PS[bank][:, h * PSZ:(h + 1) * PSZ]
                    if bank == 0:
                        S.op("act", lambda e, kc=kc, src=src, buf=buf: e.activation(out=HG[:, buf, kc, :], in_=src, func=AF.Identity, bias=MODT[:, 3, kc, 0:1],
                                                                                  scale=S2[:, kc:kc + 1]), reads=[("PS", bank)], writes=[("HG", buf, kc)])
                    else:
                        S.op("dve", lambda e, kc=kc, src=src, buf=buf: e.tensor_scalar(out=HG[:, buf, kc, :], in0=src, scalar1=S2[:, kc:kc + 1],
                                                                                     scalar2=MODT[:, 3, kc, 0:1], op0=ALU.mult, op1=ALU.add),
                             reads=[("PS", bank)], writes=[("HG", buf, kc)])
            if stage == "mC1":
                continue
            for fc in range(8):
                gbk, ubk = 2 + fc % 2, 4 + fc % 2
                for kc in range(8):
                    S.op("pe", lambda e, kc=kc, fc=fc, gbk=gbk, sl=sl, buf=buf: e.matmul(PS[gbk][:, 0:PSZ], lhsT=W1Gs[:, sl, fc * 1024 + kc * 128:fc * 1024 + (kc + 1) * 128],
                                                                                      rhs=HG[:, buf, kc, :], start=(kc == 0), stop=(kc == 7)),
                         reads=[("w1g", sl), ("HG", buf, kc)], writes=[("PS", gbk)], sig=(kc == 7))
                for kc in range(8):
                    S.op("pe", lambda e, kc=kc, fc=fc, ubk=ubk, sl=sl, buf=buf: e.matmul(PS[ubk][:, 0:PSZ], lhsT=W1Us[:, sl, fc * 1024 + kc * 128:fc * 1024 + (kc + 1) * 128],
                                                                                      rhs=HG[:, buf, kc, :], start=(kc == 0), stop=(kc == 7)),
                         reads=[("w1u", sl), ("HG", buf, kc)], writes=[("PS", ubk)], sig=(kc == 7))
                gv, gvk = tmp()
                uv, uvk = tmp()
                sg, sgk = tmp()
                S.op("dve", lambda e, gv=gv, gbk=gbk, sl=sl, fc=fc: e.tensor_scalar(out=gv[:, 0:PSZ], in0=PS[gbk][:, 0:PSZ], scalar1=B1S[:, sl, 0, fc:fc + 1], scalar2=7.0,
                                                                                 op0=ALU.add, op1=ALU.min), reads=[("PS", gbk), ("B1S", sl)], writes=[gvk])
                S.op("act", lambda e, uv=uv, ubk=ubk, sl=sl, fc=fc: e.activation(out=uv[:, 0:PSZ], in_=PS[ubk][:, 0:PSZ], func=AF.Identity, bias=B1S[:, sl, 1, fc:fc + 1], scale=1.0),
                     reads=[("PS", ubk), ("B1S", sl)], writes=[uvk])
                S.op("act", lambda e, sg=sg, gv=gv: e.activation(out=sg[:, 0:PSZ], in_=gv[:, 0:PSZ], func=AF.Silu, scale=1.702), reads=[gvk], writes=[sgk])
                S.op("dve", lambda e, uv=uv: e.tensor_scalar(out=uv[:, 0:PSZ], in0=uv[:, 0:PSZ], scalar1=-7.0, scalar2=7.0, op0=ALU.max, op1=ALU.min), reads=[uvk], writes=[uvk])
                S.op("dve", lambda e, sg=sg, uv=uv, fc=fc: e.scalar_tensor_tensor(out=ACTT[:, fc, :], in0=uv[:, 0:PSZ], scalar=1.0, in1=sg[:, 0:PSZ], op0=ALU.add, op1=ALU.mult),
                     reads=[sgk, uvk], writes=[("ACTT", fc)])
            if stage == "mC2":
                continue
            for j in range(2):
                q = 2 * p + j
                for nh in range(2):
                    bank = 6 + nh
                    for fc in range(8):
                        S.op("pe", lambda e, fc=fc, j=j, nh=nh, bank=bank, sl=sl: e.matmul(PS[bank][:], lhsT=ACTT[:, fc, j * 128:(j + 1) * 128],
                                                                                        rhs=W2s[:, sl, fc * 1024 + nh * 512:fc * 1024 + (nh + 1) * 512],
                                                                                        start=(fc == 0), stop=(fc == 7)),
                             reads=[("w2", sl), ("ACTT", fc)], writes=[("PS", bank)], sig=(fc == 7))
                    if False:
                        S.op("act", lambda e, j=j, nh=nh, bank=bank, buf=buf: e.activation(out=YS[:, j, nh * 512:(nh + 1) * 512], in_=PS[bank][:], func=AF.Identity,
                                                                                        bias=0.0, scale=GT[:, buf, j, 0:1]),
                             reads=[("PS", bank), ("GT", buf)], writes=[("YS", j, nh)])
                    else:
                        S.op("dve", lambda e, j=j, nh=nh, bank=bank, buf=buf: e.tensor_scalar(out=YS[:, j, nh * 512:(nh + 1) * 512], in0=PS[bank][:], scalar1=GT[:, buf, j, 0:1],
                                                                                           scalar2=None, op0=ALU.mult),
                             reads=[("PS", bank), ("GT", buf)], writes=[("YS", j, nh)])
                if stage == "mC":
                    continue
                S.dma("sp", lambda e, j=j, q=q: e.dma_start(out=yslot_d[q * 128:(q + 1) * 128, :], in_=YS[:, j, :]),
                      "ysc%d" % j, reads=[("YS", j, 0), ("YS", j, 1)], writes=[("YSLOT", q)])

        S.barrier()
        FG = Region([(96, 104)]).alloc([128, 1024])
        TMP = Region([(120, 128)]).alloc([128, 4, 512])
        TMPn[0] = 4
        ACB = Region([(0, 8)]).alloc([128, 2, 1024])
        YG = Region([(8, 40)]).alloc([128, 2, 4, 1024])
        S.dma("sp", lambda e: e.dma_start(out=FG[:], in_=fgrow[0:1, :].to_broadcast([128, 1024])), "c5", writes=["FG"])
        out_tokens = []
        for lt in range(16):
            xb = lt % 3
            S.dma("sp", lambda e, lt=lt, xb=xb: e.dma_start(out=XB[:, xb], in_=x1_d[lt * 128:(lt + 1) * 128, :]), "xb%d" % xb,
                  reads=[("X1D", lt)], writes=[("XB", xb)])
            ab = lt % 2
            S.dma("sp", lambda e, lt=lt, ab=ab: e.dma_start(out=ACB[:, ab], in_=acc_d[lt * 128:(lt + 1) * 128, :]), "acb%d" % ab, reads=["ACCD"], writes=[("ACB", ab)])
            for k in range(4):
                S.dma("pool", lambda e, lt=lt, ab=ab, k=k: e.indirect_dma_start(
                    out=YG[:, ab, k, :], out_offset=None, in_=yslot_d, in_offset=bass.IndirectOffsetOnAxis(ap=DKI[:, lt * 4 + k:lt * 4 + k + 1], axis=0),
                    bounds_check=bnd(e, NSLOT - 1), oob_is_err=False), "yg%d" % ab, reads=["DKI", ("YG", ab)] + [("YSLOT", q) for q in range(2 * NPASS)],
                    writes=[("YG", ab)])
            for k in range(4):
                S.op("pool" if k % 2 == 0 else "dve", lambda e, ab=ab, k=k: e.tensor_tensor(out=ACB[:, ab], in0=ACB[:, ab], in1=YG[:, ab, k, :], op=ALU.add),
                     reads=[("ACB", ab), ("YG", ab)], writes=[("ACB", ab)])
            for nh in range(2):
                mo, mok = tmp()
                S.op("dve", lambda e, mo=mo, ab=ab, nh=nh: e.tensor_tensor(out=mo[:], in0=ACB[:, ab, nh * 512:(nh + 1) * 512], in1=MODB[:, 1, nh * 512:(nh + 1) * 512],
                                                                         op=ALU.mult), reads=[("ACB", ab), ("MODB", 1, nh)], writes=[mok])
                S.op("pool", lambda e, mo=mo, xb=xb, nh=nh: e.tensor_tensor(out=XB[:, xb, nh * 512:(nh + 1) * 512], in0=XB[:, xb, nh * 512:(nh + 1) * 512],
                                                                          in1=mo[:], op=ALU.add), reads=[mok, ("XB", xb)], writes=[("XB", xb)])
            col = 48 + (lt % 16)
            xt_ap = XB[:, xb]
            xkey = ("XB", xb)
            S.op("act", lambda e, xt_ap=xt_ap: e.activation(out=JUNK[:], in_=xt_ap, func=AF.Square), reads=[xkey], writes=["JUNK"])
            S.op("dve", lambda e, col=col: e.reduce_sum(out=SMALL[:, col:col + 1], in_=JUNK[:], axis=AXL.X), reads=["JUNK"], writes=[("SM", col)])
            S.op("act", lambda e, col=col: e.activation(out=SMALL[:, col:col + 1], in_=SMALL[:, col:col + 1], func=AF.Ln, bias=EPS, scale=1.0 / D),
                 reads=[("SM", col)], writes=[("SM", col)])
            S.op("act", lambda e, col=col: e.activation(out=SMALL[:, col:col + 1], in_=SMALL[:, col:col + 1], func=AF.Exp, scale=-0.5), reads=[("SM", col)], writes=[("SM", col)])
            S.op("dve", lambda e, col=col, xt_ap=xt_ap: e.scalar_tensor_tensor(out=xt_ap, in0=xt_ap, scalar=SMALL[:, col:col + 1], in1=FG[:], op0=ALU.mult, op1=ALU.mult),
                 reads=[xkey, ("SM", col), "FG"], writes=[xkey])
            tok = S.dma("sp", lambda e, lt=lt, xb=xb: e.dma_start(out=out[lt * 128:(lt + 1) * 128, :], in_=XB[:, xb]), "xb%d" % xb, reads=[xkey], writes=[("OUT", lt)])
            out_tokens.append(tok)
        S.wait_all("sp", out_tokens)
        S.emit()
    return nc


def _fm(v):
    v = np.asarray(v, np.float32).reshape(-1, 128)
    return np.ascontiguousarray(v.T)


def pack_shared(inp):
    f32 = np.float32
    cp = np.zeros((128, NCP), f32)

    def put(name, arr):
        arr = np.asarray(arr, f32)
        cp[:, CO[name]:CO[name] + arr.shape[1]] = arr

    put("n1g", _fm(inp["norm1_g"][0]))
    put("n2g", _fm(inp["norm2_g"][0]))
    put("convw", np.concatenate([_fm(inp["lru_conv_w"][0, k]) for k in range(4)], axis=1))
    put("convb", _fm(inp["lru_conv_b"][0]))
    put("ba", np.concatenate([_fm(inp["lru_ba"][0, d].reshape(-1)) for d in range(2)], axis=1))
    put("bx", np.concatenate([_fm(inp["lru_bx"][0, d].reshape(-1)) for d in range(2)], axis=1))
    put("lam", np.concatenate([_fm(inp["lru_lam"][0, d]) for d in range(2)], axis=1))
    put("lb0", _fm(inp["hg_lb_logits"][0]))
    put("lb1", _fm(inp["hg_lb_logits"][1]))
    put("hgng", np.asarray(inp["hg_norm_g"][0], f32).reshape(128, 1))
    put("adab", _fm(inp["ada_b"][0]))
    b1 = np.asarray(inp["moe_b1"][0], f32)
    b1g = b1[:, 0::2].reshape(NE, 8, 128)
    b1u = b1[:, 1::2].reshape(NE, 8, 128)
    put("b1g", np.ascontiguousarray(b1g.transpose(2, 0, 1)).reshape(128, NE * 8))
    put("b1u", np.ascontiguousarray(b1u.transpose(2, 0, 1)).reshape(128, NE * 8))

    def kmajor(w):
        w = np.asarray(w, f32)
        return np.ascontiguousarray(w.reshape(8, 128, w.shape[1]).transpose(1, 0, 2))

    sh = {"cpack": cp}
    aw = np.asarray(inp["ada_w"][0], f32)
    sh["adaw"] = np.ascontiguousarray(aw.reshape(8, 128, 6, 1024).transpose(2, 1, 0, 3))
    sh["adabrow"] = np.asarray(inp["ada_b"][0], f32).reshape(1, 6144)
    wi = np.asarray(inp["w_in"][0], f32).reshape(8, 128, 9, 8, 128)
    sh["win"] = np.ascontiguousarray(wi[:, :, 0:7].transpose(3, 1, 2, 0, 4))
    sh["win78"] = np.ascontiguousarray(wi[:, :, 7:9].transpose(3, 1, 2, 0, 4))
    wa = np.asarray(inp["lru_wa"][0], f32)
    wx = np.asarray(inp["lru_wx"][0], f32)
    lw = np.stack([wa[0], wx[0], wa[1], wx[1]], axis=0)
    sh["lruw"] = np.ascontiguousarray(lw.transpose(2, 1, 0, 3))
    sh["wba"] = kmajor(inp["w_branch_a"][0])
    sh["wbb"] = kmajor(inp["w_branch_b"][0])
    sh["wout"] = kmajor(inp["w_out"][0])
    sh["fgrow"] = np.asarray(inp["final_g"], f32).reshape(1, 1024)
    sh["rw"] = kmajor(inp["router_w"][0])
    sh["rbrow"] = np.asarray(inp["router_b"][0], f32).reshape(1, 32)
    w1 = np.asarray(inp["moe_w1"][0], f32)
    w1r = w1.reshape(NE, 8, 128, 8, 128, 2)
    sh["w1g"] = np.ascontiguousarray(w1r[..., 0].transpose(0, 2, 3, 1, 4)).reshape(NE * 128, 8192)
    sh["w1u"] = np.ascontiguousarray(w1r[..., 1].transpose(0, 2, 3, 1, 4)).reshape(NE * 128, 8192)
    w2_ = np.asarray(inp["moe_w2"][0], f32)
    sh["w2"] = np.ascontiguousarray(w2_.reshape(NE, 8, 128, 1024).transpose(0, 2, 1, 3)).reshape(NE * 128, 8192)
    sh["b1gt"] = np.ascontiguousarray(b1g.transpose(0, 2, 1)).reshape(NE * 128, 8)
    sh["b1ut"] = np.ascontiguousarray(b1u.transpose(0, 2, 1)).reshape(NE * 128, 8)
    ic = np.zeros((128, 256), f32)
    ic[:, 0:32] = np.arange(32)[None, :]
    ic[:, 32:64] = 64 - np.arange(32)[None, :]
    ic[:, 64:128] = (PSZ * np.arange(NPASS))[None, :]
    ic[:, 128] = np.arange(128)
    ic[:, 129:145] = np.arange(16)[None, :] * 128 + np.arange(128)[:, None]
    ic[:, 145:177] = 1.0
    ic[:, 177:209] = np.repeat(np.arange(16)[None, :] * 128 + np.arange(128)[:, None], 2, axis=1)
    sh["iconst"] = ic
    sh["b2"] = np.ascontiguousarray(np.asarray(inp["moe_b2"][0], f32))
    return sh


def pack_core(inp, b):
    f32 = np.float32
    xin = np.concatenate([np.asarray(inp["ctx"][b], f32), np.asarray(inp["x"][b], f32)], axis=0)
    cv = np.stack([_fm(inp["c"][b]), _fm(inp["c_ctx"])], axis=2)
    return {"xin": np.ascontiguousarray(xin), "cvec": np.ascontiguousarray(cv)}


def kernel(**inputs):
    n = 8
    sh = pack_shared(inputs)
    in_maps = []
    for b in range(n):
        m = dict(sh)
        m.update(pack_core(inputs, b))
        in_maps.append(m)
    nc = build_nc("full")
    res = run_bass_kernel_spmd(nc, in_maps, core_ids=list(range(n)))
    return np.stack([np.asarray(r["out"], np.float32) for r in res.results], axis=0)
```

```python
import types
import numpy as np
from contextlib import ExitStack
import concourse.bass as bass
import concourse.mybir as mybir
from concourse.bass_utils import run_bass_kernel_spmd

F32 = mybir.dt.float32
BF16 = mybir.dt.bfloat16
I32 = mybir.dt.int32
AF = mybir.ActivationFunctionType
ALU = mybir.AluOpType
AXL = mybir.AxisListType

D = 1024
SEQ = 2048
CTX = 256
T = SEQ + CTX
NT = T // 128
NE = 32
EPS = 1e-6
NPASS = 64
PSZ = 256
NSLOT = NPASS * PSZ
ENGS = ("pe", "act", "dve", "pool", "sp")

CO = {}
_o = 0
for _n, _w in (("n1g", 8), ("n2g", 8), ("convw", 32), ("convb", 8), ("ba", 16), ("bx", 16), ("lam", 16),
               ("lb0", 8), ("lb1", 8), ("hgng", 1), ("adab", 48), ("b1g", 256), ("b1u", 256)):
    CO[_n] = _o
    _o += _w
NCP = _o


def _snap(fn):
    if fn.__closure__ is None:
        return fn
    cells = []
    for c in fn.__closure__:
        try:
            cells.append(types.CellType(c.cell_contents))
        except ValueError:
            cells.append(c)
    return types.FunctionType(fn.__code__, fn.__globals__, fn.__name__, fn.__defaults__, tuple(cells))


class Sched:
    def __init__(self, nc, stack):
        self.nc = nc
        self.stack = stack
        self.ops = {e: [] for e in ENGS}
        self.cnt = {e: 0 for e in ENGS}
        self.esem = {e: stack.enter_context(nc.semaphore("s_" + e)) for e in ENGS}
        self.known = {e: {} for e in ENGS}
        self.last_w = {}
        self.readers = {}
        self.dma_sems = {}

    def _need(self, reads, writes):
        need = []
        for k in reads:
            t = self.last_w.get(k)
            if t is not None:
                need.append(t)
        for k in writes:
            t = self.last_w.get(k)
            if t is not None:
                need.append(t)
            need.extend(self.readers.get(k, ()))
        return need

    def _emit_waits(self, eng, need):
        best = {}
        for (sem, val, src) in need:
            if src == eng and eng == "pe":
                continue
            key = id(sem)
            if key not in best or best[key][1] < val:
                best[key] = (sem, val)
        kn = self.known[eng]
        for key, (sem, val) in best.items():
            if kn.get(key, 0) >= val:
                continue
            kn[key] = val
            self.ops[eng].append(("wait", sem, val))

    def _commit(self, token, reads, writes):
        for k in reads:
            self.readers.setdefault(k, []).append(token)
        for k in writes:
            self.last_w[k] = token
            self.readers[k] = []

    def op(self, eng, fn, reads=(), writes=(), sig=True):
        fn = _snap(fn)
        self._emit_waits(eng, self._need(reads, writes))
        if sig:
            self.cnt[eng] += 1
            token = (self.esem[eng], self.cnt[eng], eng)
            self.ops[eng].append(("op", fn, self.esem[eng], 1))
        else:
            token = (self.esem[eng], self.cnt[eng] + 1, eng)
            self.ops[eng].append(("op", fn, None, 0))
        self._commit(token, reads, writes)
        return token

    def dma(self, eng, fn, semkey, reads=(), writes=()):
        fn = _snap(fn)
        if semkey not in self.dma_sems:
            self.dma_sems[semkey] = [self.stack.enter_context(self.nc.semaphore("d_%d" % len(self.dma_sems))), 0]
        ent = self.dma_sems[semkey]
        self._emit_waits(eng, self._need(reads, writes))
        ent[1] += 16
        token = (ent[0], ent[1], "dma")
        self.ops[eng].append(("op", fn, ent[0], 16))
        self._commit(token, reads, writes)
        return token

    def wait_all(self, eng, tokens):
        self._emit_waits(eng, tokens)

    def barrier(self):
        toks = [(self.esem[e], self.cnt[e], "bar") for e in ENGS if self.cnt[e] > 0]
        toks += [(v[0], v[1], "dma") for v in self.dma_sems.values()]
        for e in ENGS:
            self._emit_waits(e, toks)

    def emit(self):
        with self.nc.Block() as block:
            def mk(name):
                def body(e):
                    for item in self.ops[name]:
                        if item[0] == "wait":
                            e.wait_ge(item[1], item[2])
                        else:
                            ins = item[1](e)
                            if item[2] is not None:
                                ins.then_inc(item[2], item[3])
                return body
            block.tensor(mk("pe"))
            block.scalar(mk("act"))
            block.vector(mk("dve"))
            block.gpsimd(mk("pool"))
            block.sync(mk("sp"))


_BND = {}


def bnd(e, val):
    key = (id(e), val)
    if key not in _BND:
        r = e.alloc_register("bnd%d" % val)
        e.reg_mov(r, val)
        _BND[key] = r
    return _BND[key]


def chunk_range(c):
    if c == 0:
        return 0, 256
    return 256 + 512 * (c - 1), 256 + 512 * c


def build_nc(stage="full"):
    _BND.clear()
    nc = bass.Bass("TRN2", target_bir_lowering=False)

    def din(name, shape, dt=F32):
        return nc.dram_tensor(name, list(shape), dt, kind="ExternalInput").ap()

    xin = din("xin", [T, D])
    cvec = din("cvec", [128, 8, 2])
    cpack = din("cpack", [128, NCP])
    adaw = din("adaw", [6, 128, 8, 1024])
    adabrow = din("adabrow", [1, 6144])
    win = din("win", [8, 128, 7, 8, 128])
    win78 = din("win78", [8, 128, 2, 8, 128])
    lruw = din("lruw", [128, 8, 4, 128])
    wba = din("wba", [128, 8, 1024])
    wbb = din("wbb", [128, 8, 1024])
    wout = din("wout", [128, 8, 1024])
    fgrow = din("fgrow", [1, 1024])
    rw = din("rw", [128, 8, 32])
    rbrow = din("rbrow", [1, 32])
    w1g = din("w1g", [NE * 128, 8192])
    w1u = din("w1u", [NE * 128, 8192])
    w2 = din("w2", [NE * 128, 8192])
    b1gt = din("b1gt", [NE * 128, 8])
    b1ut = din("b1ut", [NE * 128, 8])
    iconst = din("iconst", [128, 256])
    b2 = din("b2", [NE, 1024])
    out = nc.dram_tensor("out", [SEQ, D], F32, kind="ExternalOutput").ap()
    dk = "Internal" if stage == "full" else "ExternalOutput"
    ya_d = nc.dram_tensor("ya_d", [8, 128, SEQ], BF16, kind=dk).ap()
    yb_d = nc.dram_tensor("yb_d", [8, 128, SEQ], BF16, kind=dk).ap()
    x1_d = nc.dram_tensor("x1_d", [SEQ, D], F32, kind=dk).ap()
    xn2_d = nc.dram_tensor("xn2_d", [SEQ, D], F32, kind="Internal").ap()
    acc_d = nc.dram_tensor("acc_d", [SEQ, D], F32, kind="Internal").ap()
    gates_d = nc.dram_tensor("gates_d", [SEQ * NE, 2], F32, kind="Internal").ap()
    slot_d = nc.dram_tensor("slot_d", [NSLOT, 2], F32, kind="Internal").ap()
    yslot_d = nc.dram_tensor("yslot_d", [NSLOT, D], F32, kind="Internal").ap()

    with ExitStack() as st:
        S = Sched(nc, st)

        def sb(name, shape, dt=F32):
            return st.enter_context(nc.sbuf_tensor(name, list(shape), dt))

        def finish():
            S.barrier()
            S.emit()
            return nc

        PS = [st.enter_context(nc.psum_tensor("ps%d" % i, [128, 512], F32)) for i in range(8)]
        ARENA = 176 * 1024
        AR = sb("AR", [128, ARENA // 2], BF16)

        class Region:
            def __init__(self, ranges):
                self.ranges = [[lo * 1024, hi * 1024] for lo, hi in ranges]

            def alloc(self, shape, dt=F32):
                nel = int(np.prod(shape[1:]))
                nb = nel * (4 if dt in (F32, I32) else 2)
                nb_al = (nb + 63) // 64 * 64
                for r in self.ranges:
                    if r[0] + nb_al <= r[1]:
                        off = r[0]
                        r[0] += nb_al
                        break
                else:
                    raise RuntimeError("arena region full for %s" % (shape,))
                v = AR[0:shape[0], off // 2:(off + nb) // 2]
                if dt in (F32, I32):
                    v = v.bitcast(dt)
                if len(shape) == 3:
                    v = v.rearrange("p (a b) -> p a b", b=shape[2])
                elif len(shape) == 4:
                    v = v.rearrange("p (a b c) -> p a b c", b=shape[2], c=shape[3])
                elif len(shape) == 5:
                    v = v.rearrange("p (a b c d) -> p a b c d", b=shape[2], c=shape[3], d=shape[4])
                return v

        CP = sb("CP", [128, NCP])
        CV = sb("CV", [128, 8, 2])
        SC = sb("SC", [128, 8, 2])
        ID32 = sb("ID32", [128, 128])
        ONESM = sb("ONESM", [128, 128])
        ONESROW = sb("ONESROW", [1, 128])
        TRI = sb("TRI", [128, 2, 128], BF16)
        MASKF = sb("MASKF", [128, 512], BF16)
        HMASK = sb("HMASK", [128, 2])
        MODT = sb("MODT", [128, 6, 8, 2])
        MODB = sb("MODB", [128, 2, 1024])
        S1 = sb("S1", [128, 8, 2])
        S2 = sb("S2", [128, 8])
        CL = sb("CL", [128, 16])
        CL2 = sb("CL2", [128, 16])
        LB = sb("LB", [128, 8])
        OML = sb("OML", [128, 8])
        NOML = sb("NOML", [128, 8])
        RWS = sb("RWS", [128, 8, 32])
        RBROW = sb("RBROW", [1, 32])
        GATES = sb("GATES", [128, 16, 32])
        SMALL = sb("SMALL", [128, 64])

        cpc = lambda name, i=0, n=1: CP[:, CO[name] + i: CO[name] + i + n]

        S.dma("sp", lambda e: e.dma_start(out=CP[:], in_=cpack), "c0", writes=["CP"])
        S.dma("sp", lambda e: e.dma_start(out=CV[:], in_=cvec), "c1", writes=["CV"])
        R0 = Region([(0, 140)])
        BIG = R0.alloc([128, 2, 8, 1024])
        SCB = R0.alloc([128, 8, 128])
        ADABROW = R0.alloc([1, 2, 1024])
        S.dma("sp", lambda e: e.dma_start(out=ADABROW[0:1, 0, :], in_=adabrow[0:1, 2048:3072]), "c2", writes=[("ADABROW", 0)])
        S.dma("sp", lambda e: e.dma_start(out=ADABROW[0:1, 1, :], in_=adabrow[0:1, 5120:6144]), "c2b", writes=[("ADABROW", 1)])
        S.dma("sp", lambda e: e.dma_start(out=RWS[:], in_=rw), "c3", writes=["RWS"])
        S.dma("sp", lambda e: e.dma_start(out=RBROW[:], in_=rbrow), "c4", writes=["RBROW"])

        S.op("pool", lambda e: e.memset(ID32[:], 0.0), writes=["ID32"])
        S.op("pool", lambda e: e.affine_select(out=ID32[:], in_=ID32[:], pattern=[[-1, 128]], compare_op=ALU.not_equal,
                                               fill=1.0, base=0, channel_multiplier=1), reads=["ID32"], writes=["ID32"])
        S.op("pool", lambda e: e.memset(ONESM[:], 1.0 / 128.0), writes=["ONESM"])
        S.op("pool", lambda e: e.memset(ONESROW[:], 1.0), writes=["ONESROW"])
        S.op("pool", lambda e: e.memset(MASKF[:], 1.0), writes=["MASKF"])
        S.op("pool", lambda e: e.memset(MASKF[:].rearrange("p (a b) -> p a b", b=64)[:, :, 0:1], 0.0),
             reads=["MASKF"], writes=["MASKF"])
        S.op("pool", lambda e: e.memset(HMASK[:], 0.0), writes=["HMASK"])
        S.op("pool", lambda e: e.memset(HMASK[0:64, 0:1], 1.0), reads=["HMASK"], writes=["HMASK"])
        S.op("pool", lambda e: e.memset(HMASK[64:128, 1:2], 1.0), reads=["HMASK"], writes=["HMASK"])
        S.op("pool", lambda e: e.memset(TRI[:], 0.0), writes=["TRI"])
        for blk in range(2):
            lo = blk * 64
            S.op("pool", lambda e, lo=lo: e.memset(TRI[lo:lo + 64, :, lo:lo + 64], 1.0), reads=["TRI"], writes=["TRI"])
        S.op("pool", lambda e: e.affine_select(out=TRI[:, 0, :], in_=TRI[:, 0, :], pattern=[[1, 128]], compare_op=ALU.is_ge,
                                               fill=0.0, base=0, channel_multiplier=-1), reads=["TRI"], writes=["TRI"])
        S.op("pool", lambda e: e.affine_select(out=TRI[:, 1, :], in_=TRI[:, 1, :], pattern=[[-1, 128]], compare_op=ALU.is_ge,
                                               fill=0.0, base=0, channel_multiplier=1), reads=["TRI"], writes=["TRI"])

        S.op("act", lambda e: e.activation(out=SC[:], in_=CV[:], func=AF.Silu), reads=["CV"], writes=["SC"])
        S.op("dve", lambda e: e.tensor_copy(out=SCB[:], in_=SC[:, :, 0:1].to_broadcast([128, 8, 128])), reads=["SC"], writes=["SCB"])
        S.op("act", lambda e: e.activation(out=CL[:], in_=cpc("lam", 0, 16), func=AF.Exp, scale=-1.0), reads=["CP"], writes=["CL"])
        S.op("act", lambda e: e.activation(out=CL[:], in_=CL[:], func=AF.Ln, bias=1.0, scale=1.0), reads=["CL"], writes=["CL"])
        S.op("dve", lambda e: e.tensor_scalar(out=CL2[:], in0=CL[:], scalar1=-16.0, scalar2=None, op0=ALU.mult), reads=["CL"], writes=["CL2"])
        S.op("dve", lambda e: e.tensor_scalar(out=CL[:], in0=CL[:], scalar1=-8.0, scalar2=None, op0=ALU.mult), reads=["CL", "CL2"], writes=["CL"])
        S.op("dve", lambda e: e.tensor_tensor(out=LB[:], in0=cpc("lb0", 0, 8), in1=cpc("lb1", 0, 8), op=ALU.subtract), reads=["CP"], writes=["LB"])
        S.op("act", lambda e: e.activation(out=LB[:], in_=LB[:], func=AF.Sigmoid), reads=["LB"], writes=["LB"])
        S.op("dve", lambda e: e.tensor_scalar(out=OML[:], in0=LB[:], scalar1=-1.0, scalar2=1.0, op0=ALU.mult, op1=ALU.add), reads=["LB"], writes=["OML"])
        S.op("dve", lambda e: e.tensor_scalar(out=NOML[:], in0=OML[:], scalar1=-1.0, scalar2=None, op0=ALU.mult), reads=["OML"], writes=["NOML"])

        HT = Region([(140, 176)]).alloc([128, 8, T], BF16)
        HT2 = Region([(64, 96)]).alloc([128, 8, SEQ], BF16)
        RX = Region([(104, 120)])
        XB = RX.alloc([128, 3, 1024])
        JUNK = RX.alloc([128, 1024])
        TMPn = [8]

        for j in range(6):
            buf = j % 2
            S.dma("sp", lambda e, j=j, buf=buf: e.dma_start(out=BIG[:, buf], in_=adaw[j]), "ada%d" % buf, writes=[("BIG", buf)])
            if j in (0, 1, 3, 4):
                pm = PS[j % 2]
                for fcn in range(8):
                    for kc in range(8):
                        S.op("pe", lambda e, pm=pm, fcn=fcn, kc=kc, buf=buf: e.matmul(
                            pm[:, fcn * 2:fcn * 2 + 2], lhsT=BIG[:, buf, kc, fcn * 128:(fcn + 1) * 128], rhs=SC[:, kc, :],
                            start=(kc == 0), stop=(kc == 7)),
                            reads=[("BIG", buf), "SC"], writes=[("PS", j % 2)], sig=(kc == 7))
                S.op("dve", lambda e, pm=pm, j=j: e.tensor_tensor(
                    out=MODT[:, j], in0=pm[:, 0:16].rearrange("p (a b) -> p a b", b=2),
                    in1=cpc("adab", j * 8, 8).unsqueeze(2).to_broadcast([128, 8, 2]), op=ALU.add),
                    reads=[("PS", j % 2), "CP"], writes=[("MODT", j)])
            else:
                jj = 0 if j == 2 else 1
                for nh in range(2):
                    pm = PS[2 + nh]
                    for kc in range(8):
                        S.op("pe", lambda e, pm=pm, kc=kc, buf=buf, nh=nh: e.matmul(
                            pm[:], lhsT=SCB[:, kc, :], rhs=BIG[:, buf, kc, nh * 512:(nh + 1) * 512], start=(kc == 0), stop=False),
                            reads=[("BIG", buf), "SCB"], writes=[("PS", 2 + nh)], sig=False)
                    S.op("pe", lambda e, pm=pm, j=j, nh=nh: e.matmul(
                        pm[:], lhsT=ONESROW[0:1, :], rhs=ADABROW[0:1, jj, nh * 512:(nh + 1) * 512], start=False, stop=True),
                        reads=["ONESROW", ("ADABROW", 0), ("ADABROW", 1)], writes=[("PS", 2 + nh)])
                    S.op("act", lambda e, pm=pm, jj=jj, nh=nh: e.copy(out=MODB[:, jj, nh * 512:(nh + 1) * 512], in_=pm[:]),
                         reads=[("PS", 2 + nh)], writes=[("MODB", jj, nh)])
        S.op("dve", lambda e: e.tensor_scalar(out=S1[:], in0=MODT[:, 1], scalar1=1.0, scalar2=None, op0=ALU.add), reads=[("MODT", 1)], writes=["S1"])
        S.op("dve", lambda e: e.tensor_tensor(out=S1[:], in0=S1[:], in1=cpc("n1g", 0, 8).unsqueeze(2).to_broadcast([128, 8, 2]), op=ALU.mult),
             reads=["S1", "CP"], writes=["S1"])
        S.op("dve", lambda e: e.tensor_scalar(out=S2[:], in0=MODT[:, 4, :, 0], scalar1=1.0, scalar2=None, op0=ALU.add), reads=[("MODT", 4)], writes=["S2"])
        S.op("dve", lambda e: e.tensor_tensor(out=S2[:], in0=S2[:], in1=cpc("n2g", 0, 8), op=ALU.mult), reads=["S2", "CP"], writes=["S2"])

        if stage == "p0":
            return finish()
        S.barrier()

        def norm_transpose(xt_ap, xkey, col, scale_ap_fn, bias_ap_fn, dst_fn, dst_keys_fn, extra=None, after_norm=None, skip_main=False):
            S.op("act", lambda e: e.activation(out=JUNK[:], in_=xt_ap, func=AF.Square), reads=[xkey], writes=["JUNK"])
            S.op("dve", lambda e: e.reduce_sum(out=SMALL[:, col:col + 1], in_=JUNK[:], axis=AXL.X), reads=["JUNK"], writes=[("SM", col)])
            S.op("act", lambda e: e.activation(out=SMALL[:, col:col + 1], in_=SMALL[:, col:col + 1], func=AF.Ln, bias=EPS, scale=1.0 / D),
                 reads=[("SM", col)], writes=[("SM", col)])
            S.op("act", lambda e: e.activation(out=SMALL[:, col:col + 1], in_=SMALL[:, col:col + 1], func=AF.Exp, scale=-0.5), reads=[("SM", col)], writes=[("SM", col)])
            S.op("dve", lambda e: e.tensor_scalar(out=xt_ap, in0=xt_ap, scalar1=SMALL[:, col:col + 1], scalar2=None, op0=ALU.mult),
                 reads=[xkey, ("SM", col)], writes=[xkey])
            if after_norm is not None:
                after_norm()
            if stage == "p1a":
                return
            for half in range(2):
                bank = 2 + half
                for q in range(4):
                    kc = half * 4 + q
                    S.op("pe", lambda e, kc=kc, q=q, bank=bank: e.transpose(out=PS[bank][:, q * 128:(q + 1) * 128],
                                                                          in_=xt_ap[:, kc * 128:(kc + 1) * 128], identity=ID32[:]),
                         reads=[xkey, "ID32"], writes=[("PS", bank)], sig=(q == 3))
                if stage == "p1b":
                    continue
                for q in range(4):
                    kc = half * 4 + q
                    src = PS[bank][:, q * 128:(q + 1) * 128]
                    if stage == "p1c" and q % 2 == 1:
                        continue
                    if stage == "p1d" and q % 2 == 0:
                        continue
                    if skip_main:
                        pass
                    elif (half == 0 if stage != "p1e" else q % 2 == 0):
                        S.op("act", lambda e, kc=kc, src=src: e.activation(out=dst_fn(kc), in_=src, func=AF.Identity,
                                                                         bias=bias_ap_fn(kc), scale=scale_ap_fn(kc)),
                             reads=[("PS", bank), "S1", "S2", ("MODT", 0), ("MODT", 3)], writes=dst_keys_fn(kc))
                    else:
                        S.op("dve", lambda e, kc=kc, src=src: e.tensor_scalar(out=dst_fn(kc), in0=src, scalar1=scale_ap_fn(kc),
                                                                            scalar2=bias_ap_fn(kc), op0=ALU.mult, op1=ALU.add),
                             reads=[("PS", bank), "S1", "S2", ("MODT", 0), ("MODT", 3)], writes=dst_keys_fn(kc))
                    if extra is not None:
                        extra(kc, src, bank)

        for tt in range(NT):
            xb = tt % 3
            S.dma("sp", lambda e, tt=tt, xb=xb: e.dma_start(out=XB[:, xb], in_=xin[tt * 128:(tt + 1) * 128, :]), "xb%d" % xb,
                  writes=[("XB", xb)])
            w = 1 if tt < 2 else 0
            norm_transpose(XB[:, xb], ("XB", xb), tt % 32,
                           lambda kc, w=w: S1[:, kc, w:w + 1], lambda kc, w=w: MODT[:, 0, kc, w:w + 1],
                           lambda kc, tt=tt: HT[:, kc, tt * 128:(tt + 1) * 128], lambda kc, tt=tt: [("HT", kc, tt)])

        if stage in ("p1", "p1a", "p1b", "p1c", "p1d", "p1e"):
            return finish()

        def ht_keys(t0, t1):
            return [("HT", kc, tt) for kc in range(8) for tt in range(t0 // 128, (t1 + 127) // 128)]

        S.barrier()
        RM = Region([(0, 140)])
        TMP = Region([(120, 136)]).alloc([128, 8, 512])
        RM = Region([(0, 120)])
        WG = RM.alloc([128, 1, 7, 8, 128], BF16)
        LW = RM.alloc([128, 1, 4, 128], BF16)
        U = RM.alloc([128, T])
        UB = RM.alloc([128, T], BF16)
        Y = RM.alloc([128, SEQ])
        GA = RM.alloc([128, SEQ], BF16)
        HC = RM.alloc([128, 2, CTX])
        HB = RM.alloc([128, 2, 512])
        TB = RM.alloc([128, 8, 512], BF16)
        YO = Region([(136, 140)]).alloc([128, 1, SEQ], BF16)
        QS = RM.alloc([128, SEQ], BF16)
        SGT = RM.alloc([128, SEQ], BF16)
        VT = RM.alloc([128, NT, 128], BF16)
        QM = RM.alloc([128, SEQ], BF16)
        KM = RM.alloc([128, SEQ], BF16)
        QG = RM.alloc([128, SEQ], BF16)
        KE = RM.alloc([128, T])
        KET = RM.alloc([128, 2, NT, 128], BF16)
        STMP = RM.alloc([128, 128])
        DEC = RM.alloc([128, 36])
        S32 = RM.alloc([128, 2, 128])
        S16 = RM.alloc([128, 32, 128], BF16)
        PT = RM.alloc([128, 3, 128], BF16)
        OF = RM.alloc([128, SEQ])

        tmp_i = [0]

        def tmp():
            i = tmp_i[0] % TMPn[0]
            tmp_i[0] += 1
            return TMP[:, i], ("TMP", i)

        tb_i = [0]

        def tbf():
            i = tb_i[0] % 8
            tb_i[0] += 1
            return TB[:, i], ("TB", i)

        pj_i = [0]

        def pj():
            i = pj_i[0] % 2
            pj_i[0] += 1
            return PS[i], ("PS", i)

        def proj(wbuf, slot, c):
            t0, t1 = chunk_range(c)
            n = t1 - t0
            ps, pk = pj()
            for kc in range(8):
                S.op("pe", lambda e, kc=kc, ps=ps: e.matmul(ps[:, 0:n], lhsT=WG[:, wbuf, slot, kc, :], rhs=HT[:, kc, t0:t1],
                                                          start=(kc == 0), stop=(kc == 7)),
                     reads=[("WG", wbuf)] + [("HT", kc, tt) for tt in range(t0 // 128, t1 // 128)], writes=[pk], sig=(kc == 7))
            return ps, pk, t0, t1, n

        ng = 8 if stage not in ("g1", "lru", "lruscan", "mix1", "h1", "h2", "h3", "h4", "h5") else 1
        for g in range(ng):
            wbuf = 0
            S.dma("pool", lambda e, g=g, wbuf=wbuf: e.dma_start(out=WG[:, wbuf], in_=win[g]), "wg%d" % wbuf, writes=[("WG", wbuf)])
            S.dma("pool", lambda e, g=g, wbuf=wbuf: e.dma_start(out=LW[:, wbuf], in_=lruw[:, g]), "lw%d" % wbuf, writes=[("LW", wbuf)])

            for c in range(5):
                ps, pk, t0, t1, n = proj(wbuf, 0, c)
                ax, axk = tmp()
                S.op("act", lambda e, ax=ax, ps=ps, n=n: e.copy(out=ax[:, 0:n], in_=ps[:, 0:n]), reads=[pk], writes=[axk])
                uk = ("U", c)
                S.op("dve", lambda e, ax=ax, t0=t0, t1=t1, n=n: e.tensor_scalar(
                    out=U[:, t0:t1], in0=ax[:, 0:n], scalar1=cpc("convw", 2 * 8 + g), scalar2=cpc("convb", g), op0=ALU.mult, op1=ALU.add),
                    reads=[axk, "CP"], writes=[uk])
                rw_ = 256 if c == 0 else 64
                for k, dlt in ((0, -2), (1, -1), (3, 1)):
                    lo, hi = max(0, -dlt), rw_ - max(0, dlt)
                    S.op("dve", lambda e, ax=ax, t0=t0, t1=t1, n=n, k=k, dlt=dlt, lo=lo, hi=hi, rw_=rw_: e.scalar_tensor_tensor(
                        out=U[:, t0:t1].rearrange("p (r c) -> p r c", c=rw_)[:, :, lo:hi],
                        in0=ax[:, 0:n].rearrange("p (r c) -> p r c", c=rw_)[:, :, lo + dlt:hi + dlt],
                        scalar=cpc("convw", k * 8 + g),
                        in1=U[:, t0:t1].rearrange("p (r c) -> p r c", c=rw_)[:, :, lo:hi], op0=ALU.mult, op1=ALU.add),
                        reads=[axk, uk, "CP"], writes=[uk])
                S.op("pool", lambda e, t0=t0, t1=t1: e.tensor_copy(out=UB[:, t0:t1], in_=U[:, t0:t1]), reads=[uk], writes=[("UB", c)])
                if c > 0:
                    ps, pk, t0, t1, n = proj(wbuf, 1, c)
                    l0 = t0 - CTX
                    ag, agk = tmp()
                    S.op("act", lambda e, ag=ag, ps=ps: e.copy(out=ag[:], in_=ps[:]), reads=[pk], writes=[agk])
                    z, zk = tmp()
                    S.op("pool", lambda e, ag=ag, z=z: e.tensor_tensor(out=z[:], in0=ag[:], in1=ag[:], op=ALU.mult), reads=[agk], writes=[zk])
                    S.op("pool", lambda e, z=z: e.tensor_scalar(out=z[:], in0=z[:], scalar1=0.044715, scalar2=1.0, op0=ALU.mult, op1=ALU.add),
                         reads=[zk], writes=[zk])
                    S.op("pool", lambda e, ag=ag, z=z: e.tensor_tensor(out=z[:], in0=z[:], in1=ag[:], op=ALU.mult), reads=[zk, agk], writes=[zk])
                    S.op("act", lambda e, z=z: e.activation(out=z[:], in_=z[:], func=AF.Sigmoid, scale=1.5957691216057308), reads=[zk], writes=[zk])
                    S.op("pool", lambda e, ag=ag, z=z, l0=l0: e.tensor_tensor(out=GA[:, l0:l0 + 512], in0=z[:], in1=ag[:], op=ALU.mult),
                         reads=[zk, agk], writes=[("GA", c)])
            for d in range(2):
                order = [0, 1, 2, 3, 4] if d == 0 else [0, 4, 3, 2, 1]
                prev_state = None
                def lstage_a(c):
                    t0, t1 = chunk_range(c)
                    n = t1 - t0
                    rps, rpk = pj()
                    S.op("pe", lambda e, rps=rps, t0=t0, t1=t1, n=n, d=d: e.matmul(rps[:, 0:n], lhsT=LW[:, wbuf, d * 2 + 0, :], rhs=UB[:, t0:t1],
                                                                                 start=True, stop=True),
                         reads=[("LW", wbuf), ("UB", c)], writes=[rpk])
                    ips, ipk = pj()
                    S.op("pe", lambda e, ips=ips, t0=t0, t1=t1, n=n, d=d: e.matmul(ips[:, 0:n], lhsT=LW[:, wbuf, d * 2 + 1, :], rhs=UB[:, t0:t1],
                                                                                 start=True, stop=True),
                         reads=[("LW", wbuf), ("UB", c)], writes=[ipk])
                    rt, rk = tmp()
                    it, ik = tmp()
                    S.op("act", lambda e, rt=rt, rps=rps, n=n, d=d: e.activation(out=rt[:, 0:n], in_=rps[:, 0:n], func=AF.Sigmoid,
                                                                               bias=cpc("ba", d * 8 + g), scale=1.0), reads=[rpk, "CP"], writes=[rk])
                    S.op("act", lambda e, it=it, ips=ips, n=n, d=d: e.activation(out=it[:, 0:n], in_=ips[:, 0:n], func=AF.Sigmoid,
                                                                               bias=cpc("bx", d * 8 + g), scale=1.0), reads=[ipk, "CP"], writes=[ik])
                    return (t0, t1, n, rt, rk, it, ik)

                pend_l = lstage_a(order[0])
                for oi, c in enumerate(order):
                    t0, t1, n, rt, rk, it, ik = pend_l
                    pend_l = lstage_a(order[oi + 1]) if oi + 1 < len(order) else None
                    a2, a2k = tmp()
                    S.op("act", lambda e, rt=rt, a2=a2, n=n, d=d: e.activation(out=a2[:, 0:n], in_=rt[:, 0:n], func=AF.Exp,
                                                                             scale=CL2[:, d * 8 + g:d * 8 + g + 1]), reads=[rk, "CL2"], writes=[a2k])
                    S.op("act", lambda e, rt=rt, n=n, d=d: e.activation(out=rt[:, 0:n], in_=rt[:, 0:n], func=AF.Exp,
                                                                      scale=CL[:, d * 8 + g:d * 8 + g + 1]), reads=[rk, "CL"], writes=[rk])
                    S.op("act", lambda e, a2=a2, n=n: e.activation(out=a2[:, 0:n], in_=a2[:, 0:n], func=AF.Sqrt, bias=1.0, scale=-1.0),
                         reads=[a2k], writes=[a2k])
                    S.op("pool", lambda e, it=it, t0=t0, t1=t1, n=n: e.tensor_tensor(out=it[:, 0:n], in0=it[:, 0:n], in1=U[:, t0:t1], op=ALU.mult),
                         reads=[ik, ("U", c)], writes=[ik])
                    S.op("pool", lambda e, it=it, a2=a2, n=n: e.tensor_tensor(out=it[:, 0:n], in0=it[:, 0:n], in1=a2[:, 0:n], op=ALU.mult),
                         reads=[ik, a2k], writes=[ik])
                    if d == 0:
                        if c == 0:
                            dst, dk = HC[:, 0, :], ("HC", 0)
                        else:
                            dst, dk = Y[:, t0 - CTX:t1 - CTX], ("Y", c)
                        init = 0.0 if prev_state is None else prev_state[0]
                        rd = [rk, ik] + ([] if prev_state is None else [prev_state[1]])
                        S.op("dve", lambda e, dst=dst, rt=rt, it=it, n=n, init=init: e.tensor_tensor_scan(
                            out=dst, data0=rt[:, 0:n], data1=it[:, 0:n], initial=init, op0=ALU.mult, op1=ALU.add), reads=rd, writes=[dk])
                        prev_state = (dst[:, n - 1:n], dk)
                    else:
                        if c == 0:
                            dst, dk = HC[:, 1, :], ("HC", 1)
                        else:
                            hb = oi % 2
                            dst, dk = HB[:, hb, :], ("HB", hb)
                        init = 0.0 if prev_state is None else prev_state[0]
                        rd = [rk, ik] + ([] if prev_state is None else [prev_state[1]])
                        S.op("dve", lambda e, dst=dst, rt=rt, it=it, n=n, init=init: e.tensor_tensor_scan(
                            out=dst[:, ::-1], data0=rt[:, 0:n][:, ::-1], data1=it[:, 0:n][:, ::-1], initial=init, op0=ALU.mult, op1=ALU.add),
                            reads=rd, writes=[dk])
                        prev_state = (dst[:, 0:1], dk)
                        if c > 0:
                            S.op("pool", lambda e, dst=dst, t0=t0, t1=t1: e.tensor_tensor(out=Y[:, t0 - CTX:t1 - CTX], in0=Y[:, t0 - CTX:t1 - CTX],
                                                                                        in1=dst, op=ALU.add), reads=[dk, ("Y", c)], writes=[("Y", c)])
            if stage == "lruscan":
                return finish()
            for c in range(1, 5):
                l0 = (c - 1) * 512
                S.op("dve", lambda e, l0=l0: e.tensor_tensor(out=YO[:, 0, l0:l0 + 512], in0=Y[:, l0:l0 + 512], in1=GA[:, l0:l0 + 512], op=ALU.mult),
                     reads=[("Y", c), ("GA", c)], writes=[("YO", 0)])
            S.dma("sp", lambda e, g=g: e.dma_start(out=ya_d[g], in_=YO[:, 0, :]), "yo0", reads=[("YO", 0)], writes=[("YAD", g)])

            if stage == "lru":
                return finish()
            for c in range(1, 5):
                ps, pk, t0, t1, n = proj(wbuf, 2, c)
                l0 = t0 - CTX
                S.op("act", lambda e, ps=ps, l0=l0: e.activation(out=QS[:, l0:l0 + 512], in_=ps[:], func=AF.Silu), reads=[pk], writes=[("QS", c)])
                ps, pk, t0, t1, n = proj(wbuf, 6, c)
                S.op("act", lambda e, ps=ps, l0=l0: e.activation(out=SGT[:, l0:l0 + 512], in_=ps[:], func=AF.Silu), reads=[pk], writes=[("SGT", c)])
            for t4 in range(0, NT, 4):
                nt4 = min(4, NT - t4)
                for q in range(nt4):
                    tt = t4 + q
                    for kc in range(8):
                        S.op("pe", lambda e, q=q, tt=tt, kc=kc: e.matmul(PS[2][:, q * 128:(q + 1) * 128], lhsT=HT[:, kc, tt * 128:(tt + 1) * 128],
                                                                      rhs=WG[:, wbuf, 5, kc, :], start=(kc == 0), stop=(kc == 7)),
                             reads=[("WG", wbuf), ("HT", kc, tt)], writes=[("PS", 2)], sig=(kc == 7))
                S.op("act", lambda e, t4=t4, nt4=nt4: e.copy(out=VT[:, t4:t4 + nt4, :], in_=PS[2][:, 0:nt4 * 128].rearrange("p (a b) -> p a b", b=128)),
                     reads=[("PS", 2)], writes=[("VT", t4 // 4)])
            if stage == "h1":
                return finish()
            for dd in range(2):
                mid = 31 if dd == 0 else 32
                end = 63 if dd == 0 else 0
                def stage_a(c):
                    ps, pk, t0, t1, n = proj(wbuf, 3 + dd, c)
                    nch = n // 64
                    t1_, k1 = tmp()
                    S.op("act", lambda e, t1_=t1_, ps=ps, n=n: e.activation(out=t1_[:, 0:n], in_=ps[:, 0:n], func=AF.Sigmoid), reads=[pk], writes=[k1])
                    kb, kbk = tbf()
                    S.op("pool", lambda e, kb=kb, t1_=t1_, n=n: e.tensor_scalar(out=kb[:, 0:n], in0=t1_[:, 0:n], scalar1=NOML[:, g:g + 1],
                                                                              scalar2=OML[:, g:g + 1], op0=ALU.mult, op1=ALU.add),
                         reads=[k1, "NOML", "OML"], writes=[kbk])
                    S.op("dve", lambda e, t1_=t1_, n=n: e.tensor_scalar(out=t1_[:, 0:n], in0=t1_[:, 0:n], scalar1=OML[:, g:g + 1],
                                                                      scalar2=LB[:, g:g + 1], op0=ALU.mult, op1=ALU.add),
                         reads=[k1, kbk, "OML", "LB"], writes=[k1])
                    return (t0, t1, n, nch, t1_, k1, kb, kbk)

                pend_a = stage_a(0)
                for c in range(5):
                    t0, t1, n, nch, t1_, k1, kb, kbk = pend_a
                    pend_a = stage_a(c + 1) if c + 1 < 5 else None
                    S.op("act", lambda e, t1_=t1_, n=n: e.activation(out=t1_[:, 0:n], in_=t1_[:, 0:n], func=AF.Ln), reads=[k1], writes=[k1])
                    gg, gk = tmp()
                    if dd == 0:
                        S.op("dve", lambda e, gg=gg, t1_=t1_, n=n: e.tensor_tensor_scan(out=gg[:, 0:n], data0=MASKF[:, 0:n], data1=t1_[:, 0:n],
                                                                                      initial=0.0, op0=ALU.mult, op1=ALU.add),
                             reads=[k1, "MASKF"], writes=[gk])
                    else:
                        S.op("dve", lambda e, gg=gg, t1_=t1_, n=n: e.tensor_tensor_scan(out=gg[:, 0:n][:, ::-1], data0=MASKF[:, 0:n],
                                                                                      data1=t1_[:, 0:n][:, ::-1], initial=0.0, op0=ALU.mult, op1=ALU.add),
                             reads=[k1, "MASKF"], writes=[gk])
                    g3 = gg[:, 0:n].rearrange("p (a b) -> p a b", b=64)
                    d1, d1k = tmp()
                    d13 = d1[:, 0:n].rearrange("p (a b) -> p a b", b=64)
                    if c > 0:
                        l0 = t0 - CTX
                        S.op("dve", lambda e, d13=d13, g3=g3, nch=nch: e.tensor_tensor(out=d13, in0=g3, in1=g3[:, :, mid:mid + 1].to_broadcast([128, nch, 64]),
                                                                                     op=ALU.subtract), reads=[gk], writes=[d1k])
                        e1, e1k = tbf()
                        S.op("act", lambda e, e1=e1, d1=d1: e.activation(out=e1[:], in_=d1[:], func=AF.Exp), reads=[d1k], writes=[e1k])
                        S.op("pool", lambda e, e1=e1, l0=l0: e.tensor_tensor(out=QM[:, l0:l0 + 512], in0=QS[:, l0:l0 + 512], in1=e1[:], op=ALU.mult),
                             reads=[e1k, ("QS", c)], writes=[("QM", c)])
                        e2, e2k = tbf()
                        S.op("act", lambda e, e2=e2, d1=d1: e.activation(out=e2[:], in_=d1[:], func=AF.Exp, scale=-1.0), reads=[d1k], writes=[e2k])
                        S.op("pool", lambda e, e2=e2, kb=kb, l0=l0: e.tensor_tensor(out=KM[:, l0:l0 + 512], in0=kb[:], in1=e2[:], op=ALU.mult),
                             reads=[e2k, kbk], writes=[("KM", c)])
                        e4, e4k = tbf()
                        S.op("act", lambda e, e4=e4, gg=gg: e.activation(out=e4[:], in_=gg[:], func=AF.Exp), reads=[gk], writes=[e4k])
                        S.op("pool", lambda e, e4=e4, l0=l0: e.tensor_tensor(out=QG[:, l0:l0 + 512], in0=QS[:, l0:l0 + 512], in1=e4[:], op=ALU.mult),
                             reads=[e4k, ("QS", c)], writes=[("QG", c)])
                    d3, d3k = tmp()
                    d33 = d3[:, 0:n].rearrange("p (a b) -> p a b", b=64)
                    S.op("dve", lambda e, d33=d33, g3=g3, nch=nch: e.tensor_tensor(out=d33, in0=g3, in1=g3[:, :, end:end + 1].to_broadcast([128, nch, 64]),
                                                                                 op=ALU.subtract), reads=[gk], writes=[d3k])
                    S.op("act", lambda e, d3=d3, n=n: e.activation(out=d3[:, 0:n], in_=d3[:, 0:n], func=AF.Exp, scale=-1.0), reads=[d3k], writes=[d3k])
                    S.op("pool", lambda e, d3=d3, kb=kb, t0=t0, t1=t1, n=n: e.tensor_tensor(out=KE[:, t0:t1], in0=kb[:, 0:n], in1=d3[:, 0:n], op=ALU.mult),
                         reads=[d3k, kbk], writes=[("KE", c)])
                    ch0 = t0 // 64
                    S.op("act", lambda e, g3=g3, ch0=ch0, nch=nch: e.activation(out=DEC[:, ch0:ch0 + nch].unsqueeze(2), in_=g3[:, :, end:end + 1], func=AF.Exp),
                         reads=[gk], writes=[("DEC", c)])
                if stage == "h2":
                    return finish()
                for t4 in range(0, NT, 4):
                    nt4 = min(4, NT - t4)
                    for q in range(nt4):
                        tt = t4 + q
                        cc = 0 if tt < 2 else 1 + (tt - 2) // 4
                        S.op("pe", lambda e, q=q, tt=tt: e.transpose(out=PS[2][:, q * 128:(q + 1) * 128], in_=KE[:, tt * 128:(tt + 1) * 128], identity=ID32[:]),
                             reads=[("KE", cc), "ID32"], writes=[("PS", 2)], sig=(q == nt4 - 1))
                    for hh in range(2):
                        S.op("dve", lambda e, t4=t4, nt4=nt4, hh=hh: e.tensor_scalar(
                            out=KET[:, hh, t4:t4 + nt4, :], in0=PS[2][:, 0:nt4 * 128].rearrange("p (a b) -> p a b", b=128),
                            scalar1=HMASK[:, hh:hh + 1], scalar2=None, op0=ALU.mult),
                            reads=[("PS", 2), "HMASK"], writes=[("KET", hh, t4 // 4)])
                if stage == "h3":
                    return finish()
                order = list(range(36)) if dd == 0 else [3, 2, 1, 0] + list(range(35, 3, -1))
                S.op("pool", lambda e: e.memset(S32[:, 0, :], 0.0), writes=[("S32", 0)])
                for i, nchk in enumerate(order):
                    par = i % 2
                    tt, half = nchk // 2, nchk % 2
                    if nchk >= 4:
                        S.op("act", lambda e, par=par, nchk=nchk: e.copy(out=S16[:, nchk - 4, :], in_=S32[:, par, :]),
                             reads=[("S32", par)], writes=[("S16", nchk - 4)])
                    if i == len(order) - 1:
                        break
                    reg = i % 4
                    cc = 0 if tt < 2 else 1 + (tt - 2) // 4
                    S.op("pe", lambda e, reg=reg, tt=tt, half=half: e.matmul(PS[3][:, reg * 128:(reg + 1) * 128],
                                                                           lhsT=KET[:, half, tt, :], rhs=VT[:, tt, :],
                                                                           start=True, stop=True),
                         reads=[("KET", half, tt // 4), ("VT", tt // 4)], writes=[("PS", 3, reg)])
                    S.op("dve", lambda e, par=par, nchk=nchk: e.tensor_scalar(out=STMP[:], in0=S32[:, par, :], scalar1=DEC[:, nchk:nchk + 1],
                                                                            scalar2=None, op0=ALU.mult),
                         reads=[("S32", par), ("DEC", cc)], writes=["STMP"])
                    S.op("dve", lambda e, reg=reg, par=par: e.tensor_tensor(out=S32[:, 1 - par, :], in0=PS[3][:, reg * 128:(reg + 1) * 128], in1=STMP[:],
                                                                          op=ALU.add),
                         reads=["STMP", ("PS", 3, reg)], writes=[("S32", 1 - par)])
                if stage == "h4":
                    return finish()
                for lt in range(16):
                    tt = lt + 2
                    c = 1 + lt // 4
                    reg = lt % 4
                    bank = 4 + lt // 4
                    S.op("pe", lambda e, reg=reg, lt=lt: e.matmul(PS[3][:, reg * 128:(reg + 1) * 128], lhsT=KM[:, lt * 128:(lt + 1) * 128],
                                                                rhs=QM[:, lt * 128:(lt + 1) * 128], start=True, stop=True),
                         reads=[("KM", c), ("QM", c)], writes=[("PS", 3, reg)])
                    pi = lt % 3
                    S.op("dve", lambda e, reg=reg, pi=pi, dd=dd: e.tensor_tensor(out=PT[:, pi, :], in0=PS[3][:, reg * 128:(reg + 1) * 128], in1=TRI[:, dd, :],
                                                                               op=ALU.mult), reads=[("PS", 3, reg), "TRI"], writes=[("PT", pi)])
                    ot = PS[bank][:, reg * 128:(reg + 1) * 128]
                    otk = ("PS", bank, reg)
                    S.op("pe", lambda e, ot=ot, tt=tt, pi=pi, dd=dd: e.matmul(ot, lhsT=VT[:, tt, :], rhs=PT[:, pi, :], start=True, stop=False,
                                                                            skip_group_check=True),
                         reads=[("VT", tt // 4), ("PT", pi)], writes=[otk], sig=False)
                    nA, nB = 2 * tt - 4, 2 * tt - 3
                    S.op("pe", lambda e, ot=ot, nA=nA, lt=lt: e.matmul(ot[:, 0:64], lhsT=S16[:, nA, :], rhs=QG[:, lt * 128:lt * 128 + 64], start=False, stop=False,
                                                                     skip_group_check=True),
                         reads=[("S16", nA), ("QG", c)], writes=[otk], sig=False)
                    S.op("pe", lambda e, ot=ot, nB=nB, lt=lt, dd=dd: e.matmul(ot[:, 64:128], lhsT=S16[:, nB, :], rhs=QG[:, lt * 128 + 64:lt * 128 + 128],
                                                                            start=False, stop=True, skip_group_check=True),
                         reads=[("S16", nB), ("QG", c)], writes=[otk])
                if dd == 0:
                    for bq in range(4):
                        S.op("act", lambda e, bq=bq: e.copy(out=OF[:, bq * 512:(bq + 1) * 512], in_=PS[4 + bq][:]),
                             reads=[("PS", 4 + bq, r) for r in range(4)], writes=[("OF", bq)])
            if stage == "h5":
                return finish()
            for bq in range(4):
                bank = 4 + bq
                bkeys = [("PS", bank, r) for r in range(4)]
                osum, osumk = tmp()
                S.op("dve", lambda e, osum=osum, bank=bank, bq=bq: e.tensor_tensor(out=osum[:], in0=PS[bank][:], in1=OF[:, bq * 512:(bq + 1) * 512], op=ALU.add),
                     reads=bkeys + [("OF", bq)], writes=[osumk])
                osq, osqk = tmp()
                S.op("act", lambda e, osq=osq, osum=osum: e.activation(out=osq[:], in_=osum[:], func=AF.Square), reads=[osumk], writes=[osqk])
                ps, pk = pj()
                S.op("pe", lambda e, ps=ps, osq=osq: e.matmul(ps[:], lhsT=ONESM[:], rhs=osq[:], start=True, stop=True), reads=["ONESM", osqk], writes=[pk])
                rs, rsk = tmp()
                S.op("act", lambda e, rs=rs, ps=ps: e.activation(out=rs[:], in_=ps[:], func=AF.Ln, bias=EPS, scale=1.0), reads=[pk], writes=[rsk])
                S.op("act", lambda e, rs=rs: e.activation(out=rs[:], in_=rs[:], func=AF.Exp, scale=-0.5), reads=[rsk], writes=[rsk])
                S.op("dve", lambda e, rs=rs, osum=osum: e.tensor_tensor(out=rs[:], in0=osum[:], in1=rs[:], op=ALU.mult), reads=[osumk, rsk], writes=[rsk])
                S.op("dve", lambda e, rs=rs, bq=bq: e.scalar_tensor_tensor(out=YO[:, 0, bq * 512:(bq + 1) * 512], in0=rs[:], scalar=cpc("hgng"),
                                                                         in1=SGT[:, bq * 512:(bq + 1) * 512], op0=ALU.mult, op1=ALU.mult),
                     reads=[rsk, ("SGT", bq + 1), "CP"], writes=[("YO", 0)])
            S.dma("sp", lambda e, g=g: e.dma_start(out=yb_d[g], in_=YO[:, 0, :]), "yo0", reads=[("YO", 0)], writes=[("YBD", g)])

        if stage == "mix1":
            return finish()
        S.barrier()
        RG = Region([(0, 64), (96, 104), (120, 140)])
        TMP = Region([(120, 128)]).alloc([128, 4, 512])
        TMPn[0] = 4
        RG = Region([(0, 64), (96, 104), (128, 140)])
        WBA = RG.alloc([128, 8, 1024], BF16)
        WBB = RG.alloc([128, 8, 1024], BF16)
        WOUT = RG.alloc([128, 8, 1024], BF16)
        YAC = RG.alloc([128, 8, 512], BF16)
        YBC = RG.alloc([128, 8, 512], BF16)
        MIX = RG.alloc([128, 8, 512], BF16)
        H2F = RG.alloc([128, 8, 128])
        W78ALL = Region([(64, 96)]).alloc([128, 8, 2, 8, 128], BF16)
        LG = RG.alloc([128, 32])
        MX8 = RG.alloc([128, 8])
        S.dma("pool", lambda e: e.dma_start(out=WBA[:], in_=wba), "wm0", writes=["WBA"])
        S.dma("pool", lambda e: e.dma_start(out=WBB[:], in_=wbb), "wm1", writes=["WBB"])
        S.dma("pool", lambda e: e.dma_start(out=WOUT[:], in_=wout), "wm2", writes=["WOUT"])
        for hf in range(2):
            S.dma("pool", lambda e, hf=hf: e.dma_start(out=W78ALL[:, hf * 4:(hf + 1) * 4], in_=win78[hf * 4:(hf + 1) * 4].rearrange("m p s k j -> p m s k j")),
                  "w78%d" % hf, writes=[("W78ALL", hf)])
        yad_keys = [("YAD", g) for g in range(ng)]
        ybd_keys = [("YBD", g) for g in range(ng)]
        for tc in range(4):
            S.dma("sp", lambda e, tc=tc: e.dma_start(out=YAC[:], in_=ya_d[:, :, tc * 512:(tc + 1) * 512].rearrange("g p t -> p g t")), "yac",
                  reads=yad_keys, writes=["YAC"])
            S.dma("sp", lambda e, tc=tc: e.dma_start(out=YBC[:], in_=yb_d[:, :, tc * 512:(tc + 1) * 512].rearrange("g p t -> p g t")), "ybc",
                  reads=ybd_keys, writes=["YBC"])
            t0 = CTX + tc * 512
            for mc in range(8):
                for kc in range(8):
                    S.op("pe", lambda e, kc=kc, mc=mc: e.matmul(PS[0][:], lhsT=WBA[:, kc, mc * 128:(mc + 1) * 128], rhs=YAC[:, kc, :], start=(kc == 0), stop=(kc == 7)),
                         reads=["WBA", "YAC"], writes=[("PS", 0)], sig=(kc == 7))
                for kc in range(8):
                    S.op("pe", lambda e, kc=kc, mc=mc: e.matmul(PS[1][:], lhsT=WBB[:, kc, mc * 128:(mc + 1) * 128], rhs=YBC[:, kc, :], start=(kc == 0), stop=(kc == 7)),
                         reads=["WBB", "YBC"], writes=[("PS", 1)], sig=(kc == 7))
                for which in range(2):
                    for kc in range(8):
                        S.op("pe", lambda e, kc=kc, mc=mc, which=which: e.matmul(PS[4 + which][:], lhsT=W78ALL[:, mc, which, kc, :], rhs=HT[:, kc, t0:t0 + 512],
                                                                              start=(kc == 0), stop=(kc == 7)),
                             reads=[("W78ALL", mc // 4)] + [("HT", kc, tt) for tt in range(t0 // 128, t0 // 128 + 4)], writes=[("PS", 4 + which)], sig=(kc == 7))
                sa, sak = tmp()
                sbb, sbk = tmp()
                S.op("act", lambda e, sa=sa: e.activation(out=sa[:], in_=PS[4][:], func=AF.Sigmoid), reads=[("PS", 4)], writes=[sak])
                S.op("act", lambda e, sbb=sbb: e.activation(out=sbb[:], in_=PS[5][:], func=AF.Sigmoid), reads=[("PS", 5)], writes=[sbk])
                S.op("dve", lambda e, sa=sa: e.tensor_tensor(out=sa[:], in0=PS[0][:], in1=sa[:], op=ALU.mult), reads=[("PS", 0), sak], writes=[sak])
                S.op("dve", lambda e, sbb=sbb: e.tensor_tensor(out=sbb[:], in0=PS[1][:], in1=sbb[:], op=ALU.mult), reads=[("PS", 1), sbk], writes=[sbk])
                S.op("pool", lambda e, sa=sa, sbb=sbb, mc=mc: e.tensor_tensor(out=MIX[:, mc, :], in0=sa[:], in1=sbb[:], op=ALU.add),
                     reads=[sak, sbk], writes=[("MIX", mc)])
            for l4 in range(4):
                lt = tc * 4 + l4
                xb = lt % 3
                S.dma("sp", lambda e, lt=lt, xb=xb: e.dma_start(out=XB[:, xb], in_=xin[CTX + lt * 128:CTX + (lt + 1) * 128, :]), "xb%d" % xb,
                      writes=[("XB", xb)])
                for nh in range(2):
                    bank = 6 + nh
                    for mc in range(8):
                        S.op("pe", lambda e, mc=mc, nh=nh, l4=l4, bank=bank: e.matmul(PS[bank][:], lhsT=MIX[:, mc, l4 * 128:(l4 + 1) * 128],
                                                                                   rhs=WOUT[:, mc, nh * 512:(nh + 1) * 512], start=(mc == 0), stop=(mc == 7)),
                             reads=["WOUT", ("MIX", mc)], writes=[("PS", bank)], sig=(mc == 7))
                    mo, mok = tmp()
                    S.op("dve", lambda e, mo=mo, bank=bank, nh=nh: e.tensor_tensor(out=mo[:], in0=PS[bank][:], in1=MODB[:, 0, nh * 512:(nh + 1) * 512], op=ALU.mult),
                         reads=[("PS", bank), ("MODB", 0, nh)], writes=[mok])
                    S.op("pool", lambda e, mo=mo, xb=xb, nh=nh: e.tensor_tensor(out=XB[:, xb, nh * 512:(nh + 1) * 512], in0=XB[:, xb, nh * 512:(nh + 1) * 512],
                                                                              in1=mo[:], op=ALU.add), reads=[mok, ("XB", xb)], writes=[("XB", xb)])
                S.dma("sp", lambda e, lt=lt, xb=xb: e.dma_start(out=x1_d[lt * 128:(lt + 1) * 128, :], in_=XB[:, xb]), "xb%d" % xb,
                      reads=[("XB", xb)], writes=[("X1D", lt)])

                def extra(kc, src, bank):
                    if bank == 2:
                        S.op("act", lambda e, kc=kc, src=src: e.activation(out=H2F[:, kc, :], in_=src, func=AF.Identity,
                                                                         bias=MODT[:, 3, kc, 0:1], scale=S2[:, kc:kc + 1]),
                             reads=[("PS", bank), "S2", ("MODT", 3)], writes=[("H2F", kc)])
                    else:
                        S.op("dve", lambda e, kc=kc, src=src: e.tensor_scalar(out=H2F[:, kc, :], in0=src, scalar1=S2[:, kc:kc + 1],
                                                                            scalar2=MODT[:, 3, kc, 0:1], op0=ALU.mult, op1=ALU.add),
                             reads=[("PS", bank), "S2", ("MODT", 3)], writes=[("H2F", kc)])
                norm_transpose(XB[:, xb], ("XB", xb), 32 + lt,
                               lambda kc: S2[:, kc:kc + 1], lambda kc: MODT[:, 3, kc, 0:1],
                               lambda kc, lt=lt: HT2[:, kc, lt * 128:(lt + 1) * 128], lambda kc, lt=lt: [("HT2", kc, lt)], extra=extra,
                               after_norm=lambda lt=lt, xb=xb: S.dma("sp", lambda e: e.dma_start(out=xn2_d[lt * 128:(lt + 1) * 128, :], in_=XB[:, xb]),
                                                                     "xb%d" % xb, reads=[("XB", xb)], writes=[("XN2D", lt)]), skip_main=True)
                for kc in range(8):
                    S.op("pe", lambda e, kc=kc: e.matmul(PS[3][:, 0:32], lhsT=H2F[:, kc, :], rhs=RWS[:, kc, :], start=(kc == 0), stop=False),
                         reads=[("H2F", kc), "RWS"], writes=[("PS", 3)], sig=False)
                S.op("pe", lambda e: e.matmul(PS[3][:, 0:32], lhsT=ONESROW[0:1, :], rhs=RBROW[0:1, :], start=False, stop=True),
                     reads=["ONESROW", "RBROW"], writes=[("PS", 3)])
                S.op("act", lambda e: e.copy(out=LG[:], in_=PS[3][:, 0:32]), reads=[("PS", 3)], writes=["LG"])
                S.op("dve", lambda e: e.max(out=MX8[:], in_=LG[:]), reads=["LG"], writes=["MX8"])
                gt = GATES[:, lt, :]
                gk_ = ("GATES", lt)
                S.op("dve", lambda e, gt=gt: e.tensor_scalar(out=gt, in0=LG[:], scalar1=MX8[:, 3:4], scalar2=None, op0=ALU.is_ge), reads=["LG", "MX8"], writes=[gk_])
                S.op("dve", lambda e: e.tensor_scalar(out=LG[:], in0=LG[:], scalar1=MX8[:, 0:1], scalar2=None, op0=ALU.subtract), reads=["LG", "MX8", gk_], writes=["LG"])
                S.op("act", lambda e: e.activation(out=LG[:], in_=LG[:], func=AF.Exp), reads=["LG"], writes=["LG"])
                S.op("dve", lambda e, gt=gt: e.tensor_tensor(out=gt, in0=gt, in1=LG[:], op=ALU.mult), reads=["LG", gk_], writes=[gk_])
                S.op("dve", lambda e, gt=gt: e.reduce_sum(out=MX8[:, 7:8], in_=gt, axis=AXL.X), reads=[gk_, "MX8"], writes=["MX8"])
                S.op("dve", lambda e: e.reciprocal(out=MX8[:, 7:8], in_=MX8[:, 7:8]), reads=["MX8"], writes=["MX8"])
                S.op("dve", lambda e, gt=gt: e.tensor_scalar(out=gt, in0=gt, scalar1=MX8[:, 7:8], scalar2=None, op0=ALU.mult), reads=[gk_, "MX8"], writes=[gk_])

        S.barrier()
        RE = Region([(0, 176)])
        W1Gs = RE.alloc([128, 2, 8192], BF16)
        W1Us = RE.alloc([128, 2, 8192], BF16)
        W2s = RE.alloc([128, 2, 8192], BF16)
        XG = RE.alloc([128, 2, 2, 1024])
        HG = RE.alloc([128, 2, 8, PSZ], BF16)
        ACTT = RE.alloc([128, 8, PSZ], BF16)
        YS = RE.alloc([128, 2, 1024])
        TMP = RE.alloc([128, 6, 512])
        TMPn[0] = 6
        B1S = RE.alloc([128, 2, 2, 8])
        SLTF = RE.alloc([128, 128])
        SLTI = RE.alloc([128, 128], I32)
        TABT = RE.alloc([128, 256])
        IC = RE.alloc([128, 256])
        MSK = RE.alloc([128, 16, 32])
        CNT = RE.alloc([128, 32])
        PADD = RE.alloc([128, 32])
        PEND = RE.alloc([128, 32])
        RUNB = RE.alloc([128, 32])
        DEST = RE.alloc([128, 32])
        KEY = RE.alloc([128, 32])
        OH = RE.alloc([128, 32])
        MX = RE.alloc([128, 8])
        EK4 = RE.alloc([128, 4])
        DK = RE.alloc([128, 64])
        DKI = RE.alloc([128, 64], I32)
        CMP = RE.alloc([128, NPASS, 32])
        EP = RE.alloc([128, NPASS])
        SK = RE.alloc([128, NPASS])
        IDXWF = RE.alloc([128, NPASS])
        IDXWI = RE.alloc([128, NPASS], I32)
        GIDXF = RE.alloc([128, 2, 2])
        GIDXI = RE.alloc([128, 2, 2], I32)
        GT = RE.alloc([128, 2, 2, 2])
        ONES1 = RE.alloc([128, 128])
        SLTM = RE.alloc([128, 128])
        OOBT = RE.alloc([128, 256])
        GTS = RE.alloc([32, 128])
        B2S = RE.alloc([32, 1024])
        ACI = RE.alloc([128, 2, 1024])
        IOTAE = IC[:, 0:32]
        W64 = IC[:, 32:64]
        THR = IC[:, 64:128]
        PIDX = IC[:, 128:129]
        TOKF = IC[:, 129:145]
        ONE32 = IC[:, 145:177]
        TOK2 = IC[:, 177:209].rearrange("p (l two) -> p l two", two=2)
        G2 = XG[:, 0, 0, :].rearrange("p (l e d) -> p l e d", e=NE, d=2)
        S.dma("sp", lambda e: e.dma_start(out=B2S[:], in_=b2), "c6", writes=["B2S"])
        S.dma("sp", lambda e: e.dma_start(out=IC[:], in_=iconst), "c7", writes=["IC"])
        S.op("pool", lambda e: e.memset(ONES1[:], 1.0), writes=["ONES1"])
        S.op("pool", lambda e: e.memset(SLTM[:], 1.0), writes=["SLTM"])
        S.op("pool", lambda e: e.affine_select(out=SLTM[:], in_=SLTM[:], pattern=[[1, 128]], compare_op=ALU.is_gt, fill=0.0, base=0, channel_multiplier=-1),
             reads=["SLTM"], writes=["SLTM"])
        S.op("pool", lambda e: e.memset(OOBT[:], 1.0e6), writes=["OOBT"])
        S.dma("sp", lambda e: e.dma_start(out=slot_d.rearrange("(q p) o -> q (p o)", p=128), in_=OOBT[:]), "c8", reads=["OOBT"], writes=["SLOTD"])
        S.op("dve", lambda e: e.tensor_copy(out=G2, in_=GATES[:].unsqueeze(3).to_broadcast([128, 16, NE, 2])), reads=[("GATES", lt) for lt in range(16)],
             writes=[("XG", 0), ("XG", 1)])
        S.dma("sp", lambda e: e.dma_start(out=gates_d.rearrange("(l p e) o -> p l (e o)", p=128, e=NE), in_=G2.rearrange("p l e d -> p l (e d)")), "c9",
              reads=[("XG", 0), ("XG", 1)], writes=["GATESD"])
        S.op("pool", lambda e: e.memset(XG[:], 0.0), writes=[("XG", 0), ("XG", 1)])
        for lt in range(16):
            S.op("pe", lambda e, lt=lt: e.transpose(out=PS[3][0:32, 0:128], in_=GATES[:, lt, :], identity=ID32[:]), reads=[("GATES", lt), "ID32"], writes=[("PS", 3)])
            S.op("act", lambda e: e.copy(out=GTS[:], in_=PS[3][0:32, 0:128]), reads=[("PS", 3)], writes=["GTS"])
            ab = lt % 2
            for nh in range(2):
                S.op("pe", lambda e, nh=nh: e.matmul(PS[nh][:], lhsT=GTS[:], rhs=B2S[:, nh * 512:(nh + 1) * 512], start=True, stop=True),
                     reads=["GTS", "B2S"], writes=[("PS", nh)])
                S.op("act", lambda e, nh=nh, ab=ab: e.copy(out=ACI[:, ab, nh * 512:(nh + 1) * 512], in_=PS[nh][:]), reads=[("PS", nh)], writes=[("ACI", ab)])
            S.dma("sp", lambda e, lt=lt, ab=ab: e.dma_start(out=acc_d[lt * 128:(lt + 1) * 128, :], in_=ACI[:, ab]), "aci%d" % ab, reads=[("ACI", ab)], writes=["ACCD"])
        gk_all = [("GATES", lt) for lt in range(16)]
        S.op("dve", lambda e: e.tensor_scalar(out=MSK[:], in0=GATES[:], scalar1=0.0, scalar2=None, op0=ALU.is_gt), reads=gk_all, writes=["MSK"])
        for lt in range(16):
            S.op("pe", lambda e, lt=lt: e.matmul(PS[2][:, 0:32], lhsT=ONES1[:], rhs=MSK[:, lt, :], start=(lt == 0), stop=(lt == 15)),
                 reads=["ONES1", "MSK"], writes=[("PS", 2)], sig=(lt == 15))
        S.op("dve", lambda e: e.tensor_copy(out=CNT[:], in_=PS[2][:, 0:32]), reads=[("PS", 2)], writes=["CNT"])
        S.op("dve", lambda e: e.tensor_scalar(out=PADD[:], in0=CNT[:], scalar1=0.0, scalar2=None, op0=ALU.is_gt), reads=["CNT"], writes=["PADD"])
        for j in range(1, 8):
            S.op("dve", lambda e, j=j: e.scalar_tensor_tensor(out=PADD[:], in0=CNT[:], scalar=float(PSZ * j), in1=PADD[:], op0=ALU.is_gt, op1=ALU.add),
                 reads=["CNT", "PADD"], writes=["PADD"])
        S.op("dve", lambda e: e.tensor_scalar(out=PADD[:], in0=PADD[:], scalar1=float(PSZ), scalar2=None, op0=ALU.mult), reads=["PADD"], writes=["PADD"])
        S.op("dve", lambda e: e.tensor_tensor_scan(out=PEND[:], data0=ONE32, data1=PADD[:], initial=0.0, op0=ALU.mult, op1=ALU.add),
             reads=["PADD", "IC"], writes=["PEND"])
        S.op("dve", lambda e: e.tensor_tensor(out=RUNB[:], in0=PEND[:], in1=PADD[:], op=ALU.subtract), reads=["PEND", "PADD"], writes=["RUNB"])
        S.op("dve", lambda e: e.tensor_tensor(out=CMP[:], in0=PEND[:].unsqueeze(1).to_broadcast([128, NPASS, 32]),
                                            in1=THR.unsqueeze(2).to_broadcast([128, NPASS, 32]), op=ALU.is_le), reads=["PEND", "IC"], writes=["CMP"])
        S.op("dve", lambda e: e.reduce_sum(out=EP[:], in_=CMP[:], axis=AXL.X), reads=["CMP"], writes=["EP"])
        S.op("dve", lambda e: e.tensor_scalar(out=EP[:], in0=EP[:], scalar1=31.0, scalar2=None, op0=ALU.min), reads=["EP"], writes=["EP"])
        S.op("dve", lambda e: e.tensor_scalar(out=IDXWF[:], in0=EP[:], scalar1=128.0, scalar2=PIDX, op0=ALU.mult, op1=ALU.add), reads=["EP", "IC"], writes=["IDXWF"])
        S.op("dve", lambda e: e.tensor_tensor(out=SK[:, 2:NPASS], in0=EP[:, 2:NPASS], in1=EP[:, 0:NPASS - 2], op=ALU.is_equal), reads=["EP"], writes=["SK"])
        S.op("dve", lambda e: e.scalar_tensor_tensor(out=IDXWF[:, 2:NPASS], in0=SK[:, 2:NPASS], scalar=1.0e7, in1=IDXWF[:, 2:NPASS], op0=ALU.mult, op1=ALU.add),
             reads=["SK", "IDXWF"], writes=["IDXWF"])
        S.op("dve", lambda e: e.tensor_copy(out=IDXWI[:], in_=IDXWF[:]), reads=["IDXWF"], writes=["IDXWI"])
        for lt in range(16):
            S.op("pe", lambda e, lt=lt: e.matmul(PS[0][:, 0:32], lhsT=SLTM[:], rhs=MSK[:, lt, :], start=True, stop=True), reads=["SLTM", "MSK"], writes=[("PS", 0)])
            S.op("pe", lambda e, lt=lt: e.matmul(PS[1][:, 0:32], lhsT=ONES1[:], rhs=MSK[:, lt, :], start=True, stop=True), reads=["ONES1", "MSK"], writes=[("PS", 1)])
            S.op("dve", lambda e: e.tensor_tensor(out=DEST[:], in0=PS[0][:, 0:32], in1=RUNB[:], op=ALU.add), reads=[("PS", 0), "RUNB"], writes=["DEST"])
            S.op("dve", lambda e: e.tensor_tensor(out=RUNB[:], in0=PS[1][:, 0:32], in1=RUNB[:], op=ALU.add), reads=[("PS", 1), "RUNB", "DEST"], writes=["RUNB"])
            S.op("dve", lambda e, lt=lt: e.tensor_tensor(out=KEY[:], in0=MSK[:, lt, :], in1=W64, op=ALU.mult), reads=["MSK", "IC"], writes=["KEY"])
            S.op("dve", lambda e: e.max(out=MX[:], in_=KEY[:]), reads=["KEY"], writes=["MX"])
            S.op("dve", lambda e: e.tensor_scalar(out=EK4[:], in0=MX[:, 0:4], scalar1=-1.0, scalar2=64.0, op0=ALU.mult, op1=ALU.add), reads=["MX"], writes=["EK4"])
            for k in range(4):
                S.op("dve", lambda e, k=k: e.tensor_scalar(out=OH[:], in0=IOTAE, scalar1=EK4[:, k:k + 1], scalar2=None, op0=ALU.is_equal), reads=["EK4", "IC"], writes=["OH"])
                S.op("dve", lambda e: e.tensor_tensor(out=OH[:], in0=OH[:], in1=DEST[:], op=ALU.mult), reads=["OH", "DEST"], writes=["OH"])
                S.op("dve", lambda e, lt=lt, k=k: e.reduce_sum(out=DK[:, lt * 4 + k:lt * 4 + k + 1], in_=OH[:], axis=AXL.X), reads=["OH"], writes=[("DK", lt)])
        S.op("dve", lambda e: e.tensor_copy(out=DKI[:], in_=DK[:]), reads=[("DK", lt) for lt in range(16)], writes=["DKI"])
        for i in range(64):
            S.dma("pool", lambda e, i=i: e.indirect_dma_start(out=slot_d, out_offset=bass.IndirectOffsetOnAxis(ap=DKI[:, i:i + 1], axis=0),
                                                             in_=TOK2[:, i // 4, :], in_offset=None, bounds_check=bnd(e, NSLOT - 1), oob_is_err=False),
                  "sct", reads=["DKI", "IC", "SLOTD"], writes=[("SLOTS", i)])
        S.dma("sp", lambda e: e.dma_start(out=TABT[:], in_=slot_d.rearrange("(q p) o -> q (p o)", p=128)), "c8",
              reads=[("SLOTS", i) for i in range(64)], writes=["TABT"])
        S.op("pe", lambda e: e.transpose(out=PS[2][:, 0:128], in_=TABT[:].rearrange("p (a two) -> p a two", two=2)[:, :, 0], identity=ID32[:]), reads=["TABT", "ID32"], writes=[("PS", 2)])
        S.op("act", lambda e: e.copy(out=SLTF[:], in_=PS[2][:, 0:128]), reads=[("PS", 2)], writes=["SLTF"])
        S.op("dve", lambda e: e.tensor_copy(out=SLTI[:], in_=SLTF[:]), reads=["SLTF"], writes=["SLTI"])

        npass = min(NPASS, (4 * SEQ + NE * (PSZ - 1)) // PSZ) if stage == "full" else (1 if stage in ("mA", "mB", "mC", "mD", "mC1", "mC2") else 0)

        def issue_loads(p):
            buf = p % 2
            for j in range(2):
                q = 2 * p + j
                S.dma("pool", lambda e, buf=buf, j=j, q=q: e.indirect_dma_start(
                    out=XG[:, buf, j, :], out_offset=None, in_=xn2_d, in_offset=bass.IndirectOffsetOnAxis(ap=SLTI[:, q:q + 1], axis=0),
                    bounds_check=bnd(e, SEQ - 1), oob_is_err=False), "xg%d" % buf,
                    reads=["SLTI", ("XG", buf)] + [("XN2D", lt) for lt in range(16)], writes=[("XG", buf)])
            S.op("dve", lambda e, buf=buf, p=p: e.tensor_scalar(out=GIDXF[:, buf, :], in0=SLTF[:, 2 * p:2 * p + 2], scalar1=32.0, scalar2=EP[:, p:p + 1],
                                                              op0=ALU.mult, op1=ALU.add), reads=["SLTF", "EP"], writes=[("GIDXF", buf)])
            S.op("dve", lambda e, buf=buf: e.tensor_copy(out=GIDXI[:, buf, :], in_=GIDXF[:, buf, :]), reads=[("GIDXF", buf)], writes=[("GIDXI", buf)])
            S.op("pool", lambda e, buf=buf: e.memset(GT[:, buf], 0.0), writes=[("GT", buf)])
            for j in range(2):
                S.dma("pool", lambda e, buf=buf, j=j: e.indirect_dma_start(
                    out=GT[:, buf, j, :], out_offset=None, in_=gates_d, in_offset=bass.IndirectOffsetOnAxis(ap=GIDXI[:, buf, j:j + 1], axis=0),
                    bounds_check=bnd(e, SEQ * NE - 1), oob_is_err=False), "gt%d" % buf, reads=[("GIDXI", buf), ("GT", buf), "GATESD"], writes=[("GT", buf)])
            S.op("dve", lambda e, buf=buf: e.tensor_scalar(out=GT[:, buf], in0=GT[:, buf], scalar1=1.0 / 1.702, scalar2=None, op0=ALU.mult),
                 reads=[("GT", buf)], writes=[("GT", buf)])
            if stage == "mA":
                return
            sl = p % 2
            idx = IDXWI[:, p:p + 1]
            for nm, dst, srcw in (("w1g", W1Gs, w1g), ("w1u", W1Us, w1u), ("w2", W2s, w2)):
                S.dma("pool", lambda e, dst=dst, srcw=srcw, sl=sl, idx=idx: e.indirect_dma_start(
                    out=dst[:, sl, :], out_offset=None, in_=srcw,
                    in_offset=bass.IndirectOffsetOnAxis(ap=idx, axis=0), bounds_check=bnd(e, NE * 128 - 1), oob_is_err=False),
                    "%s%d" % (nm, sl), reads=["IDXWI", (nm, sl)], writes=[(nm, sl)])
            for gi, srcb in ((0, b1gt), (1, b1ut)):
                S.dma("pool", lambda e, gi=gi, srcb=srcb, sl=sl, idx=idx: e.indirect_dma_start(
                    out=B1S[:, sl, gi, :], out_offset=None, in_=srcb, in_offset=bass.IndirectOffsetOnAxis(ap=idx, axis=0),
                    bounds_check=bnd(e, NE * 128 - 1), oob_is_err=False), "b1s%d" % sl, reads=["IDXWI", ("B1S", sl)], writes=[("B1S", sl)])

        if npass > 0:
            issue_loads(0)
        for p in range(npass):
            buf = p % 2
            sl = p % 2
            if p + 1 < npass:
                issue_loads(p + 1)
            if stage in ("mA", "mB"):
                continue
            for pr in range(4):
                bank = pr % 2
                for h in range(2):
                    kc = 2 * pr + h
                    off = h * PSZ
                    for j in range(2):
                        S.op("pe", lambda e, kc=kc, j=j, bank=bank, off=off, buf=buf: e.transpose(out=PS[bank][:, off + j * 128:off + (j + 1) * 128],
                                                                                               in_=XG[:, buf, j, kc * 128:(kc + 1) * 128], identity=ID32[:]),
                             reads=[("XG", buf), "ID32"], writes=[("PS", bank)], sig=(h == 1 and j == 1))
                for h in range(2):
                    kc = 2 * pr + h
                    src = PS[bank][:, h * PSZ:(h + 1) * PSZ]
                    if bank == 0:
                        S.op("act", lambda e, kc=kc, src=src, buf=buf: e.activation(out=HG[:, buf, kc, :], in_=src, func=AF.Identity, bias=MODT[:, 3, kc, 0:1],
                                                                                  scale=S2[:, kc:kc + 1]), reads=[("PS", bank)], writes=[("HG", buf, kc)])
                    else:
                        S.op("dve", lambda e, kc=kc, src=src, buf=buf: e.tensor_scalar(out=HG[:, buf, kc, :], in0=src, scalar1=S2[:, kc:kc + 1],
                                                                                     scalar2=MODT[:, 3, kc, 0:1], op0=ALU.mult, op1=ALU.add),
                             reads=[("PS", bank)], writes=[("HG", buf, kc)])
            if stage == "mC1":
                continue
            for fc in range(8):
                gbk, ubk = 2 + fc % 2, 4 + fc % 2
                for kc in range(8):
                    S.op("pe", lambda e, kc=kc, fc=fc, gbk=gbk, sl=sl, buf=buf: e.matmul(PS[gbk][:, 0:PSZ], lhsT=W1Gs[:, sl, fc * 1024 + kc * 128:fc * 1024 + (kc + 1) * 128],
                                                                                      rhs=HG[:, buf, kc, :], start=(kc == 0), stop=(kc == 7)),
                         reads=[("w1g", sl), ("HG", buf, kc)], writes=[("PS", gbk)], sig=(kc == 7))
                for kc in range(8):
                    S.op("pe", lambda e, kc=kc, fc=fc, ubk=ubk, sl=sl, buf=buf: e.matmul(PS[ubk][:, 0:PSZ], lhsT=W1Us[:, sl, fc * 1024 + kc * 128:fc * 1024 + (kc + 1) * 128],
                                                                                      rhs=HG[:, buf, kc, :], start=(kc == 0), stop=(kc == 7)),
                         reads=[("w1u", sl), ("HG", buf, kc)], writes=[("PS", ubk)], sig=(kc == 7))
                gv, gvk = tmp()
                uv, uvk = tmp()
                sg, sgk = tmp()
                S.op("dve", lambda e, gv=gv, gbk=gbk, sl=sl, fc=fc: e.tensor_scalar(out=gv[:, 0:PSZ], in0=PS[gbk][:, 0:PSZ], scalar1=B1S[:, sl, 0, fc:fc + 1], scalar2=7.0,
                                                                                 op0=ALU.add, op1=ALU.min), reads=[("PS", gbk), ("B1S", sl)], writes=[gvk])
                S.op("act", lambda e, uv=uv, ubk=ubk, sl=sl, fc=fc: e.activation(out=uv[:, 0:PSZ], in_=PS[ubk][:, 0:PSZ], func=AF.Identity, bias=B1S[:, sl, 1, fc:fc + 1], scale=1.0),
                     reads=[("PS", ubk), ("B1S", sl)], writes=[uvk])
                S.op("act", lambda e, sg=sg, gv=gv: e.activation(out=sg[:, 0:PSZ], in_=gv[:, 0:PSZ], func=AF.Silu, scale=1.702), reads=[gvk], writes=[sgk])
                S.op("dve", lambda e, uv=uv: e.tensor_scalar(out=uv[:, 0:PSZ], in0=uv[:, 0:PSZ], scalar1=-7.0, scalar2=7.0, op0=ALU.max, op1=ALU.min), reads=[uvk], writes=[uvk])
                S.op("dve", lambda e, sg=sg, uv=uv, fc=fc: e.scalar_tensor_tensor(out=ACTT[:, fc, :], in0=uv[:, 0:PSZ], scalar=1.0, in1=sg[:, 0:PSZ], op0=ALU.add, op1=ALU.mult),
                     reads=[sgk, uvk], writes=[("ACTT", fc)])
            if stage == "mC2":
                continue
            for j in range(2):
                q = 2 * p + j
                for nh in range(2):
                    bank = 6 + nh
                    for fc in range(8):
                        S.op("pe", lambda e, fc=fc, j=j, nh=nh, bank=bank, sl=sl: e.matmul(PS[bank][:], lhsT=ACTT[:, fc, j * 128:(j + 1) * 128],
                                                                                        rhs=W2s[:, sl, fc * 1024 + nh * 512:fc * 1024 + (nh + 1) * 512],
                                                                                        start=(fc == 0), stop=(fc == 7)),
                             reads=[("w2", sl), ("ACTT", fc)], writes=[("PS", bank)], sig=(fc == 7))
                    if False:
                        S.op("act", lambda e, j=j, nh=nh, bank=bank, buf=buf: e.activation(out=YS[:, j, nh * 512:(nh + 1) * 512], in_=PS[bank][:], func=AF.Identity,
                                                                                        bias=0.0, scale=GT[:, buf, j, 0:1]),
                             reads=[("PS", bank), ("GT", buf)], writes=[("YS", j, nh)])
                    else:
                        S.op("dve", lambda e, j=j, nh=nh, bank=bank, buf=buf: e.tensor_scalar(out=YS[:, j, nh * 512:(nh + 1) * 512], in0=PS[bank][:], scalar1=GT[:, buf, j, 0:1],
                                                                                           scalar2=None, op0=ALU.mult),
                             reads=[("PS", bank), ("GT", buf)], writes=[("YS", j, nh)])
                if stage == "mC":
                    continue
                S.dma("sp", lambda e, j=j, q=q: e.dma_start(out=yslot_d[q * 128:(q + 1) * 128, :], in_=YS[:, j, :]),
                      "ysc%d" % j, reads=[("YS", j, 0), ("YS", j, 1)], writes=[("YSLOT", q)])

        S.barrier()
        FG = Region([(96, 104)]).alloc([128, 1024])
        TMP = Region([(120, 128)]).alloc([128, 4, 512])
        TMPn[0] = 4
        ACB = Region([(0, 8)]).alloc([128, 2, 1024])
        YG = Region([(8, 40)]).alloc([128, 2, 4, 1024])
        S.dma("sp", lambda e: e.dma_start(out=FG[:], in_=fgrow[0:1, :].to_broadcast([128, 1024])), "c5", writes=["FG"])
        out_tokens = []
        for lt in range(16):
            xb = lt % 3
            S.dma("sp", lambda e, lt=lt, xb=xb: e.dma_start(out=XB[:, xb], in_=x1_d[lt * 128:(lt + 1) * 128, :]), "xb%d" % xb,
                  reads=[("X1D", lt)], writes=[("XB", xb)])
            ab = lt % 2
            S.dma("sp", lambda e, lt=lt, ab=ab: e.dma_start(out=ACB[:, ab], in_=acc_d[lt * 128:(lt + 1) * 128, :]), "acb%d" % ab, reads=["ACCD"], writes=[("ACB", ab)])
            for k in range(4):
                S.dma("pool", lambda e, lt=lt, ab=ab, k=k: e.indirect_dma_start(
                    out=YG[:, ab, k, :], out_offset=None, in_=yslot_d, in_offset=bass.IndirectOffsetOnAxis(ap=DKI[:, lt * 4 + k:lt * 4 + k + 1], axis=0),
                    bounds_check=bnd(e, NSLOT - 1), oob_is_err=False), "yg%d" % ab, reads=["DKI", ("YG", ab)] + [("YSLOT", q) for q in range(2 * NPASS)],
                    writes=[("YG", ab)])
            for k in range(4):
                S.op("pool" if k % 2 == 0 else "dve", lambda e, ab=ab, k=k: e.tensor_tensor(out=ACB[:, ab], in0=ACB[:, ab], in1=YG[:, ab, k, :], op=ALU.add),
                     reads=[("ACB", ab), ("YG", ab)], writes=[("ACB", ab)])
            for nh in range(2):
                mo, mok = tmp()
                S.op("dve", lambda e, mo=mo, ab=ab, nh=nh: e.tensor_tensor(out=mo[:], in0=ACB[:, ab, nh * 512:(nh + 1) * 512], in1=MODB[:, 1, nh * 512:(nh + 1) * 512],
                                                                         op=ALU.mult), reads=[("ACB", ab), ("MODB", 1, nh)], writes=[mok])
                S.op("pool", lambda e, mo=mo, xb=xb, nh=nh: e.tensor_tensor(out=XB[:, xb, nh * 512:(nh + 1) * 512], in0=XB[:, xb, nh * 512:(nh + 1) * 512],
                                                                          in1=mo[:], op=ALU.add), reads=[mok, ("XB", xb)], writes=[("XB", xb)])
            col = 48 + (lt % 16)
            xt_ap = XB[:, xb]
            xkey = ("XB", xb)
            S.op("act", lambda e, xt_ap=xt_ap: e.activation(out=JUNK[:], in_=xt_ap, func=AF.Square), reads=[xkey], writes=["JUNK"])
            S.op("dve", lambda e, col=col: e.reduce_sum(out=SMALL[:, col:col + 1], in_=JUNK[:], axis=AXL.X), reads=["JUNK"], writes=[("SM", col)])
            S.op("act", lambda e, col=col: e.activation(out=SMALL[:, col:col + 1], in_=SMALL[:, col:col + 1], func=AF.Ln, bias=EPS, scale=1.0 / D),
                 reads=[("SM", col)], writes=[("SM", col)])
            S.op("act", lambda e, col=col: e.activation(out=SMALL[:, col:col + 1], in_=SMALL[:, col:col + 1], func=AF.Exp, scale=-0.5), reads=[("SM", col)], writes=[("SM", col)])
            S.op("dve", lambda e, col=col, xt_ap=xt_ap: e.scalar_tensor_tensor(out=xt_ap, in0=xt_ap, scalar=SMALL[:, col:col + 1], in1=FG[:], op0=ALU.mult, op1=ALU.mult),
                 reads=[xkey, ("SM", col), "FG"], writes=[xkey])
            tok = S.dma("sp", lambda e, lt=lt, xb=xb: e.dma_start(out=out[lt * 128:(lt + 1) * 128, :], in_=XB[:, xb]), "xb%d" % xb, reads=[xkey], writes=[("OUT", lt)])
            out_tokens.append(tok)
        S.wait_all("sp", out_tokens)
        S.emit()
    return nc


def _fm(v):
    v = np.asarray(v, np.float32).reshape(-1, 128)
    return np.ascontiguousarray(v.T)


def pack_shared(inp):
    f32 = np.float32
    cp = np.zeros((128, NCP), f32)

    def put(name, arr):
        arr = np.asarray(arr, f32)
        cp[:, CO[name]:CO[name] + arr.shape[1]] = arr

    put("n1g", _fm(inp["norm1_g"][0]))
    put("n2g", _fm(inp["norm2_g"][0]))
    put("convw", np.concatenate([_fm(inp["lru_conv_w"][0, k]) for k in range(4)], axis=1))
    put("convb", _fm(inp["lru_conv_b"][0]))
    put("ba", np.concatenate([_fm(inp["lru_ba"][0, d].reshape(-1)) for d in range(2)], axis=1))
    put("bx", np.concatenate([_fm(inp["lru_bx"][0, d].reshape(-1)) for d in range(2)], axis=1))
    put("lam", np.concatenate([_fm(inp["lru_lam"][0, d]) for d in range(2)], axis=1))
    put("lb0", _fm(inp["hg_lb_logits"][0]))
    put("lb1", _fm(inp["hg_lb_logits"][1]))
    put("hgng", np.asarray(inp["hg_norm_g"][0], f32).reshape(128, 1))
    put("adab", _fm(inp["ada_b"][0]))
    b1 = np.asarray(inp["moe_b1"][0], f32)
    b1g = b1[:, 0::2].reshape(NE, 8, 128)
    b1u = b1[:, 1::2].reshape(NE, 8, 128)
    put("b1g", np.ascontiguousarray(b1g.transpose(2, 0, 1)).reshape(128, NE * 8))
    put("b1u", np.ascontiguousarray(b1u.transpose(2, 0, 1)).reshape(128, NE * 8))

    def kmajor(w):
        w = np.asarray(w, f32)
        return np.ascontiguousarray(w.reshape(8, 128, w.shape[1]).transpose(1, 0, 2))

    sh = {"cpack": cp}
    aw = np.asarray(inp["ada_w"][0], f32)
    sh["adaw"] = np.ascontiguousarray(aw.reshape(8, 128, 6, 1024).transpose(2, 1, 0, 3))
    sh["adabrow"] = np.asarray(inp["ada_b"][0], f32).reshape(1, 6144)
    wi = np.asarray(inp["w_in"][0], f32).reshape(8, 128, 9, 8, 128)
    sh["win"] = np.ascontiguousarray(wi[:, :, 0:7].transpose(3, 1, 2, 0, 4))
    sh["win78"] = np.ascontiguousarray(wi[:, :, 7:9].transpose(3, 1, 2, 0, 4))
    wa = np.asarray(inp["lru_wa"][0], f32)
    wx = np.asarray(inp["lru_wx"][0], f32)
    lw = np.stack([wa[0], wx[0], wa[1], wx[1]], axis=0)
    sh["lruw"] = np.ascontiguousarray(lw.transpose(2, 1, 0, 3))
    sh["wba"] = kmajor(inp["w_branch_a"][0])
    sh["wbb"] = kmajor(inp["w_branch_b"][0])
    sh["wout"] = kmajor(inp["w_out"][0])
    sh["fgrow"] = np.asarray(inp["final_g"], f32).reshape(1, 1024)
    sh["rw"] = kmajor(inp["router_w"][0])
    sh["rbrow"] = np.asarray(inp["router_b"][0], f32).reshape(1, 32)
    w1 = np.asarray(inp["moe_w1"][0], f32)
    w1r = w1.reshape(NE, 8, 128, 8, 128, 2)
    sh["w1g"] = np.ascontiguousarray(w1r[..., 0].transpose(0, 2, 3, 1, 4)).reshape(NE * 128, 8192)
    sh["w1u"] = np.ascontiguousarray(w1r[..., 1].transpose(0, 2, 3, 1, 4)).reshape(NE * 128, 8192)
    w2_ = np.asarray(inp["moe_w2"][0], f32)
    sh["w2"] = np.ascontiguousarray(w2_.reshape(NE, 8, 128, 1024).transpose(0, 2, 1, 3)).reshape(NE * 128, 8192)
    sh["b1gt"] = np.ascontiguousarray(b1g.transpose(0, 2, 1)).reshape(NE * 128, 8)
    sh["b1ut"] = np.ascontiguousarray(b1u.transpose(0, 2, 1)).reshape(NE * 128, 8)
    ic = np.zeros((128, 256), f32)
    ic[:, 0:32] = np.arange(32)[None, :]
    ic[:, 32:64] = 64 - np.arange(32)[None, :]
    ic[:, 64:128] = (PSZ * np.arange(NPASS))[None, :]
    ic[:, 128] = np.arange(128)
    ic[:, 129:145] = np.arange(16)[None, :] * 128 + np.arange(128)[:, None]
    ic[:, 145:177] = 1.0
    ic[:, 177:209] = np.repeat(np.arange(16)[None, :] * 128 + np.arange(128)[:, None], 2, axis=1)
    sh["iconst"] = ic
    sh["b2"] = np.ascontiguousarray(np.asarray(inp["moe_b2"][0], f32))
    return sh


def pack_core(inp, b):
    f32 = np.float32
    xin = np.concatenate([np.asarray(inp["ctx"][b], f32), np.asarray(inp["x"][b], f32)], axis=0)
    cv = np.stack([_fm(inp["c"][b]), _fm(inp["c_ctx"])], axis=2)
    return {"xin": np.ascontiguousarray(xin), "cvec": np.ascontiguousarray(cv)}


def kernel(**inputs):
    n = 8
    sh = pack_shared(inputs)
    in_maps = []
    for b in range(n):
        m = dict(sh)
        m.update(pack_core(inputs, b))
        in_maps.append(m)
    nc = build_nc("full")
    res = run_bass_kernel_spmd(nc, in_maps, core_ids=list(range(n)))
    return np.stack([np.asarray(r["out"], np.float32) for r in res.results], axis=0)
```

```python
import types
import numpy as np
from contextlib import ExitStack
import concourse.bass as bass
import concourse.mybir as mybir
from concourse.bass_utils import run_bass_kernel_spmd

F32 = mybir.dt.float32
BF16 = mybir.dt.bfloat16
I32 = mybir.dt.int32
AF = mybir.ActivationFunctionType
ALU = mybir.AluOpType
AXL = mybir.AxisListType

D = 1024
SEQ = 2048
CTX = 256
T = SEQ + CTX
NT = T // 128
NE = 32
EPS = 1e-6
NPASS = 64
PSZ = 256
NSLOT = NPASS * PSZ
ENGS = ("pe", "act", "dve", "pool", "sp")

CO = {}
_o = 0
for _n, _w in (("n1g", 8), ("n2g", 8), ("convw", 32), ("convb", 8), ("ba", 16), ("bx", 16), ("lam", 16),
               ("lb0", 8), ("lb1", 8), ("hgng", 1), ("adab", 48), ("b1g", 256), ("b1u", 256)):
    CO[_n] = _o
    _o += _w
NCP = _o


def _snap(fn):
    if fn.__closure__ is None:
        return fn
    cells = []
    for c in fn.__closure__:
        try:
            cells.append(types.CellType(c.cell_contents))
        except ValueError:
            cells.append(c)
    return types.FunctionType(fn.__code__, fn.__globals__, fn.__name__, fn.__defaults__, tuple(cells))


class Sched:
    def __init__(self, nc, stack):
        self.nc = nc
        self.stack = stack
        self.ops = {e: [] for e in ENGS}
        self.cnt = {e: 0 for e in ENGS}
        self.esem = {e: stack.enter_context(nc.semaphore("s_" + e)) for e in ENGS}
        self.known = {e: {} for e in ENGS}
        self.last_w = {}
        self.readers = {}
        self.dma_sems = {}

    def _need(self, reads, writes):
        need = []
        for k in reads:
            t = self.last_w.get(k)
            if t is not None:
                need.append(t)
        for k in writes:
            t = self.last_w.get(k)
            if t is not None:
                need.append(t)
            need.extend(self.readers.get(k, ()))
        return need

    def _emit_waits(self, eng, need):
        best = {}
        for (sem, val, src) in need:
            if src == eng and eng == "pe":
                continue
            key = id(sem)
            if key not in best or best[key][1] < val:
                best[key] = (sem, val)
        kn = self.known[eng]
        for key, (sem, val) in best.items():
            if kn.get(key, 0) >= val:
                continue
            kn[key] = val
            self.ops[eng].append(("wait", sem, val))

    def _commit(self, token, reads, writes):
        for k in reads:
            self.readers.setdefault(k, []).append(token)
        for k in writes:
            self.last_w[k] = token
            self.readers[k] = []

    def op(self, eng, fn, reads=(), writes=(), sig=True):
        fn = _snap(fn)
        self._emit_waits(eng, self._need(reads, writes))
        if sig:
            self.cnt[eng] += 1
            token = (self.esem[eng], self.cnt[eng], eng)
            self.ops[eng].append(("op", fn, self.esem[eng], 1))
        else:
            token = (self.esem[eng], self.cnt[eng] + 1, eng)
            self.ops[eng].append(("op", fn, None, 0))
        self._commit(token, reads, writes)
        return token

    def dma(self, eng, fn, semkey, reads=(), writes=()):
        fn = _snap(fn)
        if semkey not in self.dma_sems:
            self.dma_sems[semkey] = [self.stack.enter_context(self.nc.semaphore("d_%d" % len(self.dma_sems))), 0]
        ent = self.dma_sems[semkey]
        self._emit_waits(eng, self._need(reads, writes))
        ent[1] += 16
        token = (ent[0], ent[1], "dma")
        self.ops[eng].append(("op", fn, ent[0], 16))
        self._commit(token, reads, writes)
        return token

    def wait_all(self, eng, tokens):
        self._emit_waits(eng, tokens)

    def barrier(self):
        toks = [(self.esem[e], self.cnt[e], "bar") for e in ENGS if self.cnt[e] > 0]
        toks += [(v[0], v[1], "dma") for v in self.dma_sems.values()]
        for e in ENGS:
            self._emit_waits(e, toks)

    def emit(self):
        with self.nc.Block() as block:
            def mk(name):
                def body(e):
                    for item in self.ops[name]:
                        if item[0] == "wait":
                            e.wait_ge(item[1], item[2])
                        else:
                            ins = item[1](e)
                            if item[2] is not None:
                                ins.then_inc(item[2], item[3])
                return body
            block.tensor(mk("pe"))
            block.scalar(mk("act"))
            block.vector(mk("dve"))
            block.gpsimd(mk("pool"))
            block.sync(mk("sp"))


_BND = {}


def bnd(e, val):
    key = (id(e), val)
    if key not in _BND:
        r = e.alloc_register("bnd%d" % val)
        e.reg_mov(r, val)
        _BND[key] = r
    return _BND[key]


def chunk_range(c):
    if c == 0:
        return 0, 256
    return 256 + 512 * (c - 1), 256 + 512 * c


def build_nc(stage="full"):
    _BND.clear()
    nc = bass.Bass("TRN2", target_bir_lowering=False)

    def din(name, shape, dt=F32):
        return nc.dram_tensor(name, list(shape), dt, kind="ExternalInput").ap()

    xin = din("xin", [T, D])
    cvec = din("cvec", [128, 8, 2])
    cpack = din("cpack", [128, NCP])
    adaw = din("adaw", [6, 128, 8, 1024])
    adabrow = din("adabrow", [1, 6144])
    win = din("win", [8, 128, 7, 8, 128])
    win78 = din("win78", [8, 128, 2, 8, 128])
    lruw = din("lruw", [128, 8, 4, 128])
    wba = din("wba", [128, 8, 1024])
    wbb = din("wbb", [128, 8, 1024])
    wout = din("wout", [128, 8, 1024])
    fgrow = din("fgrow", [1, 1024])
    rw = din("rw", [128, 8, 32])
    rbrow = din("rbrow", [1, 32])
    w1g = din("w1g", [NE * 128, 8192])
    w1u = din("w1u", [NE * 128, 8192])
    w2 = din("w2", [NE * 128, 8192])
    b1gt = din("b1gt", [NE * 128, 8])
    b1ut = din("b1ut", [NE * 128, 8])
    iconst = din("iconst", [128, 256])
    b2 = din("b2", [NE, 1024])
    out = nc.dram_tensor("out", [SEQ, D], F32, kind="ExternalOutput").ap()
    dk = "Internal" if stage == "full" else "ExternalOutput"
    ya_d = nc.dram_tensor("ya_d", [8, 128, SEQ], BF16, kind=dk).ap()
    yb_d = nc.dram_tensor("yb_d", [8, 128, SEQ], BF16, kind=dk).ap()
    x1_d = nc.dram_tensor("x1_d", [SEQ, D], F32, kind=dk).ap()
    xn2_d = nc.dram_tensor("xn2_d", [SEQ, D], F32, kind="Internal").ap()
    acc_d = nc.dram_tensor("acc_d", [SEQ, D], F32, kind="Internal").ap()
    gates_d = nc.dram_tensor("gates_d", [SEQ * NE, 2], F32, kind="Internal").ap()
    slot_d = nc.dram_tensor("slot_d", [NSLOT, 2], F32, kind="Internal").ap()
    yslot_d = nc.dram_tensor("yslot_d", [NSLOT, D], F32, kind="Internal").ap()

    with ExitStack() as st:
        S = Sched(nc, st)

        def sb(name, shape, dt=F32):
            return st.enter_context(nc.sbuf_tensor(name, list(shape), dt))

        def finish():
            S.barrier()
            S.emit()
            return nc

        PS = [st.enter_context(nc.psum_tensor("ps%d" % i, [128, 512], F32)) for i in range(8)]
        ARENA = 176 * 1024
        AR = sb("AR", [128, ARENA // 2], BF16)

        class Region:
            def __init__(self, ranges):
                self.ranges = [[lo * 1024, hi * 1024] for lo, hi in ranges]

            def alloc(self, shape, dt=F32):
                nel = int(np.prod(shape[1:]))
                nb = nel * (4 if dt in (F32, I32) else 2)
                nb_al = (nb + 63) // 64 * 64
                for r in self.ranges:
                    if r[0] + nb_al <= r[1]:
                        off = r[0]
                        r[0] += nb_al
                        break
                else:
                    raise RuntimeError("arena region full for %s" % (shape,))
                v = AR[0:shape[0], off // 2:(off + nb) // 2]
                if dt in (F32, I32):
                    v = v.bitcast(dt)
                if len(shape) == 3:
                    v = v.rearrange("p (a b) -> p a b", b=shape[2])
                elif len(shape) == 4:
                    v = v.rearrange("p (a b c) -> p a b c", b=shape[2], c=shape[3])
                elif len(shape) == 5:
                    v = v.rearrange("p (a b c d) -> p a b c d", b=shape[2], c=shape[3], d=shape[4])
                return v

        CP = sb("CP", [128, NCP])
        CV = sb("CV", [128, 8, 2])
        SC = sb("SC", [128, 8, 2])
        ID32 = sb("ID32", [128, 128])
        ONESM = sb("ONESM", [128, 128])
        ONESROW = sb("ONESROW", [1, 128])
        TRI = sb("TRI", [128, 2, 128], BF16)
        MASKF = sb("MASKF", [128, 512], BF16)
        HMASK = sb("HMASK", [128, 2])
        MODT = sb("MODT", [128, 6, 8, 2])
        MODB = sb("MODB", [128, 2, 1024])
        S1 = sb("S1", [128, 8, 2])
        S2 = sb("S2", [128, 8])
        CL = sb("CL", [128, 16])
        CL2 = sb("CL2", [128, 16])
        LB = sb("LB", [128, 8])
        OML = sb("OML", [128, 8])
        NOML = sb("NOML", [128, 8])
        RWS = sb("RWS", [128, 8, 32])
        RBROW = sb("RBROW", [1, 32])
        GATES = sb("GATES", [128, 16, 32])
        SMALL = sb("SMALL", [128, 64])

        cpc = lambda name, i=0, n=1: CP[:, CO[name] + i: CO[name] + i + n]

        S.dma("sp", lambda e: e.dma_start(out=CP[:], in_=cpack), "c0", writes=["CP"])
        S.dma("sp", lambda e: e.dma_start(out=CV[:], in_=cvec), "c1", writes=["CV"])
        R0 = Region([(0, 140)])
        BIG = R0.alloc([128, 2, 8, 1024])
        SCB = R0.alloc([128, 8, 128])
        ADABROW = R0.alloc([1, 2, 1024])
        S.dma("sp", lambda e: e.dma_start(out=ADABROW[0:1, 0, :], in_=adabrow[0:1, 2048:3072]), "c2", writes=[("ADABROW", 0)])
        S.dma("sp", lambda e: e.dma_start(out=ADABROW[0:1, 1, :], in_=adabrow[0:1, 5120:6144]), "c2b", writes=[("ADABROW", 1)])
        S.dma("sp", lambda e: e.dma_start(out=RWS[:], in_=rw), "c3", writes=["RWS"])
        S.dma("sp", lambda e: e.dma_start(out=RBROW[:], in_=rbrow), "c4", writes=["RBROW"])

        S.op("pool", lambda e: e.memset(ID32[:], 0.0), writes=["ID32"])
        S.op("pool", lambda e: e.affine_select(out=ID32[:], in_=ID32[:], pattern=[[-1, 128]], compare_op=ALU.not_equal,
                                               fill=1.0, base=0, channel_multiplier=1), reads=["ID32"], writes=["ID32"])
        S.op("pool", lambda e: e.memset(ONESM[:], 1.0 / 128.0), writes=["ONESM"])
        S.op("pool", lambda e: e.memset(ONESROW[:], 1.0), writes=["ONESROW"])
        S.op("pool", lambda e: e.memset(MASKF[:], 1.0), writes=["MASKF"])
        S.op("pool", lambda e: e.memset(MASKF[:].rearrange("p (a b) -> p a b", b=64)[:, :, 0:1], 0.0),
             reads=["MASKF"], writes=["MASKF"])
        S.op("pool", lambda e: e.memset(HMASK[:], 0.0), writes=["HMASK"])
        S.op("pool", lambda e: e.memset(HMASK[0:64, 0:1], 1.0), reads=["HMASK"], writes=["HMASK"])
        S.op("pool", lambda e: e.memset(HMASK[64:128, 1:2], 1.0), reads=["HMASK"], writes=["HMASK"])
        S.op("pool", lambda e: e.memset(TRI[:], 0.0), writes=["TRI"])
        for blk in range(2):
            lo = blk * 64
            S.op("pool", lambda e, lo=lo: e.memset(TRI[lo:lo + 64, :, lo:lo + 64], 1.0), reads=["TRI"], writes=["TRI"])
        S.op("pool", lambda e: e.affine_select(out=TRI[:, 0, :], in_=TRI[:, 0, :], pattern=[[1, 128]], compare_op=ALU.is_ge,
                                               fill=0.0, base=0, channel_multiplier=-1), reads=["TRI"], writes=["TRI"])
        S.op("pool", lambda e: e.affine_select(out=TRI[:, 1, :], in_=TRI[:, 1, :], pattern=[[-1, 128]], compare_op=ALU.is_ge,
                                               fill=0.0, base=0, channel_multiplier=1), reads=["TRI"], writes=["TRI"])

        S.op("act", lambda e: e.activation(out=SC[:], in_=CV[:], func=AF.Silu), reads=["CV"], writes=["SC"])
        S.op("dve", lambda e: e.tensor_copy(out=SCB[:], in_=SC[:, :, 0:1].to_broadcast([128, 8, 128])), reads=["SC"], writes=["SCB"])
        S.op("act", lambda e: e.activation(out=CL[:], in_=cpc("lam", 0, 16), func=AF.Exp, scale=-1.0), reads=["CP"], writes=["CL"])
        S.op("act", lambda e: e.activation(out=CL[:], in_=CL[:], func=AF.Ln, bias=1.0, scale=1.0), reads=["CL"], writes=["CL"])
        S.op("dve", lambda e: e.tensor_scalar(out=CL2[:], in0=CL[:], scalar1=-16.0, scalar2=None, op0=ALU.mult), reads=["CL"], writes=["CL2"])
        S.op("dve", lambda e: e.tensor_scalar(out=CL[:], in0=CL[:], scalar1=-8.0, scalar2=None, op0=ALU.mult), reads=["CL", "CL2"], writes=["CL"])
        S.op("dve", lambda e: e.tensor_tensor(out=LB[:], in0=cpc("lb0", 0, 8), in1=cpc("lb1", 0, 8), op=ALU.subtract), reads=["CP"], writes=["LB"])
        S.op("act", lambda e: e.activation(out=LB[:], in_=LB[:], func=AF.Sigmoid), reads=["LB"], writes=["LB"])
        S.op("dve", lambda e: e.tensor_scalar(out=OML[:], in0=LB[:], scalar1=-1.0, scalar2=1.0, op0=ALU.mult, op1=ALU.add), reads=["LB"], writes=["OML"])
        S.op("dve", lambda e: e.tensor_scalar(out=NOML[:], in0=OML[:], scalar1=-1.0, scalar2=None, op0=ALU.mult), reads=["OML"], writes=["NOML"])

        HT = Region([(140, 176)]).alloc([128, 8, T], BF16)
        HT2 = Region([(64, 96)]).alloc([128, 8, SEQ], BF16)
        RX = Region([(104, 120)])
        XB = RX.alloc([128, 3, 1024])
        JUNK = RX.alloc([128, 1024])
        TMPn = [8]

        for j in range(6):
            buf = j % 2
            S.dma("sp", lambda e, j=j, buf=buf: e.dma_start(out=BIG[:, buf], in_=adaw[j]), "ada%d" % buf, writes=[("BIG", buf)])
            if j in (0, 1, 3, 4):
                pm = PS[j % 2]
                for fcn in range(8):
                    for kc in range(8):
                        S.op("pe", lambda e, pm=pm, fcn=fcn, kc=kc, buf=buf: e.matmul(
                            pm[:, fcn * 2:fcn * 2 + 2], lhsT=BIG[:, buf, kc, fcn * 128:(fcn + 1) * 128], rhs=SC[:, kc, :],
                            start=(kc == 0), stop=(kc == 7)),
                            reads=[("BIG", buf), "SC"], writes=[("PS", j % 2)], sig=(kc == 7))
                S.op("dve", lambda e, pm=pm, j=j: e.tensor_tensor(
                    out=MODT[:, j], in0=pm[:, 0:16].rearrange("p (a b) -> p a b", b=2),
                    in1=cpc("adab", j * 8, 8).unsqueeze(2).to_broadcast([128, 8, 2]), op=ALU.add),
                    reads=[("PS", j % 2), "CP"], writes=[("MODT", j)])
            else:
                jj = 0 if j == 2 else 1
                for nh in range(2):
                    pm = PS[2 + nh]
                    for kc in range(8):
                        S.op("pe", lambda e, pm=pm, kc=kc, buf=buf, nh=nh: e.matmul(
                            pm[:], lhsT=SCB[:, kc, :], rhs=BIG[:, buf, kc, nh * 512:(nh + 1) * 512], start=(kc == 0), stop=False),
                            reads=[("BIG", buf), "SCB"], writes=[("PS", 2 + nh)], sig=False)
                    S.op("pe", lambda e, pm=pm, j=j, nh=nh: e.matmul(
                        pm[:], lhsT=ONESROW[0:1, :], rhs=ADABROW[0:1, jj, nh * 512:(nh + 1) * 512], start=False, stop=True),
                        reads=["ONESROW", ("ADABROW", 0), ("ADABROW", 1)], writes=[("PS", 2 + nh)])
                    S.op("act", lambda e, pm=pm, jj=jj, nh=nh: e.copy(out=MODB[:, jj, nh * 512:(nh + 1) * 512], in_=pm[:]),
                         reads=[("PS", 2 + nh)], writes=[("MODB", jj, nh)])
        S.op("dve", lambda e: e.tensor_scalar(out=S1[:], in0=MODT[:, 1], scalar1=1.0, scalar2=None, op0=ALU.add), reads=[("MODT", 1)], writes=["S1"])
        S.op("dve", lambda e: e.tensor_tensor(out=S1[:], in0=S1[:], in1=cpc("n1g", 0, 8).unsqueeze(2).to_broadcast([128, 8, 2]), op=ALU.mult),
             reads=["S1", "CP"], writes=["S1"])
        S.op("dve", lambda e: e.tensor_scalar(out=S2[:], in0=MODT[:, 4, :, 0], scalar1=1.0, scalar2=None, op0=ALU.add), reads=[("MODT", 4)], writes=["S2"])
        S.op("dve", lambda e: e.tensor_tensor(out=S2[:], in0=S2[:], in1=cpc("n2g", 0, 8), op=ALU.mult), reads=["S2", "CP"], writes=["S2"])

        if stage == "p0":
            return finish()

        def norm_transpose(xt_ap, xkey, col, scale_ap_fn, bias_ap_fn, dst_fn, dst_keys_fn, extra=None, after_norm=None, skip_main=False):
            S.op("act", lambda e: e.activation(out=JUNK[:], in_=xt_ap, func=AF.Square), reads=[xkey], writes=["JUNK"])
            S.op("dve", lambda e: e.reduce_sum(out=SMALL[:, col:col + 1], in_=JUNK[:], axis=AXL.X), reads=["JUNK"], writes=[("SM", col)])
            S.op("act", lambda e: e.activation(out=SMALL[:, col:col + 1], in_=SMALL[:, col:col + 1], func=AF.Ln, bias=EPS, scale=1.0 / D),
                 reads=[("SM", col)], writes=[("SM", col)])
            S.op("act", lambda e: e.activation(out=SMALL[:, col:col + 1], in_=SMALL[:, col:col + 1], func=AF.Exp, scale=-0.5), reads=[("SM", col)], writes=[("SM", col)])
            S.op("dve", lambda e: e.tensor_scalar(out=xt_ap, in0=xt_ap, scalar1=SMALL[:, col:col + 1], scalar2=None, op0=ALU.mult),
                 reads=[xkey, ("SM", col)], writes=[xkey])
            if after_norm is not None:
                after_norm()
            if stage == "p1a":
                return
            for half in range(2):
                bank = 2 + half
                for q in range(4):
                    kc = half * 4 + q
                    S.op("pe", lambda e, kc=kc, q=q, bank=bank: e.transpose(out=PS[bank][:, q * 128:(q + 1) * 128],
                                                                          in_=xt_ap[:, kc * 128:(kc + 1) * 128], identity=ID32[:]),
                         reads=[xkey, "ID32"], writes=[("PS", bank)], sig=(q == 3))
                if stage == "p1b":
                    continue
                for q in range(4):
                    kc = half * 4 + q
                    src = PS[bank][:, q * 128:(q + 1) * 128]
                    if stage == "p1c" and q % 2 == 1:
                        continue
                    if stage == "p1d" and q % 2 == 0:
                        continue
                    if skip_main:
                        pass
                    elif (half == 0 if stage != "p1e" else q % 2 == 0):
                        S.op("act", lambda e, kc=kc, src=src: e.activation(out=dst_fn(kc), in_=src, func=AF.Identity,
                                                                         bias=bias_ap_fn(kc), scale=scale_ap_fn(kc)),
                             reads=[("PS", bank), "S1", "S2", ("MODT", 0), ("MODT", 3)], writes=dst_keys_fn(kc))
                    else:
                        S.op("dve", lambda e, kc=kc, src=src: e.tensor_scalar(out=dst_fn(kc), in0=src, scalar1=scale_ap_fn(kc),
                                                                            scalar2=bias_ap_fn(kc), op0=ALU.mult, op1=ALU.add),
                             reads=[("PS", bank), "S1", "S2", ("MODT", 0), ("MODT", 3)], writes=dst_keys_fn(kc))
                    if extra is not None:
                        extra(kc, src, bank)

        for tt in range(NT):
            xb = tt % 3
            S.dma("sp", lambda e, tt=tt, xb=xb: e.dma_start(out=XB[:, xb], in_=xin[tt * 128:(tt + 1) * 128, :]), "xb%d" % xb,
                  writes=[("XB", xb)])
            w = 1 if tt < 2 else 0
            norm_transpose(XB[:, xb], ("XB", xb), tt % 32,
                           lambda kc, w=w: S1[:, kc, w:w + 1], lambda kc, w=w: MODT[:, 0, kc, w:w + 1],
                           lambda kc, tt=tt: HT[:, kc, tt * 128:(tt + 1) * 128], lambda kc, tt=tt: [("HT", kc, tt)])

        if stage in ("p1", "p1a", "p1b", "p1c", "p1d", "p1e"):
            return finish()

        def ht_keys(t0, t1):
            return [("HT", kc, tt) for kc in range(8) for tt in range(t0 // 128, (t1 + 127) // 128)]

        S.barrier()
        RM = Region([(0, 140)])
        TMP = Region([(120, 136)]).alloc([128, 8, 512])
        RM = Region([(0, 120)])
        WG = RM.alloc([128, 1, 7, 8, 128], BF16)
        LW = RM.alloc([128, 1, 4, 128], BF16)
        U = RM.alloc([128, T])
        UB = RM.alloc([128, T], BF16)
        Y = RM.alloc([128, SEQ])
        GA = RM.alloc([128, SEQ], BF16)
        HC = RM.alloc([128, 2, CTX])
        HB = RM.alloc([128, 2, 512])
        TB = RM.alloc([128, 8, 512], BF16)
        YO = Region([(136, 140)]).alloc([128, 1, SEQ], BF16)
        QS = RM.alloc([128, SEQ], BF16)
        SGT = RM.alloc([128, SEQ], BF16)
        VT = RM.alloc([128, NT, 128], BF16)
        QM = RM.alloc([128, SEQ], BF16)
        KM = RM.alloc([128, SEQ], BF16)
        QG = RM.alloc([128, SEQ], BF16)
        KE = RM.alloc([128, T])
        KET = RM.alloc([128, 2, NT, 128], BF16)
        STMP = RM.alloc([128, 128])
        DEC = RM.alloc([128, 36])
        S32 = RM.alloc([128, 2, 128])
        S16 = RM.alloc([128, 32, 128], BF16)
        PT = RM.alloc([128, 3, 128], BF16)
        OF = RM.alloc([128, SEQ])

        tmp_i = [0]

        def tmp():
            i = tmp_i[0] % TMPn[0]
            tmp_i[0] += 1
            return TMP[:, i], ("TMP", i)

        tb_i = [0]

        def tbf():
            i = tb_i[0] % 8
            tb_i[0] += 1
            return TB[:, i], ("TB", i)

        pj_i = [0]

        def pj():
            i = pj_i[0] % 2
            pj_i[0] += 1
            return PS[i], ("PS", i)

        def proj(wbuf, slot, c):
            t0, t1 = chunk_range(c)
            n = t1 - t0
            ps, pk = pj()
            for kc in range(8):
                S.op("pe", lambda e, kc=kc, ps=ps: e.matmul(ps[:, 0:n], lhsT=WG[:, wbuf, slot, kc, :], rhs=HT[:, kc, t0:t1],
                                                          start=(kc == 0), stop=(kc == 7)),
                     reads=[("WG", wbuf)] + [("HT", kc, tt) for tt in range(t0 // 128, t1 // 128)], writes=[pk], sig=(kc == 7))
            return ps, pk, t0, t1, n

        ng = 8 if stage not in ("g1", "lru", "lruscan", "mix1", "h1", "h2", "h3", "h4", "h5") else 1
        for g in range(ng):
            wbuf = 0
            S.dma("pool", lambda e, g=g, wbuf=wbuf: e.dma_start(out=WG[:, wbuf], in_=win[g]), "wg%d" % wbuf, writes=[("WG", wbuf)])
            S.dma("pool", lambda e, g=g, wbuf=wbuf: e.dma_start(out=LW[:, wbuf], in_=lruw[:, g]), "lw%d" % wbuf, writes=[("LW", wbuf)])

            for c in range(5):
                ps, pk, t0, t1, n = proj(wbuf, 0, c)
                ax, axk = tmp()
                S.op("act", lambda e, ax=ax, ps=ps, n=n: e.copy(out=ax[:, 0:n], in_=ps[:, 0:n]), reads=[pk], writes=[axk])
                uk = ("U", c)
                S.op("dve", lambda e, ax=ax, t0=t0, t1=t1, n=n: e.tensor_scalar(
                    out=U[:, t0:t1], in0=ax[:, 0:n], scalar1=cpc("convw", 2 * 8 + g), scalar2=cpc("convb", g), op0=ALU.mult, op1=ALU.add),
                    reads=[axk, "CP"], writes=[uk])
                rw_ = 256 if c == 0 else 64
                for k, dlt in ((0, -2), (1, -1), (3, 1)):
                    lo, hi = max(0, -dlt), rw_ - max(0, dlt)
                    S.op("dve", lambda e, ax=ax, t0=t0, t1=t1, n=n, k=k, dlt=dlt, lo=lo, hi=hi, rw_=rw_: e.scalar_tensor_tensor(
                        out=U[:, t0:t1].rearrange("p (r c) -> p r c", c=rw_)[:, :, lo:hi],
                        in0=ax[:, 0:n].rearrange("p (r c) -> p r c", c=rw_)[:, :, lo + dlt:hi + dlt],
                        scalar=cpc("convw", k * 8 + g),
                        in1=U[:, t0:t1].rearrange("p (r c) -> p r c", c=rw_)[:, :, lo:hi], op0=ALU.mult, op1=ALU.add),
                        reads=[axk, uk, "CP"], writes=[uk])
                S.op("pool", lambda e, t0=t0, t1=t1: e.tensor_copy(out=UB[:, t0:t1], in_=U[:, t0:t1]), reads=[uk], writes=[("UB", c)])
                if c > 0:
                    ps, pk, t0, t1, n = proj(wbuf, 1, c)
                    l0 = t0 - CTX
                    ag, agk = tmp()
                    S.op("act", lambda e, ag=ag, ps=ps: e.copy(out=ag[:], in_=ps[:]), reads=[pk], writes=[agk])
                    z, zk = tmp()
                    S.op("pool", lambda e, ag=ag, z=z: e.tensor_tensor(out=z[:], in0=ag[:], in1=ag[:], op=ALU.mult), reads=[agk], writes=[zk])
                    S.op("pool", lambda e, z=z: e.tensor_scalar(out=z[:], in0=z[:], scalar1=0.044715, scalar2=1.0, op0=ALU.mult, op1=ALU.add),
                         reads=[zk], writes=[zk])
                    S.op("pool", lambda e, ag=ag, z=z: e.tensor_tensor(out=z[:], in0=z[:], in1=ag[:], op=ALU.mult), reads=[zk, agk], writes=[zk])
                    S.op("act", lambda e, z=z: e.activation(out=z[:], in_=z[:], func=AF.Sigmoid, scale=1.5957691216057308), reads=[zk], writes=[zk])
                    S.op("pool", lambda e, ag=ag, z=z, l0=l0: e.tensor_tensor(out=GA[:, l0:l0 + 512], in0=z[:], in1=ag[:], op=ALU.mult),
                         reads=[zk, agk], writes=[("GA", c)])
            for d in range(2):
                order = [0, 1, 2, 3, 4] if d == 0 else [0, 4, 3, 2, 1]
                prev_state = None
                def lstage_a(c):
                    t0, t1 = chunk_range(c)
                    n = t1 - t0
                    rps, rpk = pj()
                    S.op("pe", lambda e, rps=rps, t0=t0, t1=t1, n=n, d=d: e.matmul(rps[:, 0:n], lhsT=LW[:, wbuf, d * 2 + 0, :], rhs=UB[:, t0:t1],
                                                                                 start=True, stop=True),
                         reads=[("LW", wbuf), ("UB", c)], writes=[rpk])
                    ips, ipk = pj()
                    S.op("pe", lambda e, ips=ips, t0=t0, t1=t1, n=n, d=d: e.matmul(ips[:, 0:n], lhsT=LW[:, wbuf, d * 2 + 1, :], rhs=UB[:, t0:t1],
                                                                                 start=True, stop=True),
                         reads=[("LW", wbuf), ("UB", c)], writes=[ipk])
                    rt, rk = tmp()
                    it, ik = tmp()
                    S.op("act", lambda e, rt=rt, rps=rps, n=n, d=d: e.activation(out=rt[:, 0:n], in_=rps[:, 0:n], func=AF.Sigmoid,
                                                                               bias=cpc("ba", d * 8 + g), scale=1.0), reads=[rpk, "CP"], writes=[rk])
                    S.op("act", lambda e, it=it, ips=ips, n=n, d=d: e.activation(out=it[:, 0:n], in_=ips[:, 0:n], func=AF.Sigmoid,
                                                                               bias=cpc("bx", d * 8 + g), scale=1.0), reads=[ipk, "CP"], writes=[ik])
                    return (t0, t1, n, rt, rk, it, ik)

                pend_l = lstage_a(order[0])
                for oi, c in enumerate(order):
                    t0, t1, n, rt, rk, it, ik = pend_l
                    pend_l = lstage_a(order[oi + 1]) if oi + 1 < len(order) else None
                    a2, a2k = tmp()
                    S.op("act", lambda e, rt=rt, a2=a2, n=n, d=d: e.activation(out=a2[:, 0:n], in_=rt[:, 0:n], func=AF.Exp,
                                                                             scale=CL2[:, d * 8 + g:d * 8 + g + 1]), reads=[rk, "CL2"], writes=[a2k])
                    S.op("act", lambda e, rt=rt, n=n, d=d: e.activation(out=rt[:, 0:n], in_=rt[:, 0:n], func=AF.Exp,
                                                                      scale=CL[:, d * 8 + g:d * 8 + g + 1]), reads=[rk, "CL"], writes=[rk])
                    S.op("act", lambda e, a2=a2, n=n: e.activation(out=a2[:, 0:n], in_=a2[:, 0:n], func=AF.Sqrt, bias=1.0, scale=-1.0),
                         reads=[a2k], writes=[a2k])
                    S.op("pool", lambda e, it=it, t0=t0, t1=t1, n=n: e.tensor_tensor(out=it[:, 0:n], in0=it[:, 0:n], in1=U[:, t0:t1], op=ALU.mult),
                         reads=[ik, ("U", c)], writes=[ik])
                    S.op("pool", lambda e, it=it, a2=a2, n=n: e.tensor_tensor(out=it[:, 0:n], in0=it[:, 0:n], in1=a2[:, 0:n], op=ALU.mult),
                         reads=[ik, a2k], writes=[ik])
                    if d == 0:
                        if c == 0:
                            dst, dk = HC[:, 0, :], ("HC", 0)
                        else:
                            dst, dk = Y[:, t0 - CTX:t1 - CTX], ("Y", c)
                        init = 0.0 if prev_state is None else prev_state[0]
                        rd = [rk, ik] + ([] if prev_state is None else [prev_state[1]])
                        S.op("dve", lambda e, dst=dst, rt=rt, it=it, n=n, init=init: e.tensor_tensor_scan(
                            out=dst, data0=rt[:, 0:n], data1=it[:, 0:n], initial=init, op0=ALU.mult, op1=ALU.add), reads=rd, writes=[dk])
                        prev_state = (dst[:, n - 1:n], dk)
                    else:
                        if c == 0:
                            dst, dk = HC[:, 1, :], ("HC", 1)
                        else:
                            hb = oi % 2
                            dst, dk = HB[:, hb, :], ("HB", hb)
                        init = 0.0 if prev_state is None else prev_state[0]
                        rd = [rk, ik] + ([] if prev_state is None else [prev_state[1]])
                        S.op("dve", lambda e, dst=dst, rt=rt, it=it, n=n, init=init: e.tensor_tensor_scan(
                            out=dst[:, ::-1], data0=rt[:, 0:n][:, ::-1], data1=it[:, 0:n][:, ::-1], initial=init, op0=ALU.mult, op1=ALU.add),
                            reads=rd, writes=[dk])
                        prev_state = (dst[:, 0:1], dk)
                        if c > 0:
                            S.op("pool", lambda e, dst=dst, t0=t0, t1=t1: e.tensor_tensor(out=Y[:, t0 - CTX:t1 - CTX], in0=Y[:, t0 - CTX:t1 - CTX],
                                                                                        in1=dst, op=ALU.add), reads=[dk, ("Y", c)], writes=[("Y", c)])
            if stage == "lruscan":
                return finish()
            for c in range(1, 5):
                l0 = (c - 1) * 512
                S.op("dve", lambda e, l0=l0: e.tensor_tensor(out=YO[:, 0, l0:l0 + 512], in0=Y[:, l0:l0 + 512], in1=GA[:, l0:l0 + 512], op=ALU.mult),
                     reads=[("Y", c), ("GA", c)], writes=[("YO", 0)])
            S.dma("sp", lambda e, g=g: e.dma_start(out=ya_d[g], in_=YO[:, 0, :]), "yo0", reads=[("YO", 0)], writes=[("YAD", g)])

            if stage == "lru":
                return finish()
            for c in range(1, 5):
                ps, pk, t0, t1, n = proj(wbuf, 2, c)
                l0 = t0 - CTX
                S.op("act", lambda e, ps=ps, l0=l0: e.activation(out=QS[:, l0:l0 + 512], in_=ps[:], func=AF.Silu), reads=[pk], writes=[("QS", c)])
                ps, pk, t0, t1, n = proj(wbuf, 6, c)
                S.op("act", lambda e, ps=ps, l0=l0: e.activation(out=SGT[:, l0:l0 + 512], in_=ps[:], func=AF.Silu), reads=[pk], writes=[("SGT", c)])
            for t4 in range(0, NT, 4):
                nt4 = min(4, NT - t4)
                for q in range(nt4):
                    tt = t4 + q
                    for kc in range(8):
                        S.op("pe", lambda e, q=q, tt=tt, kc=kc: e.matmul(PS[2][:, q * 128:(q + 1) * 128], lhsT=HT[:, kc, tt * 128:(tt + 1) * 128],
                                                                      rhs=WG[:, wbuf, 5, kc, :], start=(kc == 0), stop=(kc == 7)),
                             reads=[("WG", wbuf), ("HT", kc, tt)], writes=[("PS", 2)], sig=(kc == 7))
                S.op("act", lambda e, t4=t4, nt4=nt4: e.copy(out=VT[:, t4:t4 + nt4, :], in_=PS[2][:, 0:nt4 * 128].rearrange("p (a b) -> p a b", b=128)),
                     reads=[("PS", 2)], writes=[("VT", t4 // 4)])
            if stage == "h1":
                return finish()
            for dd in range(2):
                mid = 31 if dd == 0 else 32
                end = 63 if dd == 0 else 0
                def stage_a(c):
                    ps, pk, t0, t1, n = proj(wbuf, 3 + dd, c)
                    nch = n // 64
                    t1_, k1 = tmp()
                    S.op("act", lambda e, t1_=t1_, ps=ps, n=n: e.activation(out=t1_[:, 0:n], in_=ps[:, 0:n], func=AF.Sigmoid), reads=[pk], writes=[k1])
                    kb, kbk = tbf()
                    S.op("pool", lambda e, kb=kb, t1_=t1_, n=n: e.tensor_scalar(out=kb[:, 0:n], in0=t1_[:, 0:n], scalar1=NOML[:, g:g + 1],
                                                                              scalar2=OML[:, g:g + 1], op0=ALU.mult, op1=ALU.add),
                         reads=[k1, "NOML", "OML"], writes=[kbk])
                    S.op("dve", lambda e, t1_=t1_, n=n: e.tensor_scalar(out=t1_[:, 0:n], in0=t1_[:, 0:n], scalar1=OML[:, g:g + 1],
                                                                      scalar2=LB[:, g:g + 1], op0=ALU.mult, op1=ALU.add),
                         reads=[k1, kbk, "OML", "LB"], writes=[k1])
                    return (t0, t1, n, nch, t1_, k1, kb, kbk)

                pend_a = stage_a(0)
                for c in range(5):
                    t0, t1, n, nch, t1_, k1, kb, kbk = pend_a
                    pend_a = stage_a(c + 1) if c + 1 < 5 else None
                    S.op("act", lambda e, t1_=t1_, n=n: e.activation(out=t1_[:, 0:n], in_=t1_[:, 0:n], func=AF.Ln), reads=[k1], writes=[k1])
                    gg, gk = tmp()
                    if dd == 0:
                        S.op("dve", lambda e, gg=gg, t1_=t1_, n=n: e.tensor_tensor_scan(out=gg[:, 0:n], data0=MASKF[:, 0:n], data1=t1_[:, 0:n],
                                                                                      initial=0.0, op0=ALU.mult, op1=ALU.add),
                             reads=[k1, "MASKF"], writes=[gk])
                    else:
                        S.op("dve", lambda e, gg=gg, t1_=t1_, n=n: e.tensor_tensor_scan(out=gg[:, 0:n][:, ::-1], data0=MASKF[:, 0:n],
                                                                                      data1=t1_[:, 0:n][:, ::-1], initial=0.0, op0=ALU.mult, op1=ALU.add),
                             reads=[k1, "MASKF"], writes=[gk])
                    g3 = gg[:, 0:n].rearrange("p (a b) -> p a b", b=64)
                    d1, d1k = tmp()
                    d13 = d1[:, 0:n].rearrange("p (a b) -> p a b", b=64)
                    if c > 0:
                        l0 = t0 - CTX
                        S.op("dve", lambda e, d13=d13, g3=g3, nch=nch: e.tensor_tensor(out=d13, in0=g3, in1=g3[:, :, mid:mid + 1].to_broadcast([128, nch, 64]),
                                                                                     op=ALU.subtract), reads=[gk], writes=[d1k])
                        e1, e1k = tbf()
                        S.op("act", lambda e, e1=e1, d1=d1: e.activation(out=e1[:], in_=d1[:], func=AF.Exp), reads=[d1k], writes=[e1k])
                        S.op("pool", lambda e, e1=e1, l0=l0: e.tensor_tensor(out=QM[:, l0:l0 + 512], in0=QS[:, l0:l0 + 512], in1=e1[:], op=ALU.mult),
                             reads=[e1k, ("QS", c)], writes=[("QM", c)])
                        e2, e2k = tbf()
                        S.op("act", lambda e, e2=e2, d1=d1: e.activation(out=e2[:], in_=d1[:], func=AF.Exp, scale=-1.0), reads=[d1k], writes=[e2k])
                        S.op("pool", lambda e, e2=e2, kb=kb, l0=l0: e.tensor_tensor(out=KM[:, l0:l0 + 512], in0=kb[:], in1=e2[:], op=ALU.mult),
                             reads=[e2k, kbk], writes=[("KM", c)])
                        e4, e4k = tbf()
                        S.op("act", lambda e, e4=e4, gg=gg: e.activation(out=e4[:], in_=gg[:], func=AF.Exp), reads=[gk], writes=[e4k])
                        S.op("pool", lambda e, e4=e4, l0=l0: e.tensor_tensor(out=QG[:, l0:l0 + 512], in0=QS[:, l0:l0 + 512], in1=e4[:], op=ALU.mult),
                             reads=[e4k, ("QS", c)], writes=[("QG", c)])
                    d3, d3k = tmp()
                    d33 = d3[:, 0:n].rearrange("p (a b) -> p a b", b=64)
                    S.op("dve", lambda e, d33=d33, g3=g3, nch=nch: e.tensor_tensor(out=d33, in0=g3, in1=g3[:, :, end:end + 1].to_broadcast([128, nch, 64]),
                                                                                 op=ALU.subtract), reads=[gk], writes=[d3k])
                    S.op("act", lambda e, d3=d3, n=n: e.activation(out=d3[:, 0:n], in_=d3[:, 0:n], func=AF.Exp, scale=-1.0), reads=[d3k], writes=[d3k])
                    S.op("pool", lambda e, d3=d3, kb=kb, t0=t0, t1=t1, n=n: e.tensor_tensor(out=KE[:, t0:t1], in0=kb[:, 0:n], in1=d3[:, 0:n], op=ALU.mult),
                         reads=[d3k, kbk], writes=[("KE", c)])
                    ch0 = t0 // 64
                    S.op("act", lambda e, g3=g3, ch0=ch0, nch=nch: e.activation(out=DEC[:, ch0:ch0 + nch].unsqueeze(2), in_=g3[:, :, end:end + 1], func=AF.Exp),
                         reads=[gk], writes=[("DEC", c)])
                if stage == "h2":
                    return finish()
                for t4 in range(0, NT, 4):
                    nt4 = min(4, NT - t4)
                    for q in range(nt4):
                        tt = t4 + q
                        cc = 0 if tt < 2 else 1 + (tt - 2) // 4
                        S.op("pe", lambda e, q=q, tt=tt: e.transpose(out=PS[2][:, q * 128:(q + 1) * 128], in_=KE[:, tt * 128:(tt + 1) * 128], identity=ID32[:]),
                             reads=[("KE", cc), "ID32"], writes=[("PS", 2)], sig=(q == nt4 - 1))
                    for hh in range(2):
                        S.op("dve", lambda e, t4=t4, nt4=nt4, hh=hh: e.tensor_scalar(
                            out=KET[:, hh, t4:t4 + nt4, :], in0=PS[2][:, 0:nt4 * 128].rearrange("p (a b) -> p a b", b=128),
                            scalar1=HMASK[:, hh:hh + 1], scalar2=None, op0=ALU.mult),
                            reads=[("PS", 2), "HMASK"], writes=[("KET", hh, t4 // 4)])
                if stage == "h3":
                    return finish()
                order = list(range(36)) if dd == 0 else [3, 2, 1, 0] + list(range(35, 3, -1))
                S.op("pool", lambda e: e.memset(S32[:, 0, :], 0.0), writes=[("S32", 0)])
                for i, nchk in enumerate(order):
                    par = i % 2
                    tt, half = nchk // 2, nchk % 2
                    if nchk >= 4:
                        S.op("act", lambda e, par=par, nchk=nchk: e.copy(out=S16[:, nchk - 4, :], in_=S32[:, par, :]),
                             reads=[("S32", par)], writes=[("S16", nchk - 4)])
                    if i == len(order) - 1:
                        break
                    reg = i % 4
                    cc = 0 if tt < 2 else 1 + (tt - 2) // 4
                    S.op("pe", lambda e, reg=reg, tt=tt, half=half: e.matmul(PS[3][:, reg * 128:(reg + 1) * 128],
                                                                           lhsT=KET[:, half, tt, :], rhs=VT[:, tt, :],
                                                                           start=True, stop=True),
                         reads=[("KET", half, tt // 4), ("VT", tt // 4)], writes=[("PS", 3, reg)])
                    S.op("dve", lambda e, par=par, nchk=nchk: e.tensor_scalar(out=STMP[:], in0=S32[:, par, :], scalar1=DEC[:, nchk:nchk + 1],
                                                                            scalar2=None, op0=ALU.mult),
                         reads=[("S32", par), ("DEC", cc)], writes=["STMP"])
                    S.op("dve", lambda e, reg=reg, par=par: e.tensor_tensor(out=S32[:, 1 - par, :], in0=PS[3][:, reg * 128:(reg + 1) * 128], in1=STMP[:],
                                                                          op=ALU.add),
                         reads=["STMP", ("PS", 3, reg)], writes=[("S32", 1 - par)])
                if stage == "h4":
                    return finish()
                for lt in range(16):
                    tt = lt + 2
                    c = 1 + lt // 4
                    reg = lt % 4
                    bank = 4 + lt // 4
                    S.op("pe", lambda e, reg=reg, lt=lt: e.matmul(PS[3][:, reg * 128:(reg + 1) * 128], lhsT=KM[:, lt * 128:(lt + 1) * 128],
                                                                rhs=QM[:, lt * 128:(lt + 1) * 128], start=True, stop=True),
                         reads=[("KM", c), ("QM", c)], writes=[("PS", 3, reg)])
                    pi = lt % 3
                    S.op("dve", lambda e, reg=reg, pi=pi, dd=dd: e.tensor_tensor(out=PT[:, pi, :], in0=PS[3][:, reg * 128:(reg + 1) * 128], in1=TRI[:, dd, :],
                                                                               op=ALU.mult), reads=[("PS", 3, reg), "TRI"], writes=[("PT", pi)])
                    ot = PS[bank][:, reg * 128:(reg + 1) * 128]
                    otk = ("PS", bank, reg)
                    S.op("pe", lambda e, ot=ot, tt=tt, pi=pi, dd=dd: e.matmul(ot, lhsT=VT[:, tt, :], rhs=PT[:, pi, :], start=True, stop=False,
                                                                            skip_group_check=True),
                         reads=[("VT", tt // 4), ("PT", pi)], writes=[otk], sig=False)
                    nA, nB = 2 * tt - 4, 2 * tt - 3
                    S.op("pe", lambda e, ot=ot, nA=nA, lt=lt: e.matmul(ot[:, 0:64], lhsT=S16[:, nA, :], rhs=QG[:, lt * 128:lt * 128 + 64], start=False, stop=False,
                                                                     skip_group_check=True),
                         reads=[("S16", nA), ("QG", c)], writes=[otk], sig=False)
                    S.op("pe", lambda e, ot=ot, nB=nB, lt=lt, dd=dd: e.matmul(ot[:, 64:128], lhsT=S16[:, nB, :], rhs=QG[:, lt * 128 + 64:lt * 128 + 128],
                                                                            start=False, stop=True, skip_group_check=True),
                         reads=[("S16", nB), ("QG", c)], writes=[otk])
                if dd == 0:
                    for bq in range(4):
                        S.op("act", lambda e, bq=bq: e.copy(out=OF[:, bq * 512:(bq + 1) * 512], in_=PS[4 + bq][:]),
                             reads=[("PS", 4 + bq, r) for r in range(4)], writes=[("OF", bq)])
            if stage == "h5":
                return finish()
            for bq in range(4):
                bank = 4 + bq
                bkeys = [("PS", bank, r) for r in range(4)]
                osum, osumk = tmp()
                S.op("dve", lambda e, osum=osum, bank=bank, bq=bq: e.tensor_tensor(out=osum[:], in0=PS[bank][:], in1=OF[:, bq * 512:(bq + 1) * 512], op=ALU.add),
                     reads=bkeys + [("OF", bq)], writes=[osumk])
                osq, osqk = tmp()
                S.op("act", lambda e, osq=osq, osum=osum: e.activation(out=osq[:], in_=osum[:], func=AF.Square), reads=[osumk], writes=[osqk])
                ps, pk = pj()
                S.op("pe", lambda e, ps=ps, osq=osq: e.matmul(ps[:], lhsT=ONESM[:], rhs=osq[:], start=True, stop=True), reads=["ONESM", osqk], writes=[pk])
                rs, rsk = tmp()
                S.op("act", lambda e, rs=rs, ps=ps: e.activation(out=rs[:], in_=ps[:], func=AF.Ln, bias=EPS, scale=1.0), reads=[pk], writes=[rsk])
                S.op("act", lambda e, rs=rs: e.activation(out=rs[:], in_=rs[:], func=AF.Exp, scale=-0.5), reads=[rsk], writes=[rsk])
                S.op("dve", lambda e, rs=rs, osum=osum: e.tensor_tensor(out=rs[:], in0=osum[:], in1=rs[:], op=ALU.mult), reads=[osumk, rsk], writes=[rsk])
                S.op("dve", lambda e, rs=rs, bq=bq: e.scalar_tensor_tensor(out=YO[:, 0, bq * 512:(bq + 1) * 512], in0=rs[:], scalar=cpc("hgng"),
                                                                         in1=SGT[:, bq * 512:(bq + 1) * 512], op0=ALU.mult, op1=ALU.mult),
                     reads=[rsk, ("SGT", bq + 1), "CP"], writes=[("YO", 0)])
            S.dma("sp", lambda e, g=g: e.dma_start(out=yb_d[g], in_=YO[:, 0, :]), "yo0", reads=[("YO", 0)], writes=[("YBD", g)])

        if stage == "mix1":
            return finish()
        S.barrier()
        RG = Region([(0, 64), (96, 104), (120, 140)])
        TMP = Region([(120, 128)]).alloc([128, 4, 512])
        TMPn[0] = 4
        RG = Region([(0, 64), (96, 104), (128, 140)])
        WBA = RG.alloc([128, 8, 1024], BF16)
        WBB = RG.alloc([128, 8, 1024], BF16)
        WOUT = RG.alloc([128, 8, 1024], BF16)
        YAC = RG.alloc([128, 8, 512], BF16)
        YBC = RG.alloc([128, 8, 512], BF16)
        MIX = RG.alloc([128, 8, 512], BF16)
        H2F = RG.alloc([128, 8, 128])
        W78ALL = Region([(64, 96)]).alloc([128, 8, 2, 8, 128], BF16)
        LG = RG.alloc([128, 32])
        MX8 = RG.alloc([128, 8])
        S.dma("pool", lambda e: e.dma_start(out=WBA[:], in_=wba), "wm0", writes=["WBA"])
        S.dma("pool", lambda e: e.dma_start(out=WBB[:], in_=wbb), "wm1", writes=["WBB"])
        S.dma("pool", lambda e: e.dma_start(out=WOUT[:], in_=wout), "wm2", writes=["WOUT"])
        for hf in range(2):
            S.dma("pool", lambda e, hf=hf: e.dma_start(out=W78ALL[:, hf * 4:(hf + 1) * 4], in_=win78[hf * 4:(hf + 1) * 4].rearrange("m p s k j -> p m s k j")),
                  "w78%d" % hf, writes=[("W78ALL", hf)])
        yad_keys = [("YAD", g) for g in range(ng)]
        ybd_keys = [("YBD", g) for g in range(ng)]
        for tc in range(4):
            S.dma("sp", lambda e, tc=tc: e.dma_start(out=YAC[:], in_=ya_d[:, :, tc * 512:(tc + 1) * 512].rearrange("g p t -> p g t")), "yac",
                  reads=yad_keys, writes=["YAC"])
            S.dma("sp", lambda e, tc=tc: e.dma_start(out=YBC[:], in_=yb_d[:, :, tc * 512:(tc + 1) * 512].rearrange("g p t -> p g t")), "ybc",
                  reads=ybd_keys, writes=["YBC"])
            t0 = CTX + tc * 512
            for mc in range(8):
                for kc in range(8):
                    S.op("pe", lambda e, kc=kc, mc=mc: e.matmul(PS[0][:], lhsT=WBA[:, kc, mc * 128:(mc + 1) * 128], rhs=YAC[:, kc, :], start=(kc == 0), stop=(kc == 7)),
                         reads=["WBA", "YAC"], writes=[("PS", 0)], sig=(kc == 7))
                for kc in range(8):
                    S.op("pe", lambda e, kc=kc, mc=mc: e.matmul(PS[1][:], lhsT=WBB[:, kc, mc * 128:(mc + 1) * 128], rhs=YBC[:, kc, :], start=(kc == 0), stop=(kc == 7)),
                         reads=["WBB", "YBC"], writes=[("PS", 1)], sig=(kc == 7))
                for which in range(2):
                    for kc in range(8):
                        S.op("pe", lambda e, kc=kc, mc=mc, which=which: e.matmul(PS[4 + which][:], lhsT=W78ALL[:, mc, which, kc, :], rhs=HT[:, kc, t0:t0 + 512],
                                                                              start=(kc == 0), stop=(kc == 7)),
                             reads=[("W78ALL", mc // 4)] + [("HT", kc, tt) for tt in range(t0 // 128, t0 // 128 + 4)], writes=[("PS", 4 + which)], sig=(kc == 7))
                sa, sak = tmp()
                sbb, sbk = tmp()
                S.op("act", lambda e, sa=sa: e.activation(out=sa[:], in_=PS[4][:], func=AF.Sigmoid), reads=[("PS", 4)], writes=[sak])
                S.op("act", lambda e, sbb=sbb: e.activation(out=sbb[:], in_=PS[5][:], func=AF.Sigmoid), reads=[("PS", 5)], writes=[sbk])
                S.op("dve", lambda e, sa=sa: e.tensor_tensor(out=sa[:], in0=PS[0][:], in1=sa[:], op=ALU.mult), reads=[("PS", 0), sak], writes=[sak])
                S.op("dve", lambda e, sbb=sbb: e.tensor_tensor(out=sbb[:], in0=PS[1][:], in1=sbb[:], op=ALU.mult), reads=[("PS", 1), sbk], writes=[sbk])
                S.op("pool", lambda e, sa=sa, sbb=sbb, mc=mc: e.tensor_tensor(out=MIX[:, mc, :], in0=sa[:], in1=sbb[:], op=ALU.add),
                     reads=[sak, sbk], writes=[("MIX", mc)])
            for l4 in range(4):
                lt = tc * 4 + l4
                xb = lt % 3
                S.dma("sp", lambda e, lt=lt, xb=xb: e.dma_start(out=XB[:, xb], in_=xin[CTX + lt * 128:CTX + (lt + 1) * 128, :]), "xb%d" % xb,
                      writes=[("XB", xb)])
                for nh in range(2):
                    bank = 6 + nh
                    for mc in range(8):
                        S.op("pe", lambda e, mc=mc, nh=nh, l4=l4, bank=bank: e.matmul(PS[bank][:], lhsT=MIX[:, mc, l4 * 128:(l4 + 1) * 128],
                                                                                   rhs=WOUT[:, mc, nh * 512:(nh + 1) * 512], start=(mc == 0), stop=(mc == 7)),
                             reads=["WOUT", ("MIX", mc)], writes=[("PS", bank)], sig=(mc == 7))
                    mo, mok = tmp()
                    S.op("dve", lambda e, mo=mo, bank=bank, nh=nh: e.tensor_tensor(out=mo[:], in0=PS[bank][:], in1=MODB[:, 0, nh * 512:(nh + 1) * 512], op=ALU.mult),
                         reads=[("PS", bank), ("MODB", 0, nh)], writes=[mok])
                    S.op("pool", lambda e, mo=mo, xb=xb, nh=nh: e.tensor_tensor(out=XB[:, xb, nh * 512:(nh + 1) * 512], in0=XB[:, xb, nh * 512:(nh + 1) * 512],
                                                                              in1=mo[:], op=ALU.add), reads=[mok, ("XB", xb)], writes=[("XB", xb)])
                S.dma("sp", lambda e, lt=lt, xb=xb: e.dma_start(out=x1_d[lt * 128:(lt + 1) * 128, :], in_=XB[:, xb]), "xb%d" % xb,
                      reads=[("XB", xb)], writes=[("X1D", lt)])

                def extra(kc, src, bank):
                    if bank == 2:
                        S.op("act", lambda e, kc=kc, src=src: e.activation(out=H2F[:, kc, :], in_=src, func=AF.Identity,
                                                                         bias=MODT[:, 3, kc, 0:1], scale=S2[:, kc:kc + 1]),
                             reads=[("PS", bank), "S2", ("MODT", 3)], writes=[("H2F", kc)])
                    else:
                        S.op("dve", lambda e, kc=kc, src=src: e.tensor_scalar(out=H2F[:, kc, :], in0=src, scalar1=S2[:, kc:kc + 1],
                                                                            scalar2=MODT[:, 3, kc, 0:1], op0=ALU.mult, op1=ALU.add),
                             reads=[("PS", bank), "S2", ("MODT", 3)], writes=[("H2F", kc)])
                norm_transpose(XB[:, xb], ("XB", xb), 32 + lt,
                               lambda kc: S2[:, kc:kc + 1], lambda kc: MODT[:, 3, kc, 0:1],
                               lambda kc, lt=lt: HT2[:, kc, lt * 128:(lt + 1) * 128], lambda kc, lt=lt: [("HT2", kc, lt)], extra=extra,
                               after_norm=lambda lt=lt, xb=xb: S.dma("sp", lambda e: e.dma_start(out=xn2_d[lt * 128:(lt + 1) * 128, :], in_=XB[:, xb]),
                                                                     "xb%d" % xb, reads=[("XB", xb)], writes=[("XN2D", lt)]), skip_main=True)
                for kc in range(8):
                    S.op("pe", lambda e, kc=kc: e.matmul(PS[3][:, 0:32], lhsT=H2F[:, kc, :], rhs=RWS[:, kc, :], start=(kc == 0), stop=False),
                         reads=[("H2F", kc), "RWS"], writes=[("PS", 3)], sig=False)
                S.op("pe", lambda e: e.matmul(PS[3][:, 0:32], lhsT=ONESROW[0:1, :], rhs=RBROW[0:1, :], start=False, stop=True),
                     reads=["ONESROW", "RBROW"], writes=[("PS", 3)])
                S.op("act", lambda e: e.copy(out=LG[:], in_=PS[3][:, 0:32]), reads=[("PS", 3)], writes=["LG"])
                S.op("dve", lambda e: e.max(out=MX8[:], in_=LG[:]), reads=["LG"], writes=["MX8"])
                gt = GATES[:, lt, :]
                gk_ = ("GATES", lt)
                S.op("dve", lambda e, gt=gt: e.tensor_scalar(out=gt, in0=LG[:], scalar1=MX8[:, 3:4], scalar2=None, op0=ALU.is_ge), reads=["LG", "MX8"], writes=[gk_])
                S.op("dve", lambda e: e.tensor_scalar(out=LG[:], in0=LG[:], scalar1=MX8[:, 0:1], scalar2=None, op0=ALU.subtract), reads=["LG", "MX8", gk_], writes=["LG"])
                S.op("act", lambda e: e.activation(out=LG[:], in_=LG[:], func=AF.Exp), reads=["LG"], writes=["LG"])
                S.op("dve", lambda e, gt=gt: e.tensor_tensor(out=gt, in0=gt, in1=LG[:], op=ALU.mult), reads=["LG", gk_], writes=[gk_])
                S.op("dve", lambda e, gt=gt: e.reduce_sum(out=MX8[:, 7:8], in_=gt, axis=AXL.X), reads=[gk_, "MX8"], writes=["MX8"])
                S.op("dve", lambda e: e.reciprocal(out=MX8[:, 7:8], in_=MX8[:, 7:8]), reads=["MX8"], writes=["MX8"])
                S.op("dve", lambda e, gt=gt: e.tensor_scalar(out=gt, in0=gt, scalar1=MX8[:, 7:8], scalar2=None, op0=ALU.mult), reads=[gk_, "MX8"], writes=[gk_])

        S.barrier()
        RE = Region([(0, 176)])
        W1Gs = RE.alloc([128, 2, 8192], BF16)
        W1Us = RE.alloc([128, 2, 8192], BF16)
        W2s = RE.alloc([128, 2, 8192], BF16)
        XG = RE.alloc([128, 2, 2, 1024])
        HG = RE.alloc([128, 2, 8, PSZ], BF16)
        ACTT = RE.alloc([128, 8, PSZ], BF16)
        YS = RE.alloc([128, 2, 1024])
        TMP = RE.alloc([128, 6, 512])
        TMPn[0] = 6
        B1S = RE.alloc([128, 2, 2, 8])
        SLTF = RE.alloc([128, 128])
        SLTI = RE.alloc([128, 128], I32)
        TABT = RE.alloc([128, 256])
        IC = RE.alloc([128, 256])
        MSK = RE.alloc([128, 16, 32])
        CNT = RE.alloc([128, 32])
        PADD = RE.alloc([128, 32])
        PEND = RE.alloc([128, 32])
        RUNB = RE.alloc([128, 32])
        DEST = RE.alloc([128, 32])
        KEY = RE.alloc([128, 32])
        OH = RE.alloc([128, 32])
        MX = RE.alloc([128, 8])
        EK4 = RE.alloc([128, 4])
        DK = RE.alloc([128, 64])
        DKI = RE.alloc([128, 64], I32)
        CMP = RE.alloc([128, NPASS, 32])
        EP = RE.alloc([128, NPASS])
        SK = RE.alloc([128, NPASS])
        IDXWF = RE.alloc([128, NPASS])
        IDXWI = RE.alloc([128, NPASS], I32)
        GIDXF = RE.alloc([128, 2, 2])
        GIDXI = RE.alloc([128, 2, 2], I32)
        GT = RE.alloc([128, 2, 2, 2])
        ONES1 = RE.alloc([128, 128])
        SLTM = RE.alloc([128, 128])
        OOBT = RE.alloc([128, 256])
        GTS = RE.alloc([32, 128])
        B2S = RE.alloc([32, 1024])
        ACI = RE.alloc([128, 2, 1024])
        IOTAE = IC[:, 0:32]
        W64 = IC[:, 32:64]
        THR = IC[:, 64:128]
        PIDX = IC[:, 128:129]
        TOKF = IC[:, 129:145]
        ONE32 = IC[:, 145:177]
        TOK2 = IC[:, 177:209].rearrange("p (l two) -> p l two", two=2)
        G2 = XG[:, 0, 0, :].rearrange("p (l e d) -> p l e d", e=NE, d=2)
        S.dma("sp", lambda e: e.dma_start(out=B2S[:], in_=b2), "c6", writes=["B2S"])
        S.dma("sp", lambda e: e.dma_start(out=IC[:], in_=iconst), "c7", writes=["IC"])
        S.op("pool", lambda e: e.memset(ONES1[:], 1.0), writes=["ONES1"])
        S.op("pool", lambda e: e.memset(SLTM[:], 1.0), writes=["SLTM"])
        S.op("pool", lambda e: e.affine_select(out=SLTM[:], in_=SLTM[:], pattern=[[1, 128]], compare_op=ALU.is_gt, fill=0.0, base=0, channel_multiplier=-1),
             reads=["SLTM"], writes=["SLTM"])
        S.op("pool", lambda e: e.memset(OOBT[:], 1.0e6), writes=["OOBT"])
        S.dma("sp", lambda e: e.dma_start(out=slot_d.rearrange("(q p) o -> q (p o)", p=128), in_=OOBT[:]), "c8", reads=["OOBT"], writes=["SLOTD"])
        S.op("dve", lambda e: e.tensor_copy(out=G2, in_=GATES[:].unsqueeze(3).to_broadcast([128, 16, NE, 2])), reads=[("GATES", lt) for lt in range(16)],
             writes=[("XG", 0), ("XG", 1)])
        S.dma("sp", lambda e: e.dma_start(out=gates_d.rearrange("(l p e) o -> p l (e o)", p=128, e=NE), in_=G2.rearrange("p l e d -> p l (e d)")), "c9",
              reads=[("XG", 0), ("XG", 1)], writes=["GATESD"])
        S.op("pool", lambda e: e.memset(XG[:], 0.0), writes=[("XG", 0), ("XG", 1)])
        for lt in range(16):
            S.op("pe", lambda e, lt=lt: e.transpose(out=PS[3][0:32, 0:128], in_=GATES[:, lt, :], identity=ID32[:]), reads=[("GATES", lt), "ID32"], writes=[("PS", 3)])
            S.op("act", lambda e: e.copy(out=GTS[:], in_=PS[3][0:32, 0:128]), reads=[("PS", 3)], writes=["GTS"])
            ab = lt % 2
            for nh in range(2):
                S.op("pe", lambda e, nh=nh: e.matmul(PS[nh][:], lhsT=GTS[:], rhs=B2S[:, nh * 512:(nh + 1) * 512], start=True, stop=True),
                     reads=["GTS", "B2S"], writes=[("PS", nh)])
                S.op("act", lambda e, nh=nh, ab=ab: e.copy(out=ACI[:, ab, nh * 512:(nh + 1) * 512], in_=PS[nh][:]), reads=[("PS", nh)], writes=[("ACI", ab)])
            S.dma("sp", lambda e, lt=lt, ab=ab: e.dma_start(out=acc_d[lt * 128:(lt + 1) * 128, :], in_=ACI[:, ab]), "aci%d" % ab, reads=[("ACI", ab)], writes=["ACCD"])
        gk_all = [("GATES", lt) for lt in range(16)]
        S.op("dve", lambda e: e.tensor_scalar(out=MSK[:], in0=GATES[:], scalar1=0.0, scalar2=None, op0=ALU.is_gt), reads=gk_all, writes=["MSK"])
        for lt in range(16):
            S.op("pe", lambda e, lt=lt: e.matmul(PS[2][:, 0:32], lhsT=ONES1[:], rhs=MSK[:, lt, :], start=(lt == 0), stop=(lt == 15)),
                 reads=["ONES1", "MSK"], writes=[("PS", 2)], sig=(lt == 15))
        S.op("dve", lambda e: e.tensor_copy(out=CNT[:], in_=PS[2][:, 0:32]), reads=[("PS", 2)], writes=["CNT"])
        S.op("dve", lambda e: e.tensor_scalar(out=PADD[:], in0=CNT[:], scalar1=0.0, scalar2=None, op0=ALU.is_gt), reads=["CNT"], writes=["PADD"])
        for j in range(1, 8):
            S.op("dve", lambda e, j=j: e.scalar_tensor_tensor(out=PADD[:], in0=CNT[:], scalar=float(PSZ * j), in1=PADD[:], op0=ALU.is_gt, op1=ALU.add),
                 reads=["CNT", "PADD"], writes=["PADD"])
        S.op("dve", lambda e: e.tensor_scalar(out=PADD[:], in0=PADD[:], scalar1=float(PSZ), scalar2=None, op0=ALU.mult), reads=["PADD"], writes=["PADD"])
        S.op("dve", lambda e: e.tensor_tensor_scan(out=PEND[:], data0=ONE32, data1=PADD[:], initial=0.0, op0=ALU.mult, op1=ALU.add),
             reads=["PADD", "IC"], writes=["PEND"])
        S.op("dve", lambda e: e.tensor_tensor(out=RUNB[:], in0=PEND[:], in1=PADD[:], op=ALU.subtract), reads=["PEND", "PADD"], writes=["RUNB"])
        S.op("dve", lambda e: e.tensor_tensor(out=CMP[:], in0=PEND[:].unsqueeze(1).to_broadcast([128, NPASS, 32]),
                                            in1=THR.unsqueeze(2).to_broadcast([128, NPASS, 32]), op=ALU.is_le), reads=["PEND", "IC"], writes=["CMP"])
        S.op("dve", lambda e: e.reduce_sum(out=EP[:], in_=CMP[:], axis=AXL.X), reads=["CMP"], writes=["EP"])
        S.op("dve", lambda e: e.tensor_scalar(out=EP[:], in0=EP[:], scalar1=31.0, scalar2=None, op0=ALU.min), reads=["EP"], writes=["EP"])
        S.op("dve", lambda e: e.tensor_scalar(out=IDXWF[:], in0=EP[:], scalar1=128.0, scalar2=PIDX, op0=ALU.mult, op1=ALU.add), reads=["EP", "IC"], writes=["IDXWF"])
        S.op("dve", lambda e: e.tensor_tensor(out=SK[:, 2:NPASS], in0=EP[:, 2:NPASS], in1=EP[:, 0:NPASS - 2], op=ALU.is_equal), reads=["EP"], writes=["SK"])
        S.op("dve", lambda e: e.scalar_tensor_tensor(out=IDXWF[:, 2:NPASS], in0=SK[:, 2:NPASS], scalar=1.0e7, in1=IDXWF[:, 2:NPASS], op0=ALU.mult, op1=ALU.add),
             reads=["SK", "IDXWF"], writes=["IDXWF"])
        S.op("dve", lambda e: e.tensor_copy(out=IDXWI[:], in_=IDXWF[:]), reads=["IDXWF"], writes=["IDXWI"])
        pre_w = set()

        def issue_weights(p):
            sl = p % 2
            idx = IDXWI[:, p:p + 1]
            for nm, dst, srcw in (("w1g", W1Gs, w1g), ("w1u", W1Us, w1u), ("w2", W2s, w2)):
                S.dma("pool", lambda e, dst=dst, srcw=srcw, sl=sl, idx=idx: e.indirect_dma_start(
                    out=dst[:, sl, :], out_offset=None, in_=srcw,
                    in_offset=bass.IndirectOffsetOnAxis(ap=idx, axis=0), bounds_check=bnd(e, NE * 128 - 1), oob_is_err=False),
                    "%s%d" % (nm, sl), reads=["IDXWI", (nm, sl)], writes=[(nm, sl)])
            for gi, srcb in ((0, b1gt), (1, b1ut)):
                S.dma("pool", lambda e, gi=gi, srcb=srcb, sl=sl, idx=idx: e.indirect_dma_start(
                    out=B1S[:, sl, gi, :], out_offset=None, in_=srcb, in_offset=bass.IndirectOffsetOnAxis(ap=idx, axis=0),
                    bounds_check=bnd(e, NE * 128 - 1), oob_is_err=False), "b1s%d" % sl, reads=["IDXWI", ("B1S", sl)], writes=[("B1S", sl)])

        if stage == "full":
            for p_ in (0, 1):
                issue_weights(p_)
                pre_w.add(p_)
        for lt in range(16):
            S.op("pe", lambda e, lt=lt: e.matmul(PS[0][:, 0:32], lhsT=SLTM[:], rhs=MSK[:, lt, :], start=True, stop=True), reads=["SLTM", "MSK"], writes=[("PS", 0)])
            S.op("pe", lambda e, lt=lt: e.matmul(PS[1][:, 0:32], lhsT=ONES1[:], rhs=MSK[:, lt, :], start=True, stop=True), reads=["ONES1", "MSK"], writes=[("PS", 1)])
            S.op("dve", lambda e: e.tensor_tensor(out=DEST[:], in0=PS[0][:, 0:32], in1=RUNB[:], op=ALU.add), reads=[("PS", 0), "RUNB"], writes=["DEST"])
            S.op("dve", lambda e: e.tensor_tensor(out=RUNB[:], in0=PS[1][:, 0:32], in1=RUNB[:], op=ALU.add), reads=[("PS", 1), "RUNB", "DEST"], writes=["RUNB"])
            S.op("dve", lambda e, lt=lt: e.tensor_tensor(out=KEY[:], in0=MSK[:, lt, :], in1=W64, op=ALU.mult), reads=["MSK", "IC"], writes=["KEY"])
            S.op("dve", lambda e: e.max(out=MX[:], in_=KEY[:]), reads=["KEY"], writes=["MX"])
            S.op("dve", lambda e: e.tensor_scalar(out=EK4[:], in0=MX[:, 0:4], scalar1=-1.0, scalar2=64.0, op0=ALU.mult, op1=ALU.add), reads=["MX"], writes=["EK4"])
            for k in range(4):
                S.op("dve", lambda e, k=k: e.tensor_scalar(out=OH[:], in0=IOTAE, scalar1=EK4[:, k:k + 1], scalar2=None, op0=ALU.is_equal), reads=["EK4", "IC"], writes=["OH"])
                S.op("dve", lambda e: e.tensor_tensor(out=OH[:], in0=OH[:], in1=DEST[:], op=ALU.mult), reads=["OH", "DEST"], writes=["OH"])
                S.op("dve", lambda e, lt=lt, k=k: e.reduce_sum(out=DK[:, lt * 4 + k:lt * 4 + k + 1], in_=OH[:], axis=AXL.X), reads=["OH"], writes=[("DK", lt)])
        S.op("dve", lambda e: e.tensor_copy(out=DKI[:], in_=DK[:]), reads=[("DK", lt) for lt in range(16)], writes=["DKI"])
        for i in range(64):
            S.dma("pool", lambda e, i=i: e.indirect_dma_start(out=slot_d, out_offset=bass.IndirectOffsetOnAxis(ap=DKI[:, i:i + 1], axis=0),
                                                             in_=TOK2[:, i // 4, :], in_offset=None, bounds_check=bnd(e, NSLOT - 1), oob_is_err=False),
                  "sct", reads=["DKI", "IC", "SLOTD"], writes=[("SLOTS", i)])
        S.dma("sp", lambda e: e.dma_start(out=TABT[:], in_=slot_d.rearrange("(q p) o -> q (p o)", p=128)), "c8",
              reads=[("SLOTS", i) for i in range(64)], writes=["TABT"])
        S.op("pe", lambda e: e.transpose(out=PS[2][:, 0:128], in_=TABT[:].rearrange("p (a two) -> p a two", two=2)[:, :, 0], identity=ID32[:]), reads=["TABT", "ID32"], writes=[("PS", 2)])
        S.op("act", lambda e: e.copy(out=SLTF[:], in_=PS[2][:, 0:128]), reads=[("PS", 2)], writes=["SLTF"])
        S.op("dve", lambda e: e.tensor_copy(out=SLTI[:], in_=SLTF[:]), reads=["SLTF"], writes=["SLTI"])

        npass = min(NPASS, (4 * SEQ + NE * (PSZ - 1)) // PSZ) if stage == "full" else (1 if stage in ("mA", "mB", "mC", "mD", "mC1", "mC2") else 0)

        def issue_loads(p):
            buf = p % 2
            for j in range(2):
                q = 2 * p + j
                S.dma("pool", lambda e, buf=buf, j=j, q=q: e.indirect_dma_start(
                    out=XG[:, buf, j, :], out_offset=None, in_=xn2_d, in_offset=bass.IndirectOffsetOnAxis(ap=SLTI[:, q:q + 1], axis=0),
                    bounds_check=bnd(e, SEQ - 1), oob_is_err=False), "xg%d" % buf,
                    reads=["SLTI", ("XG", buf)] + [("XN2D", lt) for lt in range(16)], writes=[("XG", buf)])
            S.op("dve", lambda e, buf=buf, p=p: e.tensor_scalar(out=GIDXF[:, buf, :], in0=SLTF[:, 2 * p:2 * p + 2], scalar1=32.0, scalar2=EP[:, p:p + 1],
                                                              op0=ALU.mult, op1=ALU.add), reads=["SLTF", "EP"], writes=[("GIDXF", buf)])
            S.op("dve", lambda e, buf=buf: e.tensor_copy(out=GIDXI[:, buf, :], in_=GIDXF[:, buf, :]), reads=[("GIDXF", buf)], writes=[("GIDXI", buf)])
            S.op("pool", lambda e, buf=buf: e.memset(GT[:, buf], 0.0), writes=[("GT", buf)])
            for j in range(2):
                S.dma("pool", lambda e, buf=buf, j=j: e.indirect_dma_start(
                    out=GT[:, buf, j, :], out_offset=None, in_=gates_d, in_offset=bass.IndirectOffsetOnAxis(ap=GIDXI[:, buf, j:j + 1], axis=0),
                    bounds_check=bnd(e, SEQ * NE - 1), oob_is_err=False), "gt%d" % buf, reads=[("GIDXI", buf), ("GT", buf), "GATESD"], writes=[("GT", buf)])
            S.op("dve", lambda e, buf=buf: e.tensor_scalar(out=GT[:, buf], in0=GT[:, buf], scalar1=1.0 / 1.702, scalar2=None, op0=ALU.mult),
                 reads=[("GT", buf)], writes=[("GT", buf)])
            if stage == "mA":
                return
            if p in pre_w:
                return
            issue_weights(p)

        if npass > 0:
            issue_loads(0)
        for p in range(npass):
            buf = p % 2
            sl = p % 2
            if p + 1 < npass:
                issue_loads(p + 1)
            if stage in ("mA", "mB"):
                continue
            for pr in range(4):
                bank = pr % 2
                for h in range(2):
                    kc = 2 * pr + h
                    off = h * PSZ
                    for j in range(2):
                        S.op("pe", lambda e, kc=kc, j=j, bank=bank, off=off, buf=buf: e.transpose(out=PS[bank][:, off + j * 128:off + (j + 1) * 128],
                                                                                               in_=XG[:, buf, j, kc * 128:(kc + 1) * 128], identity=ID32[:]),
                             reads=[("XG", buf), "ID32"], writes=[("PS", bank)], sig=(h == 1 and j == 1))
                for h in range(2):
                    kc = 2 * pr + h
                    src = PS[bank][:, h * PSZ:(h + 1) * PSZ]
                    if bank == 0:
                        S.op("act", lambda e, kc=kc, src=src, buf=buf: e.activation(out=HG[:, buf, kc, :], in_=src, func=AF.Identity, bias=MODT[:, 3, kc, 0:1],
                                                                                  scale=S2[:, kc:kc + 1]), reads=[("PS", bank)], writes=[("HG", buf, kc)])
                    else:
                        S.op("dve", lambda e, kc=kc, src=src, buf=buf: e.tensor_scalar(out=HG[:, buf, kc, :], in0=src, scalar1=S2[:, kc:kc + 1],
                                                                                     scalar2=MODT[:, 3, kc, 0:1], op0=ALU.mult, op1=ALU.add),
                             reads=[("PS", bank)], writes=[("HG", buf, kc)])
            if stage == "mC1":
                continue
            for fc in range(8):
                gbk, ubk = 2 + fc % 2, 4 + fc % 2
                for kc in range(8):
                    S.op("pe", lambda e, kc=kc, fc=fc, gbk=gbk, sl=sl, buf=buf: e.matmul(PS[gbk][:, 0:PSZ], lhsT=W1Gs[:, sl, fc * 1024 + kc * 128:fc * 1024 + (kc + 1) * 128],
                                                                                      rhs=HG[:, buf, kc, :], start=(kc == 0), stop=(kc == 7)),
                         reads=[("w1g", sl), ("HG", buf, kc)], writes=[("PS", gbk)], sig=(kc == 7))
                for kc in range(8):
                    S.op("pe", lambda e, kc=kc, fc=fc, ubk=ubk, sl=sl, buf=buf: e.matmul(PS[ubk][:, 0:PSZ], lhsT=W1Us[:, sl, fc * 1024 + kc * 128:fc * 1024 + (kc + 1) * 128],
                                                                                      rhs=HG[:, buf, kc, :], start=(kc == 0), stop=(kc == 7)),
                         reads=[("w1u", sl), ("HG", buf, kc)], writes=[("PS", ubk)], sig=(kc == 7))
                gv, gvk = tmp()
                uv, uvk = tmp()
                sg, sgk = tmp()
                S.op("dve", lambda e, gv=gv, gbk=gbk, sl=sl, fc=fc: e.tensor_scalar(out=gv[:, 0:PSZ], in0=PS[gbk][:, 0:PSZ], scalar1=B1S[:, sl, 0, fc:fc + 1], scalar2=7.0,
                                                                                 op0=ALU.add, op1=ALU.min), reads=[("PS", gbk), ("B1S", sl)], writes=[gvk])
                S.op("act", lambda e, uv=uv, ubk=ubk, sl=sl, fc=fc: e.activation(out=uv[:, 0:PSZ], in_=PS[ubk][:, 0:PSZ], func=AF.Identity, bias=B1S[:, sl, 1, fc:fc + 1], scale=1.0),
                     reads=[("PS", ubk), ("B1S", sl)], writes=[uvk])
                S.op("act", lambda e, sg=sg, gv=gv: e.activation(out=sg[:, 0:PSZ], in_=gv[:, 0:PSZ], func=AF.Silu, scale=1.702), reads=[gvk], writes=[sgk])
                S.op("dve", lambda e, uv=uv: e.tensor_scalar(out=uv[:, 0:PSZ], in0=uv[:, 0:PSZ], scalar1=-7.0, scalar2=7.0, op0=ALU.max, op1=ALU.min), reads=[uvk], writes=[uvk])
                S.op("dve", lambda e, sg=sg, uv=uv, fc=fc: e.scalar_tensor_tensor(out=ACTT[:, fc, :], in0=uv[:, 0:PSZ], scalar=1.0, in1=sg[:, 0:PSZ], op0=ALU.add, op1=ALU.mult),
                     reads=[sgk, uvk], writes=[("ACTT", fc)])
            if stage == "mC2":
                continue
            for j in range(2):
                q = 2 * p + j
                for nh in range(2):
                    bank = 6 + nh
                    for fc in range(8):
                        S.op("pe", lambda e, fc=fc, j=j, nh=nh, bank=bank, sl=sl: e.matmul(PS[bank][:], lhsT=ACTT[:, fc, j * 128:(j + 1) * 128],
                                                                                        rhs=W2s[:, sl, fc * 1024 + nh * 512:fc * 1024 + (nh + 1) * 512],
                                                                                        start=(fc == 0), stop=(fc == 7)),
                             reads=[("w2", sl), ("ACTT", fc)], writes=[("PS", bank)], sig=(fc == 7))
                    if False:
                        S.op("act", lambda e, j=j, nh=nh, bank=bank, buf=buf: e.activation(out=YS[:, j, nh * 512:(nh + 1) * 512], in_=PS[bank][:], func=AF.Identity,
                                                                                        bias=0.0, scale=GT[:, buf, j, 0:1]),
                             reads=[("PS", bank), ("GT", buf)], writes=[("YS", j, nh)])
                    else:
                        S.op("dve", lambda e, j=j, nh=nh, bank=bank, buf=buf: e.tensor_scalar(out=YS[:, j, nh * 512:(nh + 1) * 512], in0=PS[bank][:], scalar1=GT[:, buf, j, 0:1],
                                                                                           scalar2=None, op0=ALU.mult),
                             reads=[("PS", bank), ("GT", buf)], writes=[("YS", j, nh)])
                if stage == "mC":
                    continue
                S.dma("sp", lambda e, j=j, q=q: e.dma_start(out=yslot_d[q * 128:(q + 1) * 128, :], in_=YS[:, j, :]),
                      "ysc%d" % j, reads=[("YS", j, 0), ("YS", j, 1)], writes=[("YSLOT", q)])

        S.barrier()
        FG = Region([(96, 104)]).alloc([128, 1024])
        TMP = Region([(120, 128)]).alloc([128, 4, 512])
        TMPn[0] = 4
        ACB = Region([(0, 8)]).alloc([128, 2, 1024])
        YG = Region([(8, 40)]).alloc([128, 2, 4, 1024])
        S.dma("sp", lambda e: e.dma_start(out=FG[:], in_=fgrow[0:1, :].to_broadcast([128, 1024])), "c5", writes=["FG"])
        out_tokens = []
        for lt in range(16):
            xb = lt % 3
            S.dma("sp", lambda e, lt=lt, xb=xb: e.dma_start(out=XB[:, xb], in_=x1_d[lt * 128:(lt + 1) * 128, :]), "xb%d" % xb,
                  reads=[("X1D", lt)], writes=[("XB", xb)])
            ab = lt % 2
            S.dma("sp", lambda e, lt=lt, ab=ab: e.dma_start(out=ACB[:, ab], in_=acc_d[lt * 128:(lt + 1) * 128, :]), "acb%d" % ab, reads=["ACCD"], writes=[("ACB", ab)])
            for k in range(4):
                S.dma("pool", lambda e, lt=lt, ab=ab, k=k: e.indirect_dma_start(
                    out=YG[:, ab, k, :], out_offset=None, in_=yslot_d, in_offset=bass.IndirectOffsetOnAxis(ap=DKI[:, lt * 4 + k:lt * 4 + k + 1], axis=0),
                    bounds_check=bnd(e, NSLOT - 1), oob_is_err=False), "yg%d" % ab, reads=["DKI", ("YG", ab)] + [("YSLOT", q) for q in range(2 * NPASS)],
                    writes=[("YG", ab)])
            for k in range(4):
                S.op("pool" if k % 2 == 0 else "dve", lambda e, ab=ab, k=k: e.tensor_tensor(out=ACB[:, ab], in0=ACB[:, ab], in1=YG[:, ab, k, :], op=ALU.add),
                     reads=[("ACB", ab), ("YG", ab)], writes=[("ACB", ab)])
            for nh in range(2):
                mo, mok = tmp()
                S.op("dve", lambda e, mo=mo, ab=ab, nh=nh: e.tensor_tensor(out=mo[:], in0=ACB[:, ab, nh * 512:(nh + 1) * 512], in1=MODB[:, 1, nh * 512:(nh + 1) * 512],
                                                                         op=ALU.mult), reads=[("ACB", ab), ("MODB", 1, nh)], writes=[mok])
                S.op("pool", lambda e, mo=mo, xb=xb, nh=nh: e.tensor_tensor(out=XB[:, xb, nh * 512:(nh + 1) * 512], in0=XB[:, xb, nh * 512:(nh + 1) * 512],
                                                                          in1=mo[:], op=ALU.add), reads=[mok, ("XB", xb)], writes=[("XB", xb)])
            col = 48 + (lt % 16)
            xt_ap = XB[:, xb]
            xkey = ("XB", xb)
            S.op("act", lambda e, xt_ap=xt_ap: e.activation(out=JUNK[:], in_=xt_ap, func=AF.Square), reads=[xkey], writes=["JUNK"])
            S.op("dve", lambda e, col=col: e.reduce_sum(out=SMALL[:, col:col + 1], in_=JUNK[:], axis=AXL.X), reads=["JUNK"], writes=[("SM", col)])
            S.op("act", lambda e, col=col: e.activation(out=SMALL[:, col:col + 1], in_=SMALL[:, col:col + 1], func=AF.Ln, bias=EPS, scale=1.0 / D),
                 reads=[("SM", col)], writes=[("SM", col)])
            S.op("act", lambda e, col=col: e.activation(out=SMALL[:, col:col + 1], in_=SMALL[:, col:col + 1], func=AF.Exp, scale=-0.5), reads=[("SM", col)], writes=[("SM", col)])
            S.op("dve", lambda e, col=col, xt_ap=xt_ap: e.scalar_tensor_tensor(out=xt_ap, in0=xt_ap, scalar=SMALL[:, col:col + 1], in1=FG[:], op0=ALU.mult, op1=ALU.mult),
                 reads=[xkey, ("SM", col), "FG"], writes=[xkey])
            tok = S.dma("sp", lambda e, lt=lt, xb=xb: e.dma_start(out=out[lt * 128:(lt + 1) * 128, :], in_=XB[:, xb]), "xb%d" % xb, reads=[xkey], writes=[("OUT", lt)])
            out_tokens.append(tok)
        S.wait_all("sp", out_tokens)
        S.emit()
    return nc


def _fm(v):
    v = np.asarray(v, np.float32).reshape(-1, 128)
    return np.ascontiguousarray(v.T)


def pack_shared(inp):
    f32 = np.float32
    cp = np.zeros((128, NCP), f32)

    def put(name, arr):
        arr = np.asarray(arr, f32)
        cp[:, CO[name]:CO[name] + arr.shape[1]] = arr

    put("n1g", _fm(inp["norm1_g"][0]))
    put("n2g", _fm(inp["norm2_g"][0]))
    put("convw", np.concatenate([_fm(inp["lru_conv_w"][0, k]) for k in range(4)], axis=1))
    put("convb", _fm(inp["lru_conv_b"][0]))
    put("ba", np.concatenate([_fm(inp["lru_ba"][0, d].reshape(-1)) for d in range(2)], axis=1))
    put("bx", np.concatenate([_fm(inp["lru_bx"][0, d].reshape(-1)) for d in range(2)], axis=1))
    put("lam", np.concatenate([_fm(inp["lru_lam"][0, d]) for d in range(2)], axis=1))
    put("lb0", _fm(inp["hg_lb_logits"][0]))
    put("lb1", _fm(inp["hg_lb_logits"][1]))
    put("hgng", np.asarray(inp["hg_norm_g"][0], f32).reshape(128, 1))
    put("adab", _fm(inp["ada_b"][0]))
    b1 = np.asarray(inp["moe_b1"][0], f32)
    b1g = b1[:, 0::2].reshape(NE, 8, 128)
    b1u = b1[:, 1::2].reshape(NE, 8, 128)
    put("b1g", np.ascontiguousarray(b1g.transpose(2, 0, 1)).reshape(128, NE * 8))
    put("b1u", np.ascontiguousarray(b1u.transpose(2, 0, 1)).reshape(128, NE * 8))

    def kmajor(w):
        w = np.asarray(w, f32)
        return np.ascontiguousarray(w.reshape(8, 128, w.shape[1]).transpose(1, 0, 2))

    sh = {"cpack": cp}
    aw = np.asarray(inp["ada_w"][0], f32)
    sh["adaw"] = np.ascontiguousarray(aw.reshape(8, 128, 6, 1024).transpose(2, 1, 0, 3))
    sh["adabrow"] = np.asarray(inp["ada_b"][0], f32).reshape(1, 6144)
    wi = np.asarray(inp["w_in"][0], f32).reshape(8, 128, 9, 8, 128)
    sh["win"] = np.ascontiguousarray(wi[:, :, 0:7].transpose(3, 1, 2, 0, 4))
    sh["win78"] = np.ascontiguousarray(wi[:, :, 7:9].transpose(3, 1, 2, 0, 4))
    wa = np.asarray(inp["lru_wa"][0], f32)
    wx = np.asarray(inp["lru_wx"][0], f32)
    lw = np.stack([wa[0], wx[0], wa[1], wx[1]], axis=0)
    sh["lruw"] = np.ascontiguousarray(lw.transpose(2, 1, 0, 3))
    sh["wba"] = kmajor(inp["w_branch_a"][0])
    sh["wbb"] = kmajor(inp["w_branch_b"][0])
    sh["wout"] = kmajor(inp["w_out"][0])
    sh["fgrow"] = np.asarray(inp["final_g"], f32).reshape(1, 1024)
    sh["rw"] = kmajor(inp["router_w"][0])
    sh["rbrow"] = np.asarray(inp["router_b"][0], f32).reshape(1, 32)
    w1 = np.asarray(inp["moe_w1"][0], f32)
    w1r = w1.reshape(NE, 8, 128, 8, 128, 2)
    sh["w1g"] = np.ascontiguousarray(w1r[..., 0].transpose(0, 2, 3, 1, 4)).reshape(NE * 128, 8192)
    sh["w1u"] = np.ascontiguousarray(w1r[..., 1].transpose(0, 2, 3, 1, 4)).reshape(NE * 128, 8192)
    w2_ = np.asarray(inp["moe_w2"][0], f32)
    sh["w2"] = np.ascontiguousarray(w2_.reshape(NE, 8, 128, 1024).transpose(0, 2, 1, 3)).reshape(NE * 128, 8192)
    sh["b1gt"] = np.ascontiguousarray(b1g.transpose(0, 2, 1)).reshape(NE * 128, 8)
    sh["b1ut"] = np.ascontiguousarray(b1u.transpose(0, 2, 1)).reshape(NE * 128, 8)
    ic = np.zeros((128, 256), f32)
    ic[:, 0:32] = np.arange(32)[None, :]
    ic[:, 32:64] = 64 - np.arange(32)[None, :]
    ic[:, 64:128] = (PSZ * np.arange(NPASS))[None, :]
    ic[:, 128] = np.arange(128)
    ic[:, 129:145] = np.arange(16)[None, :] * 128 + np.arange(128)[:, None]
    ic[:, 145:177] = 1.0
    ic[:, 177:209] = np.repeat(np.arange(16)[None, :] * 128 + np.arange(128)[:, None], 2, axis=1)
    sh["iconst"] = ic
    sh["b2"] = np.ascontiguousarray(np.asarray(inp["moe_b2"][0], f32))
    return sh


def pack_core(inp, b):
    f32 = np.float32
    xin = np.concatenate([np.asarray(inp["ctx"][b], f32), np.asarray(inp["x"][b], f32)], axis=0)
    cv = np.stack([_fm(inp["c"][b]), _fm(inp["c_ctx"])], axis=2)
    return {"xin": np.ascontiguousarray(xin), "cvec": np.ascontiguousarray(cv)}


def kernel(**inputs):
    n = 8
    sh = pack_shared(inputs)
    in_maps = []
    for b in range(n):
        m = dict(sh)
        m.update(pack_core(inputs, b))
        in_maps.append(m)
    nc = build_nc("full")
    res = run_bass_kernel_spmd(nc, in_maps, core_ids=list(range(n)))
    return np.stack([np.asarray(r["out"], np.float32) for r in res.results], axis=0)
```
